# Optimizing a Trainium2 kernel written in Bass

```python
import math
import jax, jax.numpy as jnp
from jax import lax
import numpy as np

D_MODEL = 1024
BATCH = 2
SEQ = 8192
DEPTH = 2

HEAD_DIM = 64
N_HEADS = D_MODEL // HEAD_DIM
N_HEADS_SB = N_HEADS // 2
N_HEADS_DIL = N_HEADS - N_HEADS_SB
D_SB = N_HEADS_SB * HEAD_DIM
D_DIL = N_HEADS_DIL * HEAD_DIM
DIL_PATTERNS = ((128, 1), (512, 4), (2048, 16))
BLOCK = 128
N_BUCKETS = 32
MAX_DISTANCE = 2048
N_EXPERTS = 16
N_GROUPS = 4
EXPERTS_PER_GROUP = N_EXPERTS // N_GROUPS
TOP_K = 2
D_FF_EXPERT = 1024
ALPHA = (2.0 * DEPTH) ** 0.25
BETA = (8.0 * DEPTH) ** -0.25
LN_EPS = 1e-5
NEG_INF = -1e30

kernel_name = "hymba_style_sb_dilated_grouped_moe_deepnorm"


def layer_norm(x, g, b):
    xf = x.astype(jnp.float32)
    mu = xf.mean(-1, keepdims=True)
    var = jnp.square(xf - mu).mean(-1, keepdims=True)
    return ((xf - mu) * lax.rsqrt(var + LN_EPS) * g.astype(jnp.float32) + b.astype(jnp.float32)).astype(x.dtype)


def head_rms_merge(o, g):
    b, h, s, dh = o.shape
    of = o.astype(jnp.float32)
    of = of * lax.rsqrt(jnp.mean(jnp.square(of), -1, keepdims=True) + 1e-6)
    of = of.transpose(0, 2, 1, 3).reshape(b, s, h * dh)
    return (of * g.astype(jnp.float32)).astype(o.dtype)


def t5_bucket(dist):
    max_exact = N_BUCKETS // 2
    d = jnp.maximum(dist, 0)
    large = max_exact + (jnp.log(jnp.maximum(d, 1).astype(jnp.float32) / max_exact)
                         / math.log(MAX_DISTANCE / max_exact) * (N_BUCKETS - max_exact)).astype(jnp.int32)
    large = jnp.minimum(large, N_BUCKETS - 1)
    return jnp.where(d < max_exact, d, large)


def stick_breaking_attention(q, k, v):
    b, h, s, dh = q.shape
    nb = s // BLOCK
    scale = dh ** -0.5
    qb = q.reshape(b, h, nb, BLOCK, dh).transpose(2, 0, 1, 3, 4)
    kpos = jnp.arange(s)

    def one_block(args):
        qblk, n = args
        z = jnp.einsum('bhqd,bhkd->bhqk', qblk, k).astype(jnp.float32) * scale
        qpos = n * BLOCK + jnp.arange(BLOCK)
        causal = kpos[None, :] < qpos[:, None]
        log_beta = jax.nn.log_sigmoid(z)
        log_1m = jnp.where(causal, jax.nn.log_sigmoid(-z), 0.0)
        between = lax.cumsum(log_1m, axis=3, reverse=True) - log_1m
        a = jnp.where(causal, jnp.exp(log_beta + between), 0.0)
        return jnp.einsum('bhqk,bhkd->bhqd', a.astype(v.dtype), v)

    out = lax.map(one_block, (qb, jnp.arange(nb)))
    return out.transpose(1, 2, 0, 3, 4).reshape(b, h, s, dh)


def dilated_branch(q, k, v, rel_bias, window, dilation):
    b, h, s, dh = q.shape
    span = window // dilation
    n_cls = s // dilation
    nb = -(-n_cls // BLOCK)
    pad = nb * BLOCK - n_cls

    def to_classes(t):
        t = t.reshape(b, h, n_cls, dilation, dh).transpose(0, 1, 3, 2, 4)
        t = jnp.pad(t, ((0, 0), (0, 0), (0, 0), (0, pad), (0, 0)))
        return t.reshape(b, h, dilation, nb, BLOCK, dh)

    def with_prev(t):
        prev = jnp.pad(t, ((0, 0), (0, 0), (0, 0), (1, 0), (0, 0), (0, 0)))[:, :, :, :nb]
        return jnp.concatenate([prev, t], axis=4)

    qc = to_classes(q)
    kk = with_prev(to_classes(k))
    vv = with_prev(to_classes(v))
    logits = jnp.einsum('bhrnqd,bhrnkd->bhrnqk', qc, kk).astype(jnp.float32) * dh ** -0.5

    qi = jnp.arange(BLOCK)[:, None]
    kj = jnp.arange(2 * BLOCK)[None, :]
    steps = qi + BLOCK - kj
    bias = rel_bias[t5_bucket(jnp.maximum(steps, 0) * dilation)].astype(jnp.float32)
    bias = bias.transpose(2, 0, 1)
    key_cls = (jnp.arange(nb)[:, None, None] - 1) * BLOCK + kj[None]
    valid = (steps >= 0) & (steps <= span) & (key_cls >= 0)
    logits = jnp.where(valid, logits + bias[None, :, None, None], NEG_INF)

    m = logits.max(-1, keepdims=True)
    p = jnp.exp(logits - m)
    denom = p.sum(-1, keepdims=True)
    o = jnp.einsum('bhrnqk,bhrnkd->bhrnqd', p.astype(vv.dtype), vv).astype(jnp.float32) / denom
    lse = (m + jnp.log(denom))[..., 0]

    def from_classes(t):
        t = t.reshape((b, h, dilation, nb * BLOCK) + t.shape[5:])[:, :, :, :n_cls]
        t = jnp.moveaxis(t, 2, 3)
        return t.reshape((b, h, s) + t.shape[4:])

    return from_classes(o), from_classes(lse)


def dilated_attention(q, k, v, rel_bias):
    outs, lses = [], []
    for window, dilation in DIL_PATTERNS:
        o, l = dilated_branch(q, k, v, rel_bias, window, dilation)
        outs.append(o)
        lses.append(l)
    w = jax.nn.softmax(jnp.stack(lses, 0), axis=0)
    return jnp.einsum('gbhs,gbhsd->bhsd', w, jnp.stack(outs, 0)).astype(q.dtype)


def token_mixer(x, w_in, g_sb, g_dil, w_out, rel_bias):
    b, s, _ = x.shape
    proj = jnp.einsum('bsd,de->bse', x, w_in)
    q_sb, k_sb, v_sb, q_dl, k_dl, v_dl = jnp.split(
        proj, [D_SB, 2 * D_SB, 3 * D_SB, 3 * D_SB + D_DIL, 3 * D_SB + 2 * D_DIL], axis=-1)
    to_heads = lambda t: t.reshape(b, s, -1, HEAD_DIM).transpose(0, 2, 1, 3)
    o_sb = stick_breaking_attention(to_heads(q_sb), to_heads(k_sb), to_heads(v_sb))
    o_dl = dilated_attention(to_heads(q_dl), to_heads(k_dl), to_heads(v_dl), rel_bias)
    y = jnp.concatenate([head_rms_merge(o_sb, g_sb), head_rms_merge(o_dl, g_dil)], axis=-1)
    return jnp.einsum('bse,ed->bsd', y, w_out)


def grouped_moe(x, w_router, b_router, w_gate, w_up, w_down):
    b, s, d = x.shape
    t = x.reshape(b * s, d)
    logits = (t @ w_router).astype(jnp.float32) + b_router.astype(jnp.float32)
    probs = jax.nn.softmax(logits, axis=-1)
    pg = probs.reshape(-1, N_GROUPS, EXPERTS_PER_GROUP)
    vals, idx = lax.top_k(pg, TOP_K)
    g_sel = jnp.argmax(vals.sum(-1), axis=-1)
    vals_sel = jnp.take_along_axis(vals, g_sel[:, None, None], axis=1)[:, 0]
    idx_sel = jnp.take_along_axis(idx, g_sel[:, None, None], axis=1)[:, 0]
    expert_ids = g_sel[:, None] * EXPERTS_PER_GROUP + idx_sel
    gates = vals_sel / vals_sel.sum(-1, keepdims=True)
    gate_dense = (jax.nn.one_hot(expert_ids, N_EXPERTS, dtype=jnp.float32) * gates[..., None]).sum(1)
    y = jnp.zeros_like(t)
    for e in range(N_EXPERTS):
        h = jax.nn.silu(t @ w_gate[e]) * (t @ w_up[e])
        y = y + gate_dense[:, e:e + 1].astype(t.dtype) * (h @ w_down[e])
    return y.reshape(b, s, d)


def setup_inputs(seed: int = 0) -> dict:
    key = jax.random.key(seed)
    ks = jax.random.split(key, 20)
    f32 = jnp.float32
    d = D_MODEL
    s_in = d ** -0.5
    nrm = lambda k, shape: jax.random.normal(k, shape, f32)
    x = nrm(ks[0], (BATCH, SEQ, d))

    def qkv_cols(k, width):
        kq, kk, kv = jax.random.split(k, 3)
        return [nrm(kq, (DEPTH, d, width)) * s_in,
                nrm(kk, (DEPTH, d, width)) * s_in,
                nrm(kv, (DEPTH, d, width)) * (s_in * BETA)]

    w_in = jnp.concatenate(qkv_cols(ks[1], D_SB) + qkv_cols(ks[2], D_DIL), axis=-1)
    g_sb = 1.0 + 0.05 * nrm(ks[3], (DEPTH, D_SB))
    g_dil = 1.0 + 0.05 * nrm(ks[4], (DEPTH, D_DIL))
    w_out = nrm(ks[5], (DEPTH, d, d)) * (s_in * BETA)
    ln1_g = 1.0 + 0.05 * nrm(ks[6], (DEPTH, d))
    ln1_b = 0.02 * nrm(ks[7], (DEPTH, d))
    ln2_g = 1.0 + 0.05 * nrm(ks[8], (DEPTH, d))
    ln2_b = 0.02 * nrm(ks[9], (DEPTH, d))
    rel_bias = 0.5 * nrm(ks[10], (N_BUCKETS, N_HEADS_DIL))
    w_router = nrm(ks[11], (d, N_EXPERTS)) * s_in
    b_router = 0.01 * nrm(ks[12], (N_EXPERTS,))
    w_gate = nrm(ks[13], (DEPTH, N_EXPERTS, d, D_FF_EXPERT)) * s_in
    w_up = nrm(ks[14], (DEPTH, N_EXPERTS, d, D_FF_EXPERT)) * (s_in * BETA)
    w_down = nrm(ks[15], (DEPTH, N_EXPERTS, D_FF_EXPERT, d)) * (D_FF_EXPERT ** -0.5 * BETA)
    return {"x": x, "w_in": w_in, "g_sb": g_sb, "g_dil": g_dil, "w_out": w_out,
            "ln1_g": ln1_g, "ln1_b": ln1_b, "ln2_g": ln2_g, "ln2_b": ln2_b,
            "rel_bias": rel_bias, "w_router": w_router, "b_router": b_router,
            "w_gate": w_gate, "w_up": w_up, "w_down": w_down}


def reference(x, w_in, g_sb, g_dil, w_out, ln1_g, ln1_b, ln2_g, ln2_b,
              rel_bias, w_router, b_router, w_gate, w_up, w_down):
    for l in range(DEPTH):
        h = token_mixer(x, w_in[l], g_sb[l], g_dil[l], w_out[l], rel_bias)
        x = layer_norm(ALPHA * x + h, ln1_g[l], ln1_b[l])
        h = grouped_moe(x, w_router, b_router, w_gate[l], w_up[l], w_down[l])
        x = layer_norm(ALPHA * x + h, ln2_g[l], ln2_b[l])
    return x
```

```python
from contextlib import ExitStack
from concourse.bass_utils import run_bass_kernel_spmd
import numpy as np
import concourse.bass as bass
import concourse.mybir as mybir

F32 = mybir.dt.float32
BF16 = mybir.dt.bfloat16
AF = mybir.ActivationFunctionType
ALU = mybir.AluOpType

COMPUTE = ("pe", "act", "dve", "pool")


class Sched:
    def __init__(self, nc):
        self.nc = nc
        self.ops = []
        self.last_writer = {}
        self.readers = {}
        self.dma_groups = {}

    def add(self, eng, fn, reads=(), writes=(), grp=None):
        idx = len(self.ops)
        deps = set()
        for b in reads:
            w = self.last_writer.get(b)
            if w is not None:
                deps.add(w)
        for b in writes:
            w = self.last_writer.get(b)
            if w is not None:
                deps.add(w)
            for r in self.readers.get(b, ()):
                deps.add(r)
        deps.discard(idx)
        is_dma = grp is not None
        if eng == "pe":
            deps = {d for d in deps if not (self.ops[d]["eng"] == "pe" and not self.ops[d]["dma"])}
        op = dict(eng=eng, fn=fn, deps=deps, dma=is_dma, grp=grp, needed=False)
        self.ops.append(op)
        for b in reads:
            self.readers.setdefault(b, []).append(idx)
        for b in writes:
            self.last_writer[b] = idx
            self.readers[b] = []
        return idx

    def emit(self, final_wait_ops=()):
        nc = self.nc
        ops = self.ops
        for op in ops:
            for d in op["deps"]:
                ops[d]["needed"] = True
        for d in final_wait_ops:
            ops[d]["needed"] = True
        sem_names = []
        counters = {}
        for i, op in enumerate(ops):
            if not op["needed"]:
                continue
            key = ("dma", op["grp"]) if op["dma"] else ("eng", op["eng"])
            if key not in counters:
                counters[key] = 0
                sem_names.append(key)
            counters[key] += 16 if op["dma"] else 1
            op["sem"] = key
            op["val"] = counters[key]
        self.sem_keys = sem_names
        return sem_names

    def run(self, sems, final_wait_ops=()):
        nc = self.nc
        ops = self.ops
        per_eng = {}
        for i, op in enumerate(ops):
            per_eng.setdefault(op["eng"], []).append(i)

        def body(engname, eng):
            known = {}
            for i in per_eng.get(engname, []):
                op = ops[i]
                need = {}
                for d in op["deps"]:
                    p = ops[d]
                    k = p["sem"]
                    need[k] = max(need.get(k, 0), p["val"])
                for k, v in need.items():
                    if known.get(k, 0) >= v:
                        continue
                    eng.wait_ge(sems[k], v)
                    known[k] = v
                ins = op["fn"](eng)
                if op["needed"]:
                    ins.then_inc(sems[op["sem"]], 16 if op["dma"] else 1)
            if engname == "sp":
                fin = {}
                for d in final_wait_ops:
                    p = ops[d]
                    fin[p["sem"]] = max(fin.get(p["sem"], 0), p["val"])
                for k, v in fin.items():
                    if known.get(k, 0) < v:
                        eng.wait_ge(sems[k], v)
                        known[k] = v

        with nc.Block() as block:
            @block.sync
            def _(e):
                body("sp", e)

            @block.tensor
            def _(e):
                body("pe", e)

            @block.scalar
            def _(e):
                body("act", e)

            @block.vector
            def _(e):
                body("dve", e)

            @block.gpsimd
            def _(e):
                body("pool", e)


import math
import numpy as np
import ml_dtypes
import concourse.bass as bass
import concourse.mybir as mybir

NEG = -30000.0
SKIP = ['dveonly']
SC = 2048


def t5_bucket_np(dist):
    max_exact = 16
    d = np.maximum(dist, 0)
    large = max_exact + (np.log(np.maximum(d, 1).astype(np.float32) / np.float32(max_exact))
                         / np.float32(math.log(2048 / max_exact)) * np.float32(32 - max_exact)).astype(np.int32)
    large = np.minimum(large, 31)
    return np.where(d < max_exact, d, large)


def consts_A():
    j = np.arange(128)[:, None]
    s = np.arange(128)[None, :]
    negtri = np.where(j >= s, -1.0, 0.0).astype(ml_dtypes.bfloat16)
    negones = np.full((128, 128), -1.0, dtype=ml_dtypes.bfloat16)
    ident = np.eye(128, dtype=ml_dtypes.bfloat16)
    t = np.arange(512)[None, :]
    sbmask = np.stack([np.where(128 * a + j >= t, NEG, 0.0) for a in range(4)], 0).astype(ml_dtypes.bfloat16)
    onesblk = np.zeros((128, 128), np.float32)
    onesblk[:64, :64] = 1.0
    onesblk[64:, 64:] = 1.0
    sel65 = np.zeros((65, 64), np.float32)
    sel65[64, :] = 1.0
    ones65 = np.zeros((65, 64), np.float32)
    ones65[:64, :] = 1.0
    return dict(negtri=negtri, negones=negones, ident=ident, sbmask=sbmask, onesblk=onesblk, sel65=sel65, ones65=ones65)


def dil_bias_tables(rel_bias_heads):
    kj = np.arange(128)[:, None]
    qi = np.arange(128)[None, :]
    out = np.zeros((3, 2, 128, 256), np.float32)
    for ri, r in enumerate((1, 4, 16)):
        steps_cur = qi - kj
        steps_prev = qi - kj + 128
        b_cur = t5_bucket_np(np.maximum(steps_cur, 0) * r)
        b_prev = t5_bucket_np(np.maximum(steps_prev, 0) * r)
        for h in range(2):
            cur = np.where(steps_cur >= 0, rel_bias_heads[b_cur, h], np.float32(NEG))
            prev = np.where(steps_prev <= 128, rel_bias_heads[b_prev, h], np.float32(NEG))
            out[ri, h, :, :128] = prev
            out[ri, h, :, 128:] = cur
    return out


class Arena:
    def __init__(self, ap_bf16):
        self.ap = ap_bf16
        self.off = 0
        self.total = ap_bf16.shape[1]

    def take(self, nelem, dtype=BF16):
        nb = nelem * (4 if dtype == F32 else 2)
        nb = (nb + 63) // 64 * 64
        n16 = nb // 2
        assert self.off + n16 <= self.total, ("arena overflow", self.off, n16, self.total)
        v = self.ap[:, self.off:self.off + n16]
        self.off += n16
        if dtype == F32:
            return v.bitcast(F32)[:, :nelem]
        return v[:, :nelem]


def emit_A(nc, s, ar, ps, D, S, src_is_f32, lname, stop=99):
    NSC = S // SC
    NB = S // 128
    L = lname
    K = lambda *a: (L,) + a

    Wb = ar.take(8 * 768).rearrange("p (f c) -> p f c", f=8)
    XT = ar.take(8 * SC).rearrange("p (f t) -> p f t", f=8)
    QTs = ar.take(SC)
    QTd = ar.take(SC)
    KTs = ar.take(S)
    KTd = ar.take(S)
    Vs = ar.take(NB * 128).rearrange("p (b c) -> p b c", c=128)
    NW1 = NB
    Vd1 = ar.take(NW1 * 132).rearrange("p (b h c) -> p b h c", h=2, c=66)
    Vd4 = ar.take(32 * 132).rearrange("p (b h c) -> p b h c", h=2, c=66)
    Vd16 = ar.take(32 * 132).rearrange("p (b h c) -> p b h c", h=2, c=66)
    negtri = ar.take(128)
    negones = ar.take(128)
    ident = ar.take(128)
    sbmask = ar.take(4 * 512).rearrange("p (a t) -> p a t", a=4)
    onesblk = ar.take(128, F32)
    sel65 = ar.take(64, F32)
    ones65 = ar.take(64, F32)
    gvec = ar.take(4, F32)
    biasm = ar.take(6 * 256, F32).rearrange("p (r h c) -> p r h c", r=3, h=2)
    E32 = [ar.take(512, F32) for _ in range(2)]
    Lp16 = [ar.take(512) for _ in range(3)]
    LA32 = [ar.take(512, F32) for _ in range(2)]
    A16 = [ar.take(512) for _ in range(3)]
    ncar = [ar.take(512, F32) for _ in range(2)]
    dT32 = [ar.take(512, F32) for _ in range(2)]
    P16 = [ar.take(512) for _ in range(4)]
    acc = [ar.take(SC, F32) for _ in range(2)]
    o32 = ar.take(512, F32)
    sq32 = ar.take(512, F32)
    r32 = ar.take(512, F32)
    Yb = [ar.take(512) for _ in range(2)]

    def dma(eng, out, in_, reads, writes, grp):
        return s.add(eng, lambda e: e.dma_start(out=out, in_=in_), reads=reads, writes=writes, grp=grp)

    for fc in range(8):
        dma("pool", Wb[:, fc, :], D["w"][fc * 128:(fc + 1) * 128, :], [], [K("W")], K("W"))
    dma("sp", negtri, D["negtri"], [], [K("c")], K("c"))
    dma("sp", negones, D["negones"], [], [K("c")], K("c"))
    dma("sp", ident, D["ident"], [], [K("c")], K("c"))
    for a in range(4):
        dma("sp", sbmask[:, a, :], D["sbmask"][a], [], [K("c")], K("c"))
    dma("sp", onesblk, D["onesblk"], [], [K("c")], K("c"))
    dma("sp", sel65[0:65, :], D["sel65"], [], [K("c")], K("c"))
    dma("sp", ones65[0:65, :], D["ones65"], [], [K("c")], K("c"))
    dma("sp", gvec[:, 0:1], D["g"][0:128, :], [], [K("c")], K("c"))
    dma("sp", gvec[0:64, 1:2], D["g"][128:192, :], [], [K("c")], K("c"))
    dma("sp", gvec[0:64, 2:3], D["g"][192:256, :], [], [K("c")], K("c"))
    for ri in range(3):
        for h in range(2):
            dma("sp", biasm[:, ri, h, :], D["biasm"][ri, h], [], [K("c")], K("c"))
    for vb in (Vd1, Vd4, Vd16):
        s.add("dve", (lambda vb: lambda e: e.memset(vb[:, :, :, 64:65], 1.0))(vb), writes=[K("vones")])

    psX = [ps[6], ps[7]]
    def dummy_out():
        return [dma("sp", D["yT"][0:128, 0:512], QTs[:, 0:512], [K("c"), K("W"), K("XT"), K("vones"), K("QTs"), K("QTd"), K("Vd4", 0), K("Vd16", 0), K("Vs", 0)], [], K("yout"))]
    if stop == 0:
        return dummy_out()
    xcnt = [0]

    def nextX():
        i = xcnt[0] % 2
        xcnt[0] += 1
        return i

    evac_cnt = [0]

    def evac(out, in_, reads, writes, scale=None):
        evac_cnt[0] += 1
        if evac_cnt[0] % 2 == 0 or 'dveonly' in SKIP:
            if scale is None:
                return s.add("dve", lambda e: e.tensor_copy(out=out, in_=in_), reads=reads, writes=writes)
            return s.add("dve", lambda e: e.tensor_scalar(out=out, in0=in_, scalar1=float(scale), scalar2=None, op0=ALU.mult),
                         reads=reads, writes=writes)
        sc = 1.0 if scale is None else float(scale)
        return s.add("act", lambda e: e.activation(out=out, in_=in_, func=AF.Copy, scale=sc), reads=reads, writes=writes)

    out_dmas = []
    ycnt = [0]

    def ss(start, n, step):
        return slice(start, start + (n - 1) * step + 1, step)

    for sc_i in range(NSC):
        t0 = sc_i * SC
        for fc in range(8):
            dma("pool", XT[:, fc, :], D["xT"][fc * 128:(fc + 1) * 128, t0:t0 + SC], [], [K("XT")], K("XT"))
        if stop == 1:
            return dummy_out()
        for c in range(4):
            tl = c * 512
            for kind, col0, dst, scale in (("qs", 0, QTs[:, tl:tl + 512], 0.125), ("ks", 128, KTs[:, t0 + tl:t0 + tl + 512], None),
                                           ("qd", 256, QTd[:, tl:tl + 512], 0.125), ("kd", 384, KTd[:, t0 + tl:t0 + tl + 512], None)):
                xi = nextX()
                for fc in range(8):
                    s.add("pe", (lambda xi, fc, col0, tl: lambda e: e.matmul(psX[xi][:, :], Wb[:, fc, col0:col0 + 128], XT[:, fc, tl:tl + 512],
                                                                             start=(fc == 0), stop=(fc == 7)))(xi, fc, col0, tl),
                          reads=[K("W"), K("XT")], writes=[("ps", 6 + xi)])
                wkey = {"qs": K("QTs"), "ks": K("KTs", sc_i), "qd": K("QTd"), "kd": K("KTd", sc_i)}[kind]
                evac(dst, psX[xi][:, :], [("ps", 6 + xi)], [wkey], scale)
            for sub in range(4 if 'vnat' not in SKIP else 0):
                tt = tl + sub * 128
                blk = (t0 + tt) // 128
                xi = nextX()
                for fc in range(8):
                    s.add("pe", (lambda xi, fc, tt: lambda e: e.matmul(psX[xi][:, 0:256], XT[:, fc, tt:tt + 128], Wb[:, fc, 512:768],
                                                                       start=(fc == 0), stop=(fc == 7)))(xi, fc, tt),
                          reads=[K("W"), K("XT")], writes=[("ps", 6 + xi)])
                if 'evs' not in SKIP: evac(Vs[:, blk, :], psX[xi][:, 0:128], [("ps", 6 + xi)], [K("Vs", blk)])
                for hh in range(2 if 'evd1' not in SKIP else 0):
                    evac(Vd1[:, blk, hh, 0:64], psX[xi][:, 128 + 64 * hh:192 + 64 * hh], [("ps", 6 + xi)], [K("Vd1", blk)])
            n4 = 4 * sc_i + c
            xi = nextX()
            for cls in range(4 if 'v4' not in SKIP else 0):
                for fc in range(8):
                    s.add("pe", (lambda xi, fc, cls, tl: lambda e: e.matmul(psX[xi][:, cls * 128:(cls + 1) * 128],
                                                                            XT[:, fc, ss(tl + cls, 128, 4)], Wb[:, fc, 640:768],
                                                                            start=(fc == 0), stop=(fc == 7)))(xi, fc, cls, tl),
                          reads=[K("W"), K("XT")], writes=[("ps", 6 + xi)])
            slot0 = (n4 % 8) * 4
            for kk_ in range(4 if 'v4' not in SKIP else 0):
                for hh in range(2):
                    evac(Vd4[:, slot0 + kk_, hh, 0:64], psX[xi][:, kk_ * 128 + 64 * hh:kk_ * 128 + 64 * hh + 64], [("ps", 6 + xi)], [K("Vd4", n4 % 8)])
        for c4 in range(4 if 'v16' not in SKIP else 0):
            xi = nextX()
            for k in range(4):
                cls = c4 * 4 + k
                for fc in range(8):
                    s.add("pe", (lambda xi, fc, cls, k: lambda e: e.matmul(psX[xi][:, k * 128:(k + 1) * 128],
                                                                           XT[:, fc, ss(cls, 128, 16)], Wb[:, fc, 640:768],
                                                                           start=(fc == 0), stop=(fc == 7)))(xi, fc, cls, k),
                          reads=[K("W"), K("XT")], writes=[("ps", 6 + xi)])
            slot0 = (sc_i % 2) * 16 + c4 * 4
            for kk_ in range(4 if 'v16' not in SKIP else 0):
                for hh in range(2):
                    evac(Vd16[:, slot0 + kk_, hh, 0:64], psX[xi][:, kk_ * 128 + 64 * hh:kk_ * 128 + 64 * hh + 64], [("ps", 6 + xi)], [K("Vd16", sc_i % 2)])

        if stop == 2:
            return dummy_out()
        pcnt = [0]
        tcnt = [0]
        for h in range(2):
            hp = slice(64 * h, 64 * h + 64)
            for ri, r in enumerate((1, 4, 16)):
                nblk_sc = SC // (128 * r)
                vkey = {1: "Vd1", 4: "Vd4", 16: "Vd16"}[r]
                for c in range(r):
                    n0 = sc_i * nblk_sc
                    for g0 in range(n0, n0 + nblk_sc, 4):
                        blks = list(range(g0, min(g0 + 4, n0 + nblk_sc)))
                        units = []
                        for n in blks:
                            units += [(n - 1, n), (n, n)]
                        uinfo = []
                        for t_i in range(0, len(units), 4):
                            tu = units[t_i:t_i + 4]
                            xi = nextX()
                            pslot = pcnt[0] % 4
                            pcnt[0] += 1
                            tslot = tcnt[0] % 2
                            tcnt[0] += 1
                            lo = None
                            for u, (kb, n) in enumerate(tu):
                                co = u * 128
                                uinfo.append((pslot, co, kb))
                                if kb < 0:
                                    continue
                                if lo is None:
                                    lo = co
                                qstart = c + r * 128 * n - t0
                                kstart = c + r * 128 * kb
                                s.add("pe", (lambda xi, co, kstart, qstart, r, hp: lambda e: e.matmul(
                                    psX[xi][:, co:co + 128], KTd[hp, ss(kstart, 128, r)], QTd[hp, ss(qstart, 128, r)],
                                    start=True, stop=True))(xi, co, kstart, qstart, r, hp),
                                    reads=[K("KTd", kstart // SC), K("QTd")], writes=[("ps", 6 + xi)])
                            hi = len(tu) * 128
                            for half in range(0, hi, 256):
                                a0 = max(half, lo)
                                a1 = half + 256
                                bo = a0 - half
                                s.add("dve", (lambda xi, a0, a1, bo, tslot, ri, h: lambda e: e.tensor_tensor(
                                    out=dT32[tslot][:, a0:a1], in0=psX[xi][:, a0:a1], in1=biasm[:, ri, h, bo:256], op=ALU.add))(
                                    xi, a0, a1, bo, tslot, ri, h),
                                    reads=[("ps", 6 + xi), K("c")], writes=[K("dT", tslot)])
                            s.add("act", (lambda tslot, pslot, lo, hi: lambda e: e.activation(out=P16[pslot][:, lo:hi], in_=dT32[tslot][:, lo:hi], func=AF.Exp))(
                                tslot, pslot, lo, hi), reads=[K("dT", tslot)], writes=[K("P", pslot)])
                        for bi, n in enumerate(blks):
                            first = True
                            for uu in (2 * bi, 2 * bi + 1):
                                pslot, co, kb = uinfo[uu]
                                if kb < 0:
                                    continue
                                if r == 1:
                                    vap = Vd1[:, kb, h, 0:65]
                                elif r == 4:
                                    vap = Vd4[:, (kb % 8) * 4 + c, h, 0:65]
                                else:
                                    vap = Vd16[:, (kb % 2) * 16 + c, h, 0:65]
                                last = (uu == 2 * bi + 1)
                                s.add("pe", (lambda bi, vap, pslot, co, first, last: lambda e: e.matmul(
                                    ps[4][0:65, bi * 128:(bi + 1) * 128], vap, P16[pslot][:, co:co + 128],
                                    start=first, stop=last))(bi, vap, pslot, co, first, last),
                                    reads=[K("P", pslot), K(vkey, kb if r == 1 else (kb % 8 if r == 4 else kb % 2)), K("vones")], writes=[("ps", 4)])
                                first = False
                        nb_ = len(blks)
                        col = c + r * (128 * g0) - t0
                        if r == 1:
                            s.add("dve", (lambda h, col, nb_: lambda e: e.tensor_copy(out=acc[h][0:65, col:col + 128 * nb_], in_=ps[4][0:65, 0:128 * nb_]))(h, col, nb_),
                                  reads=[("ps", 4)], writes=[K("acc", h)])
                        else:
                            s.add("dve", (lambda h, col, nb_, r: lambda e: e.tensor_tensor(
                                out=acc[h][0:65, ss(col, 128 * nb_, r)], in0=ps[4][0:65, 0:128 * nb_], in1=acc[h][0:65, ss(col, 128 * nb_, r)], op=ALU.add))(h, col, nb_, r),
                                reads=[("ps", 4), K("acc", h)], writes=[K("acc", h)])
            for tq in range(4):
                cs = slice(tq * 512, tq * 512 + 512)
                xi = nextX()
                s.add("pe", (lambda xi, h, cs: lambda e: e.matmul(psX[xi][0:64, :], sel65[0:65, :], acc[h][0:65, cs], start=True, stop=True))(xi, h, cs),
                      reads=[K("acc", h), K("c")], writes=[("ps", 6 + xi)])
                s.add("dve", (lambda xi: lambda e: e.reciprocal(out=r32[0:64, :], in_=psX[xi][0:64, :]))(xi), reads=[("ps", 6 + xi)], writes=[K("r32")])
                s.add("dve", (lambda h, cs: lambda e: e.tensor_tensor(out=o32[0:64, :], in0=acc[h][0:64, cs], in1=r32[0:64, :], op=ALU.mult))(h, cs),
                      reads=[K("acc", h), K("r32")], writes=[K("o32")])
                s.add("act", lambda e: e.activation(out=sq32[0:64, :], in_=o32[0:64, :], func=AF.Square), reads=[K("o32")], writes=[K("sq32")])
                xi2 = nextX()
                s.add("pe", (lambda xi2: lambda e: e.matmul(psX[xi2][0:64, :], ones65[0:64, :], sq32[0:64, :], start=True, stop=True))(xi2),
                      reads=[K("sq32"), K("c")], writes=[("ps", 6 + xi2)])
                s.add("act", (lambda xi2: lambda e: e.activation(out=r32[0:64, :], in_=psX[xi2][0:64, :], func=AF.Sqrt, scale=1.0 / 64, bias=1e-6))(xi2),
                      reads=[("ps", 6 + xi2)], writes=[K("r32")])
                s.add("dve", lambda e: e.reciprocal(out=sq32[0:64, :], in_=r32[0:64, :]), reads=[K("r32")], writes=[K("sq32")])
                yi = ycnt[0] % 2
                ycnt[0] += 1
                s.add("dve", (lambda yi, h: lambda e: e.scalar_tensor_tensor(out=Yb[yi][0:64, :], in0=o32[0:64, :], scalar=gvec[0:64, 1 + h:2 + h],
                                                                              in1=sq32[0:64, :], op0=ALU.mult, op1=ALU.mult))(yi, h),
                      reads=[K("o32"), K("sq32"), K("c")], writes=[K("Yb", yi)])
                od = dma("sp", D["yT"][128 + 64 * h:128 + 64 * h + 64, t0 + tq * 512:t0 + tq * 512 + 512], Yb[yi][0:64, :], [K("Yb", yi)], [], K("yout"))
                out_dmas.append(od)

        if stop == 3:
            return out_dmas
        jobs = []
        for qt in range(4 * sc_i, 4 * sc_i + 4):
            kbl = list(range(4 * qt + 3, -1, -1))
            for ki, kb in enumerate(kbl):
                for h in range(2):
                    jobs.append(dict(qt=qt, kb=kb, h=h, first=(ki == 0), last=(ki == len(kbl) - 1), idx=len(jobs)))

        def stage1(j):
            qt, kb, h, i = j["qt"], j["kb"], j["h"], j["idx"]
            hp = slice(64 * h, 64 * h + 64)
            a = kb - 4 * qt
            sa, sb_, se, sl = i % 2, i % 2, i % 2, i % 3
            ql = (qt - 4 * sc_i) * 512
            diag = a >= 0
            kk = K("KTs", (kb * 128) // SC)
            s.add("pe", lambda e: e.matmul(ps[sa][:, :], KTs[hp, kb * 128:(kb + 1) * 128], QTs[hp, ql:ql + 512], start=True, stop=not diag),
                  reads=[kk, K("QTs")], writes=[("ps", sa)])
            if diag:
                s.add("pe", lambda e: e.matmul(ps[sa][:, :], ident, sbmask[:, a, :], start=False, stop=True), reads=[K("c")], writes=[("ps", sa)])
            s.add("pe", lambda e: e.matmul(ps[2 + sb_][:, :], KTs[hp, kb * 128:(kb + 1) * 128], QTs[hp, ql:ql + 512], start=True, stop=False),
                  reads=[kk, K("QTs")], writes=[("ps", 2 + sb_)])
            if diag:
                s.add("pe", lambda e: e.matmul(ps[2 + sb_][:, :], ident, sbmask[:, a, :], start=False, stop=False), reads=[K("c")], writes=[("ps", 2 + sb_)])
            s.add("act", lambda e: e.activation(out=E32[se], in_=ps[sa][:, :], func=AF.Exp), reads=[("ps", sa)], writes=[K("E", se)])
            s.add("act", lambda e: e.activation(out=Lp16[sl], in_=E32[se], func=AF.Ln, bias=1.0), reads=[K("E", se)], writes=[K("Lp", sl)])

        def stage2(j):
            qt, kb, h, i = j["qt"], j["kb"], j["h"], j["idx"]
            sb_, sl, sla, sa16 = i % 2, i % 3, i % 2, i % 3
            s.add("pe", lambda e: e.matmul(ps[2 + sb_][:, :], negtri, Lp16[sl], start=False, stop=True), reads=[K("Lp", sl), K("c")], writes=[("ps", 2 + sb_)])
            if not j["last"]:
                s.add("pe", lambda e: e.matmul(ps[5][:, :], negones, Lp16[sl], start=True, stop=True), reads=[K("Lp", sl), K("c")], writes=[("ps", 5)])
            if j["first"]:
                s.add("act", lambda e: e.activation(out=A16[sa16], in_=ps[2 + sb_][:, :], func=AF.Exp), reads=[("ps", 2 + sb_)], writes=[K("A", sa16)])
                if not j["last"]:
                    s.add("dve", lambda e: e.tensor_copy(out=ncar[h], in_=ps[5][:, :]), reads=[("ps", 5)], writes=[K("ncar", h)])
            else:
                s.add("dve", lambda e: e.tensor_tensor(out=LA32[sla], in0=ps[2 + sb_][:, :], in1=ncar[h], op=ALU.add),
                      reads=[("ps", 2 + sb_), K("ncar", h)], writes=[K("LA", sla)])
                if not j["last"]:
                    s.add("dve", lambda e: e.tensor_tensor(out=ncar[h], in0=ps[5][:, :], in1=ncar[h], op=ALU.add),
                          reads=[("ps", 5), K("ncar", h)], writes=[K("ncar", h)])
                s.add("act", lambda e: e.activation(out=A16[sa16], in_=LA32[sla], func=AF.Exp), reads=[K("LA", sla)], writes=[K("A", sa16)])

        def stage3(j):
            qt, kb, h, i = j["qt"], j["kb"], j["h"], j["idx"]
            sa16 = i % 3
            s.add("pe", lambda e: e.matmul(ps[4][64 * h:64 * h + 64, :], Vs[:, kb, 64 * h:64 * h + 64], A16[sa16],
                                           start=j["first"], stop=j["last"]),
                  reads=[K("A", sa16), K("Vs", kb)], writes=[("ps", 4)])
            if j["last"] and h == 1:
                finalize_sb(qt)

        def finalize_sb(qt):
            s.add("dve", lambda e: e.tensor_copy(out=o32, in_=ps[4][:, :]), reads=[("ps", 4)], writes=[K("o32")])
            s.add("act", lambda e: e.activation(out=sq32, in_=o32, func=AF.Square), reads=[K("o32")], writes=[K("sq32")])
            xi = nextX()
            s.add("pe", lambda e: e.matmul(psX[xi][:, :], onesblk, sq32, start=True, stop=True), reads=[K("sq32"), K("c")], writes=[("ps", 6 + xi)])
            s.add("act", lambda e: e.activation(out=r32, in_=psX[xi][:, :], func=AF.Sqrt, scale=1.0 / 64, bias=1e-6), reads=[("ps", 6 + xi)], writes=[K("r32")])
            s.add("dve", lambda e: e.reciprocal(out=sq32, in_=r32), reads=[K("r32")], writes=[K("sq32")])
            yi = ycnt[0] % 2
            ycnt[0] += 1
            s.add("dve", lambda e: e.scalar_tensor_tensor(out=Yb[yi], in0=o32, scalar=gvec[:, 0:1], in1=sq32, op0=ALU.mult, op1=ALU.mult),
                  reads=[K("o32"), K("sq32"), K("c")], writes=[K("Yb", yi)])
            od = dma("sp", D["yT"][0:128, qt * 512:qt * 512 + 512], Yb[yi], [K("Yb", yi)], [], K("yout"))
            out_dmas.append(od)

        n = len(jobs)
        for step in range(n + 2):
            if step < n:
                stage1(jobs[step])
            if 0 <= step - 1 < n:
                stage2(jobs[step - 1])
            if 0 <= step - 2 < n:
                stage3(jobs[step - 2])
    return out_dmas


import numpy as np
import ml_dtypes
import concourse.bass as bass
import concourse.mybir as mybir

ALPHA = (2.0 * 2) ** 0.25
LN_EPS = 1e-5
AX = mybir.AxisListType


def emit_B(nc, s, ar, ps, D, T, lname, NE=16):
    L = lname
    K = lambda *a: (L,) + a
    NT = T // 128
    TT = min(512, T)
    NT4 = T // TT
    NSUB = TT // 128

    yacc = ar.take(NT * 1024, F32).rearrange("p (t d) -> p t d", t=NT)
    X1T = ar.take(8 * T).rearrange("p (c t) -> p c t", c=8)
    wslot = [ar.take(3 * 4096) for _ in range(2)]
    Wg = [w[:, 0:4096].rearrange("p (c f) -> p c f", c=8) for w in wslot]
    Wu = [w[:, 4096:8192].rearrange("p (c f) -> p c f", c=8) for w in wslot]
    Wd = [w[:, 8192:12288].rearrange("p (c f) -> p c f", c=4) for w in wslot]
    Wo = wslot[1][:, 0:8192].rearrange("p (c f) -> p c f", c=8)
    YT = [ar.take(8 * 128).rearrange("p (c t) -> p c t", c=8) for _ in range(2)]
    xt = [ar.take(1024, F32) for _ in range(2)]
    v32 = ar.take(1024, F32)
    lnp = [ar.take(1024, F32) for _ in range(4)]
    X1T32 = ar.take(8 * 128, F32).rearrange("p (c t) -> p c t", c=8)
    hT = [ar.take(4 * TT).rearrange("p (c t) -> p c t", c=4) for _ in range(2)]
    sg = [ar.take(TT, F32) for _ in range(2)]
    gates = ar.take(NT * 16, F32).rearrange("p (t e) -> p t e", t=NT)
    Wr = ar.take(8 * 16, F32).rearrange("p (c e) -> p c e", c=8)
    br = ar.take(16, F32)
    ident = ar.take(128, F32)
    stats = ar.take(16, F32)
    mv = ar.take(4, F32)
    rt = [ar.take(16, F32) for _ in range(6)]
    rs = [ar.take(4, F32) for _ in range(6)]

    def dma(eng, out, in_, reads, writes, grp):
        return s.add(eng, lambda e: e.dma_start(out=out, in_=in_), reads=reads, writes=writes, grp=grp)

    for i, nm in enumerate(("ln1g", "ln1b", "ln2g", "ln2b")):
        dma("sp", lnp[i], D[nm], [], [K("c")], K("c"))
    for dc in range(8):
        dma("sp", Wr[:, dc, :], D["wr"][dc * 128:(dc + 1) * 128, :], [], [K("c")], K("c"))
    dma("sp", br, D["br"], [], [K("c")], K("c"))
    dma("sp", ident, D["ident32"], [], [K("c")], K("c"))
    for ec in range(8):
        dma("pool", Wo[:, ec, :], D["wo"][ec * 128:(ec + 1) * 128, :], [], [K("wslot", 1)], K("wslot", 1))

    def load_unit(u):
        e, fh = u // 2, u % 2
        sl = u % 2
        key = K("wslot", sl)
        for dc in range(8):
            dma("pool", Wg[sl][:, dc, :], D["wg"][e, dc * 128:(dc + 1) * 128, fh * 512:(fh + 1) * 512], [], [key], key)
        for dc in range(8):
            dma("pool", Wu[sl][:, dc, :], D["wu"][e, dc * 128:(dc + 1) * 128, fh * 512:(fh + 1) * 512], [], [key], key)
        for fc in range(4):
            dma("pool", Wd[sl][:, fc, :], D["wd"][e, fh * 512 + fc * 128:fh * 512 + (fc + 1) * 128, :], [], [key], key)

    load_unit(0)

    def layer_norm(src, dst, gi, bi, dkey):
        for hh in range(2):
            s.add("dve", (lambda hh: lambda e: e.bn_stats(out=stats[:, hh * 6:(hh + 1) * 6], in_=src[:, hh * 512:(hh + 1) * 512]))(hh),
                  reads=[K("lnsrc")], writes=[K("stats")])
        s.add("dve", lambda e: e.bn_aggr(out=mv[:, 0:2], in_=stats[:, 0:12]), reads=[K("stats")], writes=[K("mv")])
        s.add("act", lambda e: e.activation(out=mv[:, 2:3], in_=mv[:, 1:2], func=AF.Sqrt, bias=LN_EPS), reads=[K("mv")], writes=[K("mv2")])
        s.add("dve", lambda e: e.reciprocal(out=mv[:, 3:4], in_=mv[:, 2:3]), reads=[K("mv2")], writes=[K("mv3")])
        s.add("dve", lambda e: e.tensor_scalar(out=src, in0=src, scalar1=mv[:, 0:1], scalar2=mv[:, 3:4], op0=ALU.subtract, op1=ALU.mult),
              reads=[K("mv"), K("mv3"), K("lnsrc")], writes=[K("lnsrc")])
        s.add("dve", lambda e: e.tensor_tensor(out=src, in0=src, in1=lnp[gi], op=ALU.mult), reads=[K("lnsrc"), K("c")], writes=[K("lnsrc")])
        return s.add("dve", lambda e: e.tensor_tensor(out=dst, in0=src, in1=lnp[bi], op=ALU.add), reads=[K("lnsrc"), K("c")], writes=[dkey])

    for ti in range(NT):
        sl = ti % 2
        for ec in range(8):
            dma("sp", YT[sl][:, ec, :], D["yT"][ec * 128:(ec + 1) * 128, ti * 128:(ti + 1) * 128], [], [K("YT", sl)], K("YT", sl))
        dma("sp", xt[sl], D["x"][ti * 128:(ti + 1) * 128, :], [], [K("xt", sl)], K("xt", sl))
        for hh in range(2):
            for ec in range(8):
                s.add("pe", (lambda sl, hh, ec: lambda e: e.matmul(ps[hh][:, :], YT[sl][:, ec, :], Wo[:, ec, hh * 512:(hh + 1) * 512],
                                                                   start=(ec == 0), stop=(ec == 7)))(sl, hh, ec),
                      reads=[K("YT", sl), K("wslot", 1)], writes=[("ps", hh)])
            s.add("dve", (lambda sl, hh: lambda e: e.scalar_tensor_tensor(out=v32[:, hh * 512:(hh + 1) * 512], in0=xt[sl][:, hh * 512:(hh + 1) * 512],
                                                                        scalar=float(ALPHA), in1=ps[hh][:, :], op0=ALU.mult, op1=ALU.add))(sl, hh),
                  reads=[K("xt", sl), ("ps", hh)], writes=[K("lnsrc")])
        layer_norm(v32, xt[sl], 0, 1, K("xt", sl))
        s.add("pool", (lambda sl, ti: lambda e: e.tensor_scalar(out=yacc[:, ti, :], in0=xt[sl], scalar1=float(ALPHA), scalar2=None, op0=ALU.mult))(sl, ti),
              reads=[K("xt", sl)], writes=[K("yacc", ti)])
        for q in range(2):
            for k in range(4):
                dc = q * 4 + k
                s.add("pe", (lambda sl, q, k, dc: lambda e: e.transpose(out=ps[2 + q][:, k * 128:(k + 1) * 128], in_=xt[sl][:, dc * 128:(dc + 1) * 128], identity=ident))(sl, q, k, dc),
                      reads=[K("xt", sl), K("c")], writes=[("ps", 2 + q)])
            s.add("dve", (lambda q: lambda e: e.tensor_copy(out=X1T32[:, q * 4:(q + 1) * 4, :], in_=ps[2 + q][:, :].rearrange("p (c t) -> p c t", c=4)))(q),
                  reads=[("ps", 2 + q)], writes=[K("X1T32", q)])
            s.add("pool", (lambda q, ti: lambda e: e.tensor_copy(out=X1T[:, q * 4:(q + 1) * 4, ti * 128:(ti + 1) * 128], in_=X1T32[:, q * 4:(q + 1) * 4, :]))(q, ti),
                  reads=[K("X1T32", q)], writes=[K("X1T", ti)])
        for dc in range(8):
            s.add("pe", (lambda dc: lambda e: e.matmul(ps[4][:, 0:16], X1T32[:, dc, :], Wr[:, dc, :], start=(dc == 0), stop=(dc == 7)))(dc),
                  reads=[K("X1T32", dc // 4), K("c")], writes=[("ps", 4)])
        lg, ex, eq, p2, selm, msk = rt
        v1, v2, gs, gsel, gmx, den = rs
        R = K("rt")
        s.add("dve", lambda e: e.tensor_tensor(out=lg, in0=ps[4][:, 0:16], in1=br, op=ALU.add), reads=[("ps", 4), K("c")], writes=[R])
        s.add("dve", lambda e: e.tensor_reduce(out=gmx[:, 0:1], in_=lg, axis=AX.X, op=ALU.max), reads=[R], writes=[R])
        s.add("dve", lambda e: e.tensor_scalar(out=lg, in0=lg, scalar1=gmx[:, 0:1], scalar2=None, op0=ALU.subtract), reads=[R], writes=[R])
        s.add("act", lambda e: e.activation(out=ex, in_=lg, func=AF.Exp), reads=[R], writes=[K("rt2")])
        R2 = K("rt2")
        ex3 = ex.rearrange("p (g k) -> p g k", g=4)
        eq3 = eq.rearrange("p (g k) -> p g k", g=4)
        p23 = p2.rearrange("p (g k) -> p g k", g=4)
        sel3 = selm.rearrange("p (g k) -> p g k", g=4)
        msk3 = msk.rearrange("p (g k) -> p g k", g=4)
        s.add("dve", lambda e: e.tensor_reduce(out=v1, in_=ex3, axis=AX.X, op=ALU.max), reads=[R2], writes=[R2])
        s.add("dve", lambda e: e.tensor_tensor(out=eq3, in0=ex3, in1=v1.unsqueeze(2).to_broadcast([128, 4, 4]), op=ALU.is_equal), reads=[R2], writes=[R2])
        s.add("dve", lambda e: e.scalar_tensor_tensor(out=p2, in0=eq, scalar=-2.0, in1=ex, op0=ALU.mult, op1=ALU.add), reads=[R2], writes=[R2])
        s.add("dve", lambda e: e.tensor_reduce(out=v2, in_=p23, axis=AX.X, op=ALU.max), reads=[R2], writes=[R2])
        s.add("dve", lambda e: e.tensor_tensor(out=gs, in0=v1, in1=v2, op=ALU.add), reads=[R2], writes=[R2])
        s.add("dve", lambda e: e.tensor_reduce(out=gmx[:, 1:2], in_=gs, axis=AX.X, op=ALU.max), reads=[R2], writes=[R2])
        s.add("dve", lambda e: e.tensor_scalar(out=gsel, in0=gs, scalar1=gmx[:, 1:2], scalar2=None, op0=ALU.is_equal), reads=[R2], writes=[R2])
        s.add("dve", lambda e: e.tensor_tensor(out=sel3, in0=ex3, in1=v2.unsqueeze(2).to_broadcast([128, 4, 4]), op=ALU.is_ge), reads=[R2], writes=[R2])
        s.add("dve", lambda e: e.tensor_tensor(out=msk3, in0=sel3, in1=gsel.unsqueeze(2).to_broadcast([128, 4, 4]), op=ALU.mult), reads=[R2], writes=[R2])
        s.add("dve", lambda e: e.tensor_tensor(out=v1, in0=gs, in1=gsel, op=ALU.mult), reads=[R2], writes=[R2])
        s.add("dve", lambda e: e.tensor_reduce(out=den[:, 0:1], in_=v1, axis=AX.X, op=ALU.add), reads=[R2], writes=[R2])
        s.add("dve", lambda e: e.reciprocal(out=den[:, 1:2], in_=den[:, 0:1]), reads=[R2], writes=[R2])
        s.add("dve", lambda e: e.tensor_tensor(out=msk, in0=msk, in1=ex, op=ALU.mult), reads=[R2], writes=[R2])
        s.add("dve", (lambda ti: lambda e: e.tensor_scalar(out=gates[:, ti, :], in0=msk, scalar1=den[:, 1:2], scalar2=None, op0=ALU.mult))(ti),
              reads=[R2], writes=[K("gates", ti)])

    NU = NE * 2
    hcnt = [0]
    for u in range(NU):
        e_, fh = u // 2, u % 2
        sl = u % 2
        if u + 1 < NU:
            load_unit(u + 1)
        wkey = K("wslot", sl)
        for t4 in range(NT4):
            hs = hcnt[0] % 2
            hcnt[0] += 1
            tok = slice(t4 * TT, (t4 + 1) * TT)
            for fc in range(4):
                gi = 0 + (fc % 2)
                ui = 2 + (fc % 2)
                for dc in range(8):
                    s.add("pe", (lambda sl, dc, fc, gi, tok: lambda e: e.matmul(ps[gi][:, 0:TT], Wg[sl][:, dc, fc * 128:(fc + 1) * 128], X1T[:, dc, tok],
                                                                               start=(dc == 0), stop=(dc == 7)))(sl, dc, fc, gi, tok),
                          reads=[wkey] + [K("X1T", t4 * NSUB + i) for i in range(NSUB)], writes=[("ps", gi)])
                for dc in range(8):
                    s.add("pe", (lambda sl, dc, fc, ui, tok: lambda e: e.matmul(ps[ui][:, 0:TT], Wu[sl][:, dc, fc * 128:(fc + 1) * 128], X1T[:, dc, tok],
                                                                               start=(dc == 0), stop=(dc == 7)))(sl, dc, fc, ui, tok),
                          reads=[wkey] + [K("X1T", t4 * NSUB + i) for i in range(NSUB)], writes=[("ps", ui)])
                si = fc % 2
                s.add("act", (lambda gi, si: lambda e: e.activation(out=sg[si][:, 0:TT], in_=ps[gi][:, 0:TT], func=AF.Silu))(gi, si),
                      reads=[("ps", gi)], writes=[K("sg", si)])
                s.add("dve", (lambda hs, fc, si, ui: lambda e: e.tensor_tensor(out=hT[hs][:, fc, :], in0=sg[si][:, 0:TT], in1=ps[ui][:, 0:TT], op=ALU.mult))(hs, fc, si, ui),
                      reads=[K("sg", si), ("ps", ui)], writes=[K("hT", hs, fc)])
            for sub in range(NSUB):
                ti = t4 * NSUB + sub
                for hh in range(2):
                    di = 4 + ((sub * 2 + hh) % 2)
                    for fc in range(4):
                        s.add("pe", (lambda sl, hs, fc, sub, hh, di: lambda e: e.matmul(ps[di][:, :], hT[hs][:, fc, sub * 128:(sub + 1) * 128], Wd[sl][:, fc, hh * 512:(hh + 1) * 512],
                                                                                         start=(fc == 0), stop=(fc == 3)))(sl, hs, fc, sub, hh, di),
                              reads=[wkey, K("hT", hs, fc)], writes=[("ps", di)])
                    s.add("dve", (lambda ti, hh, di, e_: lambda e: e.scalar_tensor_tensor(out=yacc[:, ti, hh * 512:(hh + 1) * 512], in0=ps[di][:, :], scalar=gates[:, ti, e_:e_ + 1],
                                                                                          in1=yacc[:, ti, hh * 512:(hh + 1) * 512], op0=ALU.mult, op1=ALU.add))(ti, hh, di, e_),
                          reads=[("ps", di), K("gates", ti), K("yacc", ti)], writes=[K("yacc", ti)])

    outs = []
    for ti in range(NT):
        sl = ti % 2
        s.add("dve", (lambda ti: lambda e: e.tensor_copy(out=v32, in_=yacc[:, ti, :]))(ti), reads=[K("yacc", ti)], writes=[K("lnsrc")])
        layer_norm(v32, xt[sl], 2, 3, K("xt", sl))
        outs.append(dma("sp", D["x2"][ti * 128:(ti + 1) * 128, :], xt[sl], [K("xt", sl)], [], K("x2out")))
    return outs


def _build_A(S):
    nc = bass.Bass("TRN2", target_bir_lowering=False)
    D = {}
    D["xT"] = nc.dram_tensor("xT", [1024, S], F32, kind="ExternalInput").ap()
    D["w"] = nc.dram_tensor("w", [1024, 768], F32, kind="ExternalInput").ap()
    D["g"] = nc.dram_tensor("g", [256, 1], F32, kind="ExternalInput").ap()
    D["biasm"] = nc.dram_tensor("biasm", [3, 2, 128, 256], F32, kind="ExternalInput").ap()
    C = consts_A()
    for k, v in C.items():
        D[k] = nc.dram_tensor(k, list(v.shape), BF16 if v.dtype == ml_dtypes.bfloat16 else F32, kind="ExternalInput").ap()
    D["yT"] = nc.dram_tensor("yT", [256, S], BF16, kind="ExternalOutput").ap()
    with ExitStack() as es:
        arena = es.enter_context(nc.sbuf_tensor("arena", [128, 103 * 1024], BF16))
        ps = [es.enter_context(nc.psum_tensor("ps%d" % i, [128, 512], F32)) for i in range(8)]
        ar = Arena(arena)
        s = Sched(nc)
        outs = emit_A(nc, s, ar, ps, D, S, True, "A")
        keys = s.emit(final_wait_ops=outs)
        sems = {k: es.enter_context(nc.semaphore("s%d" % i)) for i, k in enumerate(keys)}
        s.run(sems, final_wait_ops=outs)
    return nc, C


def _build_B(T, NE=16):
    nc = bass.Bass("TRN2", target_bir_lowering=False)
    D = {}

    def inp(name, shape, dt=F32):
        D[name] = nc.dram_tensor(name, shape, dt, kind="ExternalInput").ap()
    inp("yT", [1024, T], BF16)
    inp("x", [T, 1024])
    inp("wo", [1024, 1024])
    for nm in ("ln1g", "ln1b", "ln2g", "ln2b"):
        inp(nm, [128, 1024])
    inp("wr", [1024, 16])
    inp("br", [128, 16])
    inp("ident32", [128, 128])
    for nm in ("wg", "wu", "wd"):
        inp(nm, [NE, 1024, 1024])
    D["x2"] = nc.dram_tensor("x2", [T, 1024], F32, kind="ExternalOutput").ap()
    with ExitStack() as es:
        arena = es.enter_context(nc.sbuf_tensor("arena", [128, 103 * 1024], BF16))
        ps = [es.enter_context(nc.psum_tensor("ps%d" % i, [128, 512], F32)) for i in range(8)]
        ar = Arena(arena)
        s = Sched(nc)
        outs = emit_B(nc, s, ar, ps, D, T, "B", NE=NE)
        keys = s.emit(final_wait_ops=outs)
        sems = {k: es.enter_context(nc.semaphore("s%d" % i)) for i, k in enumerate(keys)}
        s.run(sems, final_wait_ops=outs)
    return nc


def kernel(x, w_in, g_sb, g_dil, w_out, ln1_g, ln1_b, ln2_g, ln2_b, rel_bias, w_router, b_router, w_gate, w_up, w_down):
    x = np.asarray(x, np.float32)
    B, S, Dm = x.shape
    depth = w_in.shape[0]
    T = 2048
    ncA, C = _build_A(S)
    ncB = _build_B(T)
    rep = lambda v: np.ascontiguousarray(np.broadcast_to(np.asarray(v, np.float32)[None, :], (128, v.shape[0])))
    ident32 = np.eye(128, dtype=np.float32)
    rel_bias = np.asarray(rel_bias, np.float32)
    xcur = x
    for l in range(depth):
        wl = np.asarray(w_in[l], np.float32)
        in_maps = []
        for c in range(8):
            b, j = c // 4, c % 4
            cols = [wl[:, o + 128 * j:o + 128 * j + 128] for o in (0, 512, 1536, 2048, 1024, 2560)]
            m = {"xT": np.ascontiguousarray(xcur[b].T), "w": np.ascontiguousarray(np.concatenate(cols, axis=1)),
                 "g": np.ascontiguousarray(np.concatenate([np.asarray(g_sb[l], np.float32)[128 * j:128 * j + 128],
                                                            np.asarray(g_dil[l], np.float32)[128 * j:128 * j + 128]])[:, None]),
                 "biasm": dil_bias_tables(rel_bias[:, 2 * j:2 * j + 2])}
            m.update(C)
            in_maps.append(m)
        resA = run_bass_kernel_spmd(ncA, in_maps, core_ids=list(range(8)))
        yT = np.zeros((B, 1024, S), dtype=ml_dtypes.bfloat16)
        for c in range(8):
            b, j = c // 4, c % 4
            r = np.asarray(resA.results[c]["yT"])
            yT[b, 128 * j:128 * j + 128] = r[0:128]
            yT[b, 512 + 128 * j:512 + 128 * j + 128] = r[128:256]
        in_maps = []
        shared = {"wo": np.ascontiguousarray(np.asarray(w_out[l], np.float32)), "ln1g": rep(ln1_g[l]), "ln1b": rep(ln1_b[l]),
                  "ln2g": rep(ln2_g[l]), "ln2b": rep(ln2_b[l]), "wr": np.ascontiguousarray(np.asarray(w_router, np.float32)),
                  "br": rep(b_router), "ident32": ident32, "wg": np.ascontiguousarray(np.asarray(w_gate[l], np.float32)),
                  "wu": np.ascontiguousarray(np.asarray(w_up[l], np.float32)), "wd": np.ascontiguousarray(np.asarray(w_down[l], np.float32))}
        for c in range(8):
            b, q = c // 4, c % 4
            m = {"yT": np.ascontiguousarray(yT[b][:, q * T:(q + 1) * T]), "x": np.ascontiguousarray(xcur[b, q * T:(q + 1) * T, :])}
            m.update(shared)
            in_maps.append(m)
        resB = run_bass_kernel_spmd(ncB, in_maps, core_ids=list(range(8)))
        xn = np.zeros_like(xcur)
        for c in range(8):
            b, q = c // 4, c % 4
            xn[b, q * T:(q + 1) * T, :] = np.asarray(resB.results[c]["x2"], np.float32)
        xcur = xn
    return xcur
```

```python
from contextlib import ExitStack
from concourse.bass_utils import run_bass_kernel_spmd
import numpy as np
import concourse.bass as bass
import concourse.mybir as mybir

F32 = mybir.dt.float32
BF16 = mybir.dt.bfloat16
AF = mybir.ActivationFunctionType
ALU = mybir.AluOpType

COMPUTE = ("pe", "act", "dve", "pool")


class Sched:
    def __init__(self, nc):
        self.nc = nc
        self.ops = []
        self.last_writer = {}
        self.readers = {}
        self.dma_groups = {}
        self.fence_deps = set()
        self.fence_pending = set()

    def fence(self):
        last = {}
        for i, op in enumerate(self.ops):
            k = ("dma", op["grp"]) if op["dma"] else ("eng", op["eng"])
            last[k] = i
        self.fence_deps = set(last.values())
        self.fence_pending = {"pe", "act", "dve", "pool", "sp"}

    def add(self, eng, fn, reads=(), writes=(), grp=None, inc=16):
        idx = len(self.ops)
        deps = set()
        if eng in self.fence_pending:
            deps |= self.fence_deps
            self.fence_pending.discard(eng)
        for b in reads:
            w = self.last_writer.get(b)
            if w is not None:
                deps.add(w)
        for b in writes:
            w = self.last_writer.get(b)
            if w is not None:
                deps.add(w)
            for r in self.readers.get(b, ()):
                deps.add(r)
        deps.discard(idx)
        is_dma = grp is not None
        if eng == "pe":
            deps = {d for d in deps if not (self.ops[d]["eng"] == "pe" and not self.ops[d]["dma"])}
        op = dict(eng=eng, fn=fn, deps=deps, dma=is_dma, grp=grp, needed=False, inc=(inc if is_dma else 1))
        self.ops.append(op)
        for b in reads:
            self.readers.setdefault(b, []).append(idx)
        for b in writes:
            self.last_writer[b] = idx
            self.readers[b] = []
        return idx

    def emit(self, final_wait_ops=()):
        nc = self.nc
        ops = self.ops
        for op in ops:
            for d in op["deps"]:
                ops[d]["needed"] = True
        for d in final_wait_ops:
            ops[d]["needed"] = True
        sem_names = []
        counters = {}
        for i, op in enumerate(ops):
            if not op["needed"]:
                continue
            key = ("dma", op["grp"]) if op["dma"] else ("eng", op["eng"])
            if key not in counters:
                counters[key] = 0
                sem_names.append(key)
            counters[key] += op["inc"]
            op["sem"] = key
            op["val"] = counters[key]
        self.sem_keys = sem_names
        return sem_names

    def run(self, sems, final_wait_ops=()):
        nc = self.nc
        ops = self.ops
        per_eng = {}
        for i, op in enumerate(ops):
            per_eng.setdefault(op["eng"], []).append(i)

        def body(engname, eng):
            known = {}
            for i in per_eng.get(engname, []):
                op = ops[i]
                need = {}
                for d in op["deps"]:
                    p = ops[d]
                    k = p["sem"]
                    need[k] = max(need.get(k, 0), p["val"])
                for k, v in need.items():
                    if known.get(k, 0) >= v:
                        continue
                    eng.wait_ge(sems[k], v)
                    known[k] = v
                ins = op["fn"](eng)
                if op["needed"]:
                    ins.then_inc(sems[op["sem"]], op["inc"])
            if engname == "sp":
                fin = {}
                for d in final_wait_ops:
                    p = ops[d]
                    fin[p["sem"]] = max(fin.get(p["sem"], 0), p["val"])
                for k, v in fin.items():
                    if known.get(k, 0) < v:
                        eng.wait_ge(sems[k], v)
                        known[k] = v

        with nc.Block() as block:
            @block.sync
            def _(e):
                body("sp", e)

            @block.tensor
            def _(e):
                body("pe", e)

            @block.scalar
            def _(e):
                body("act", e)

            @block.vector
            def _(e):
                body("dve", e)

            @block.gpsimd
            def _(e):
                body("pool", e)


import math
import numpy as np
import ml_dtypes
import concourse.bass as bass
import concourse.mybir as mybir

NEG = -30000.0
SKIP = ['dveonly']
SC = 2048


def t5_bucket_np(dist):
    max_exact = 16
    d = np.maximum(dist, 0)
    large = max_exact + (np.log(np.maximum(d, 1).astype(np.float32) / np.float32(max_exact))
                         / np.float32(math.log(2048 / max_exact)) * np.float32(32 - max_exact)).astype(np.int32)
    large = np.minimum(large, 31)
    return np.where(d < max_exact, d, large)


def consts_A():
    j = np.arange(128)[:, None]
    s = np.arange(128)[None, :]
    negtri = np.where(j >= s, -1.0, 0.0).astype(ml_dtypes.bfloat16)
    negones = np.full((128, 128), -1.0, dtype=ml_dtypes.bfloat16)
    ident = np.eye(128, dtype=ml_dtypes.bfloat16)
    t = np.arange(512)[None, :]
    sbmask = np.stack([np.where(128 * a + j >= t, NEG, 0.0) for a in range(4)], 0).astype(ml_dtypes.bfloat16)
    onesblk = np.zeros((128, 128), np.float32)
    onesblk[:64, :64] = 1.0
    onesblk[64:, 64:] = 1.0
    sel65 = np.zeros((65, 64), np.float32)
    sel65[64, :] = 1.0
    ones65 = np.zeros((65, 64), np.float32)
    ones65[:64, :] = 1.0
    return dict(negtri=negtri, negones=negones, ident=ident, sbmask=sbmask, onesblk=onesblk, sel65=sel65, ones65=ones65)


def dil_bias_tables(rel_bias_heads):
    kj = np.arange(128)[:, None]
    qi = np.arange(128)[None, :]
    out = np.zeros((3, 2, 128, 256), np.float32)
    for ri, r in enumerate((1, 4, 16)):
        steps_cur = qi - kj
        steps_prev = qi - kj + 128
        b_cur = t5_bucket_np(np.maximum(steps_cur, 0) * r)
        b_prev = t5_bucket_np(np.maximum(steps_prev, 0) * r)
        for h in range(2):
            cur = np.where(steps_cur >= 0, rel_bias_heads[b_cur, h], np.float32(NEG))
            prev = np.where(steps_prev <= 128, rel_bias_heads[b_prev, h], np.float32(NEG))
            out[ri, h, :, :128] = prev
            out[ri, h, :, 128:] = cur
    return out


class Arena:
    def __init__(self, ap_bf16):
        self.ap = ap_bf16
        self.off = 0
        self.total = ap_bf16.shape[1]

    def take(self, nelem, dtype=BF16):
        nb = nelem * (4 if dtype == F32 else 2)
        nb = (nb + 63) // 64 * 64
        n16 = nb // 2
        assert self.off + n16 <= self.total, ("arena overflow", self.off, n16, self.total)
        v = self.ap[:, self.off:self.off + n16]
        self.off += n16
        if dtype == F32:
            return v.bitcast(F32)[:, :nelem]
        return v[:, :nelem]


def emit_A(nc, s, ar, ps, D, S, src_is_f32, lname, stop=99, cc=None):
    NSC = S // SC
    NB = S // 128
    L = lname
    K = lambda *a: (L,) + a

    Wb = ar.take(8 * 768).rearrange("p (f c) -> p f c", f=8)
    XT = ar.take(8 * SC).rearrange("p (f t) -> p f t", f=8)
    QTs = ar.take(SC)
    QTsB = ar.take(SC)
    QTd = ar.take(SC)
    QTdB = ar.take(SC)
    KTs = ar.take(S)
    KTd = ar.take(S)
    Vs = ar.take(NB * 128).rearrange("p (b c) -> p b c", c=128)
    NW1 = NB
    Vd1 = ar.take(NW1 * 132).rearrange("p (b h c) -> p b h c", h=2, c=66)
    Vd4 = ar.take(32 * 132).rearrange("p (b h c) -> p b h c", h=2, c=66)
    Vd16 = ar.take(32 * 132).rearrange("p (b h c) -> p b h c", h=2, c=66)
    negtri = ar.take(128)
    negones = ar.take(128)
    ident = ar.take(128)
    sbmask = ar.take(4 * 512).rearrange("p (a t) -> p a t", a=4)
    onesblk = ar.take(128, F32)
    sel65 = ar.take(64, F32)
    ones65 = ar.take(64, F32)
    gvec = ar.take(4, F32)
    biasm16 = ar.take(6 * 256).rearrange("p (r h c) -> p r h c", r=3, h=2)
    E32 = [ar.take(512, F32) for _ in range(2)]
    Lp16 = [ar.take(512) for _ in range(4)]
    LA32 = [ar.take(512, F32) for _ in range(2)]
    A16 = [ar.take(512) for _ in range(4)]
    ncar = [ar.take(512, F32) for _ in range(2)]
    P16 = [ar.take(512) for _ in range(6)]
    acc = [ar.take(SC, F32) for _ in range(2)]
    o32 = ar.take(512, F32)
    sq32 = ar.take(512, F32)
    r32 = ar.take(512, F32)
    Yb = [ar.take(512) for _ in range(2)]

    def dma(eng, out, in_, reads, writes, grp):
        return s.add(eng, lambda e: e.dma_start(out=out, in_=in_), reads=reads, writes=writes, grp=grp)

    for fc in range(8):
        dma("pool", Wb[:, fc, :], D["w"][fc * 128:(fc + 1) * 128, :], [], [K("W")], K("W"))
    dma("sp", negtri, D["negtri"], [], [K("c")], K("c"))
    dma("sp", negones, D["negones"], [], [K("c")], K("c"))
    dma("sp", ident, D["ident"], [], [K("c")], K("c"))
    for a in range(4):
        dma("sp", sbmask[:, a, :], D["sbmask"][a], [], [K("c")], K("c"))
    dma("sp", onesblk, D["onesblk"], [], [K("c")], K("c"))
    dma("sp", sel65[0:65, :], D["sel65"], [], [K("c")], K("c"))
    dma("sp", ones65[0:65, :], D["ones65"], [], [K("c")], K("c"))
    dma("sp", gvec[:, 0:1], D["g"][0:128, :], [], [K("c")], K("c"))
    dma("sp", gvec[0:64, 1:2], D["g"][128:192, :], [], [K("c")], K("c"))
    dma("sp", gvec[0:64, 2:3], D["g"][192:256, :], [], [K("c")], K("c"))
    for ri in range(3):
        for h in range(2):
            dma("pool", biasm16[:, ri, h, :], D["biasm"][ri, h], [], [K("c")], K("c"))
    for vb in (Vd1, Vd4, Vd16):
        s.add("dve", (lambda vb: lambda e: e.memset(vb[:, :, :, 64:65], 1.0))(vb), writes=[K("vones")])

    psX = [ps[6], ps[7]]
    def dummy_out():
        return [dma("sp", D["yT"][0:128, 0:512], QTs[:, 0:512], [K("c"), K("W"), K("XT"), K("vones"), K("QTs"), K("QTd"), K("Vd4", 0), K("Vd16", 0), K("Vs", 0)], [], K("yout"))]
    if stop == 0:
        return dummy_out()
    xcnt = [0]

    def nextX():
        i = xcnt[0] % 2
        xcnt[0] += 1
        return i

    evac_cnt = [0]

    def evac(out, in_, reads, writes, scale=None):
        evac_cnt[0] += 1
        if evac_cnt[0] % 2 == 0 or 'dveonly' in SKIP:
            if scale is None:
                return s.add("dve", lambda e: e.tensor_copy(out=out, in_=in_), reads=reads, writes=writes)
            return s.add("dve", lambda e: e.tensor_scalar(out=out, in0=in_, scalar1=float(scale), scalar2=None, op0=ALU.mult),
                         reads=reads, writes=writes)
        sc = 1.0 if scale is None else float(scale)
        return s.add("act", lambda e: e.activation(out=out, in_=in_, func=AF.Copy, scale=sc), reads=reads, writes=writes)

    out_dmas = []
    ycnt = [0]

    def ss(start, n, step):
        return slice(start, start + (n - 1) * step + 1, step)

    QTs2 = [QTs, QTsB]
    QTd2 = [QTd, QTdB]

    def make(sc_i):
        t0 = sc_i * SC

        def load_xt(sci):
            for fc in range(8):
                if src_is_f32:
                    dma("pool", XT[:, fc, :], D["xT"][fc * 128:(fc + 1) * 128, sci * SC:(sci + 1) * SC], [], [K("XT")], K("XT"))
                else:
                    for p in range(4):
                        dma("sp", XT[:, fc, p * 512:(p + 1) * 512], D["GX"][p][sci * 1024 + fc * 128:sci * 1024 + (fc + 1) * 128, :], [("GX", p)], [K("XT")], K("XT"))

        def proj_gen():
            if sc_i == 0:
                load_xt(0)
            for c in range(4):
                tl = c * 512
                for kind, col0, dst, scale in (("qs", 0, QTs2[sc_i % 2][:, tl:tl + 512], 0.125), ("ks", 128, KTs[:, t0 + tl:t0 + tl + 512], None),
                                               ("qd", 256, QTd2[sc_i % 2][:, tl:tl + 512], 0.125), ("kd", 384, KTd[:, t0 + tl:t0 + tl + 512], None)):
                    yield
                    xi = nextX()
                    for fc in range(8):
                        s.add("pe", (lambda xi, fc, col0, tl: lambda e: e.matmul(psX[xi][:, :], Wb[:, fc, col0:col0 + 128], XT[:, fc, tl:tl + 512],
                                                                                 start=(fc == 0), stop=(fc == 7)))(xi, fc, col0, tl),
                              reads=[K("W"), K("XT")], writes=[("ps", 6 + xi)])
                    wkey = {"qs": K("QTs", sc_i % 2), "ks": K("KTs", sc_i), "qd": K("QTd", sc_i % 2), "kd": K("KTd", sc_i)}[kind]
                    evac(dst, psX[xi][:, :], [("ps", 6 + xi)], [wkey], scale)
                for sub in range(4 if 'vnat' not in SKIP else 0):
                    tt = tl + sub * 128
                    blk = (t0 + tt) // 128
                    yield
                    xi = nextX()
                    for fc in range(8):
                        s.add("pe", (lambda xi, fc, tt: lambda e: e.matmul(psX[xi][:, 0:256], XT[:, fc, tt:tt + 128], Wb[:, fc, 512:768],
                                                                           start=(fc == 0), stop=(fc == 7)))(xi, fc, tt),
                              reads=[K("W"), K("XT")], writes=[("ps", 6 + xi)])
                    if 'evs' not in SKIP: evac(Vs[:, blk, :], psX[xi][:, 0:128], [("ps", 6 + xi)], [K("Vs", blk)])
                    for hh in range(2 if 'evd1' not in SKIP else 0):
                        evac(Vd1[:, blk, hh, 0:64], psX[xi][:, 128 + 64 * hh:192 + 64 * hh], [("ps", 6 + xi)], [K("Vd1", blk)])
                n4 = 4 * sc_i + c
                yield
                xi = nextX()
                for cls in range(4 if 'v4' not in SKIP else 0):
                    for fc in range(8):
                        s.add("pe", (lambda xi, fc, cls, tl: lambda e: e.matmul(psX[xi][:, cls * 128:(cls + 1) * 128],
                                                                                XT[:, fc, ss(tl + cls, 128, 4)], Wb[:, fc, 640:768],
                                                                                start=(fc == 0), stop=(fc == 7)))(xi, fc, cls, tl),
                              reads=[K("W"), K("XT")], writes=[("ps", 6 + xi)])
                slot0 = (n4 % 8) * 4
                for kk_ in range(4 if 'v4' not in SKIP else 0):
                    for hh in range(2):
                        evac(Vd4[:, slot0 + kk_, hh, 0:64], psX[xi][:, kk_ * 128 + 64 * hh:kk_ * 128 + 64 * hh + 64], [("ps", 6 + xi)], [K("Vd4", n4 % 8)])
            for c4 in range(4 if 'v16' not in SKIP else 0):
                yield
                xi = nextX()
                for k in range(4):
                    cls = c4 * 4 + k
                    for fc in range(8):
                        s.add("pe", (lambda xi, fc, cls, k: lambda e: e.matmul(psX[xi][:, k * 128:(k + 1) * 128],
                                                                               XT[:, fc, ss(cls, 128, 16)], Wb[:, fc, 640:768],
                                                                               start=(fc == 0), stop=(fc == 7)))(xi, fc, cls, k),
                              reads=[K("W"), K("XT")], writes=[("ps", 6 + xi)])
                slot0 = (sc_i % 2) * 16 + c4 * 4
                for kk_ in range(4 if 'v16' not in SKIP else 0):
                    for hh in range(2):
                        evac(Vd16[:, slot0 + kk_, hh, 0:64], psX[xi][:, kk_ * 128 + 64 * hh:kk_ * 128 + 64 * hh + 64], [("ps", 6 + xi)], [K("Vd16", sc_i % 2)])

            yield
            if sc_i + 1 < NSC:
                load_xt(sc_i + 1)
            yield

        def dil_gen():
            groups = []
            for h in range(2):
                for ri, r in enumerate((1, 4, 16)):
                    nblk_sc = SC // (128 * r)
                    for c in range(r):
                        n0 = sc_i * nblk_sc
                        for g0 in range(n0, n0 + nblk_sc, 4):
                            blks = list(range(g0, min(g0 + 4, n0 + nblk_sc)))
                            groups.append(dict(h=h, ri=ri, r=r, c=c, g0=g0, blks=blks, gi=len(groups)))
            SBANKS = [(3, 6), (3, 6), (3, 6)]

            def d_s1(g):
                h, ri, r, c, blks, gi = g["h"], g["ri"], g["r"], g["c"], g["blks"], g["gi"]
                hp = slice(64 * h, 64 * h + 64)
                units = []
                for n in blks:
                    units += [(n - 1, n), (n, n)]
                g["uinfo"] = []
                g["tls"] = []
                for t_i in range(0, len(units), 4):
                    tu = units[t_i:t_i + 4]
                    bank = SBANKS[gi % 3][t_i // 4]
                    pslot = (gi % 3) * 2 + t_i // 4
                    lo = None
                    for u, (kb, n) in enumerate(tu):
                        co = u * 128
                        g["uinfo"].append((pslot, co, kb))
                        if kb < 0:
                            continue
                        if lo is None:
                            lo = co
                        qstart = c + r * 128 * n - t0
                        kstart = c + r * 128 * kb
                        role = u % 2
                        s.add("pe", (lambda bank, co, kstart, qstart, r, hp: lambda e: e.matmul(
                            ps[bank][:, co:co + 128], KTd[hp, ss(kstart, 128, r)], QTd2[sc_i % 2][hp, ss(qstart, 128, r)],
                            start=True, stop=False))(bank, co, kstart, qstart, r, hp),
                            reads=[K("KTd", kstart // SC), K("QTd", sc_i % 2)], writes=[("ps", bank)])
                        s.add("pe", (lambda bank, co, ri, h, role: lambda e: e.matmul(
                            ps[bank][:, co:co + 128], ident, biasm16[:, ri, h, role * 128:(role + 1) * 128],
                            start=False, stop=True))(bank, co, ri, h, role),
                            reads=[K("c")], writes=[("ps", bank)])
                    g["tls"].append((bank, pslot, lo, len(tu) * 128))

            def d_s2(g):
                for (bank, pslot, lo, hi) in g["tls"]:
                    s.add("act", (lambda bank, pslot, lo, hi: lambda e: e.activation(out=P16[pslot][:, lo:hi], in_=ps[bank][:, lo:hi], func=AF.Exp))(bank, pslot, lo, hi),
                          reads=[("ps", bank)], writes=[K("P", pslot)])

            def d_s3(g):
                h, r, c, blks, gi = g["h"], g["r"], g["c"], g["blks"], g["gi"]
                ob = 7
                vkey = {1: "Vd1", 4: "Vd4", 16: "Vd16"}[r]
                for bi, n in enumerate(blks):
                    first = True
                    for uu in (2 * bi, 2 * bi + 1):
                        pslot, co, kb = g["uinfo"][uu]
                        if kb < 0:
                            continue
                        if r == 1:
                            vap = Vd1[:, kb, h, 0:65]
                        elif r == 4:
                            vap = Vd4[:, (kb % 8) * 4 + c, h, 0:65]
                        else:
                            vap = Vd16[:, (kb % 2) * 16 + c, h, 0:65]
                        last = (uu == 2 * bi + 1)
                        s.add("pe", (lambda ob, bi, vap, pslot, co, first, last: lambda e: e.matmul(
                            ps[ob][0:65, bi * 128:(bi + 1) * 128], vap, P16[pslot][:, co:co + 128],
                            start=first, stop=last))(ob, bi, vap, pslot, co, first, last),
                            reads=[K("P", pslot), K(vkey, kb if r == 1 else (kb % 8 if r == 4 else kb % 2)), K("vones")], writes=[("ps", ob)])
                        first = False

            def d_s4(g):
                h, r, c, blks, gi, g0 = g["h"], g["r"], g["c"], g["blks"], g["gi"], g["g0"]
                ob = 7
                nb_ = len(blks)
                col = c + r * (128 * g0) - t0
                if r == 1:
                    s.add("dve", (lambda h, col, nb_, ob: lambda e: e.tensor_copy(out=acc[h][0:65, col:col + 128 * nb_], in_=ps[ob][0:65, 0:128 * nb_]))(h, col, nb_, ob),
                          reads=[("ps", ob)], writes=[K("acc", h)])
                else:
                    s.add("dve", (lambda h, col, nb_, r, ob: lambda e: e.tensor_tensor(
                        out=acc[h][0:65, ss(col, 128 * nb_, r)], in0=ps[ob][0:65, 0:128 * nb_], in1=acc[h][0:65, ss(col, 128 * nb_, r)], op=ALU.add))(h, col, nb_, r, ob),
                        reads=[("ps", ob), K("acc", h)], writes=[K("acc", h)])

            for g in groups:
                d_s1(g)
                yield
                d_s2(g)
                yield
                d_s3(g)
                yield
                d_s4(g)
                yield
            for h in range(2):
                for tq in range(4):
                    yield
                    cs = slice(tq * 512, tq * 512 + 512)
                    xi = nextX()
                    s.add("pe", (lambda xi, h, cs: lambda e: e.matmul(psX[xi][0:64, :], sel65[0:65, :], acc[h][0:65, cs], start=True, stop=True))(xi, h, cs),
                          reads=[K("acc", h), K("c")], writes=[("ps", 6 + xi)])
                    s.add("dve", (lambda xi: lambda e: e.reciprocal(out=r32[0:64, :], in_=psX[xi][0:64, :]))(xi), reads=[("ps", 6 + xi)], writes=[K("r32")])
                    s.add("dve", (lambda h, cs: lambda e: e.tensor_tensor(out=o32[0:64, :], in0=acc[h][0:64, cs], in1=r32[0:64, :], op=ALU.mult))(h, cs),
                          reads=[K("acc", h), K("r32")], writes=[K("o32")])
                    s.add("act", lambda e: e.activation(out=sq32[0:64, :], in_=o32[0:64, :], func=AF.Square), reads=[K("o32")], writes=[K("sq32")])
                    xi2 = nextX()
                    s.add("pe", (lambda xi2: lambda e: e.matmul(psX[xi2][0:64, :], ones65[0:64, :], sq32[0:64, :], start=True, stop=True))(xi2),
                          reads=[K("sq32"), K("c")], writes=[("ps", 6 + xi2)])
                    s.add("act", (lambda xi2: lambda e: e.activation(out=r32[0:64, :], in_=psX[xi2][0:64, :], func=AF.Sqrt, scale=1.0 / 64, bias=1e-6))(xi2),
                          reads=[("ps", 6 + xi2)], writes=[K("r32")])
                    s.add("dve", lambda e: e.reciprocal(out=sq32[0:64, :], in_=r32[0:64, :]), reads=[K("r32")], writes=[K("sq32")])
                    yi = ycnt[0] % 2
                    ycnt[0] += 1
                    s.add("dve", (lambda yi, h: lambda e: e.scalar_tensor_tensor(out=Yb[yi][0:64, :], in0=o32[0:64, :], scalar=gvec[0:64, 1 + h:2 + h],
                                                                                  in1=sq32[0:64, :], op0=ALU.mult, op1=ALU.mult))(yi, h),
                          reads=[K("o32"), K("sq32"), K("c")], writes=[K("Yb", yi)])
                    od = dma("sp", D["yA"][sc_i][128 + 64 * h:128 + 64 * h + 64, tq * 512:tq * 512 + 512], Yb[yi][0:64, :], [K("Yb", yi)], [K("yA", sc_i)], K("yout"))
                    out_dmas.append(od)


        def run_sb(drip):
            jobs = []
            for qt in range(4 * sc_i, 4 * sc_i + 4):
                kbl = list(range(4 * qt + 3, -1, -1))
                for ki, kb in enumerate(kbl):
                    for h in range(2):
                        jobs.append(dict(qt=qt, kb=kb, h=h, first=(ki == 0), last=(ki == len(kbl) - 1), idx=len(jobs)))

            def stage1(j):
                qt, kb, h, i = j["qt"], j["kb"], j["h"], j["idx"]
                hp = slice(64 * h, 64 * h + 64)
                a = kb - 4 * qt
                bb, se, sl = i % 3, i % 2, i % 4
                ql = (qt - 4 * sc_i) * 512
                diag = a >= 0
                kk = K("KTs", (kb * 128) // SC)
                s.add("pe", lambda e: e.matmul(ps[bb][:, :], KTs[hp, kb * 128:(kb + 1) * 128], QTs2[sc_i % 2][hp, ql:ql + 512], start=True, stop=False),
                      reads=[kk, K("QTs", sc_i % 2)], writes=[("ps", bb)])
                if diag:
                    s.add("pe", lambda e: e.matmul(ps[bb][:, :], ident, sbmask[:, a, :], start=False, stop=False), reads=[K("c")], writes=[("ps", bb)])
                s.add("act", lambda e: e.activation(out=E32[se], in_=ps[bb][:, :], func=AF.Exp), reads=[("ps", bb)], writes=[K("E", se)])
                s.add("act", lambda e: e.activation(out=Lp16[sl], in_=E32[se], func=AF.Ln, bias=1.0), reads=[K("E", se)], writes=[K("Lp", sl)])

            def stage2(j):
                qt, kb, h, i = j["qt"], j["kb"], j["h"], j["idx"]
                bb, sl, sla, sa16 = i % 3, i % 4, i % 2, i % 4
                cb = 5
                s.add("pe", lambda e: e.matmul(ps[bb][:, :], negtri, Lp16[sl], start=False, stop=True), reads=[K("Lp", sl), K("c")], writes=[("ps", bb)])
                if not j["last"]:
                    s.add("pe", lambda e: e.matmul(ps[cb][:, :], negones, Lp16[sl], start=True, stop=True), reads=[K("Lp", sl), K("c")], writes=[("ps", cb)])
                if j["first"]:
                    s.add("act", lambda e: e.activation(out=A16[sa16], in_=ps[bb][:, :], func=AF.Exp), reads=[("ps", bb)], writes=[K("A", sa16)])
                    if not j["last"]:
                        s.add("dve", lambda e: e.tensor_copy(out=ncar[h], in_=ps[cb][:, :]), reads=[("ps", cb)], writes=[K("ncar", h)])
                else:
                    s.add("dve", lambda e: e.tensor_tensor(out=LA32[sla], in0=ps[bb][:, :], in1=ncar[h], op=ALU.add),
                          reads=[("ps", bb), K("ncar", h)], writes=[K("LA", sla)])
                    if not j["last"]:
                        s.add("dve", lambda e: e.tensor_tensor(out=ncar[h], in0=ps[cb][:, :], in1=ncar[h], op=ALU.add),
                              reads=[("ps", cb), K("ncar", h)], writes=[K("ncar", h)])
                    s.add("act", lambda e: e.activation(out=A16[sa16], in_=LA32[sla], func=AF.Exp), reads=[K("LA", sla)], writes=[K("A", sa16)])

            def stage3(j):
                qt, kb, h, i = j["qt"], j["kb"], j["h"], j["idx"]
                sa16 = i % 4
                s.add("pe", lambda e: e.matmul(ps[4][64 * h:64 * h + 64, :], Vs[:, kb, 64 * h:64 * h + 64], A16[sa16],
                                               start=j["first"], stop=j["last"]),
                      reads=[K("A", sa16), K("Vs", kb)], writes=[("ps", 4)])
                if j["last"] and h == 1:
                    finalize_sb(qt)

            def finalize_sb(qt):
                s.add("dve", lambda e: e.tensor_copy(out=o32, in_=ps[4][:, :]), reads=[("ps", 4)], writes=[K("o32")])
                s.add("act", lambda e: e.activation(out=sq32, in_=o32, func=AF.Square), reads=[K("o32")], writes=[K("sq32")])
                s.add("pe", lambda e: e.matmul(ps[5][:, :], onesblk, sq32, start=True, stop=True), reads=[K("sq32"), K("c")], writes=[("ps", 5)])
                s.add("act", lambda e: e.activation(out=r32, in_=ps[5][:, :], func=AF.Sqrt, scale=1.0 / 64, bias=1e-6), reads=[("ps", 5)], writes=[K("r32")])
                s.add("dve", lambda e: e.reciprocal(out=sq32, in_=r32), reads=[K("r32")], writes=[K("sq32")])
                yi = ycnt[0] % 2
                ycnt[0] += 1
                s.add("dve", lambda e: e.scalar_tensor_tensor(out=Yb[yi], in0=o32, scalar=gvec[:, 0:1], in1=sq32, op0=ALU.mult, op1=ALU.mult),
                      reads=[K("o32"), K("sq32"), K("c")], writes=[K("Yb", yi)])
                od = dma("sp", D["yA"][sc_i][0:128, (qt % 4) * 512:(qt % 4) * 512 + 512], Yb[yi], [K("Yb", yi)], [K("yA", sc_i)], K("yout"))
                out_dmas.append(od)

            n = len(jobs)
            for step in range(n + 2):
                if step < n:
                    stage1(jobs[step])
                if 0 <= step - 1 < n:
                    stage2(jobs[step - 1])
                if 0 <= step - 2 < n:
                    stage3(jobs[step - 2])
                drip(n)
        return proj_gen, dil_gen, run_sb

    mk = [make(i) for i in range(NSC)]
    for _ in mk[0][0]():
        pass
    for sc_i in range(NSC):
        tasks = [mk[sc_i][1]()]
        nsteps = 205
        if sc_i + 1 < NSC:
            tasks.append(mk[sc_i + 1][0]())
            nsteps += 45
        state = [tasks, 0.0, nsteps]

        def drip(njobs, state=state):
            state[1] += state[2] / float(njobs)
            while state[1] >= 1.0 and state[0]:
                state[1] -= 1.0
                try:
                    next(state[0][0])
                except StopIteration:
                    state[0].pop(0)
        mk[sc_i][2](drip)
        for t in tasks:
            for _ in t:
                pass
        if cc is not None:
            cc(sc_i)
    return out_dmas


import numpy as np
import ml_dtypes
import concourse.bass as bass
import concourse.mybir as mybir

ALPHA = (2.0 * 2) ** 0.25
LN_EPS = 1e-5
AX = mybir.AxisListType


def emit_B(nc, s, ar, ps, D, T, lname, NE=16, st=None, last=True, cc2=None):
    L = lname
    K = lambda *a: (L,) + a
    NT = T // 128
    TT = min(512, T)
    NT4 = T // TT
    NSUB = TT // 128

    yacc = ar.take(NT * 1024, F32).rearrange("p (t d) -> p t d", t=NT)
    X1T = ar.take(8 * T).rearrange("p (c t) -> p c t", c=8)
    wslot = [ar.take(3 * 4096) for _ in range(2)]
    Wg = [w[:, 0:4096].rearrange("p (c f) -> p c f", c=8) for w in wslot]
    Wu = [w[:, 4096:8192].rearrange("p (c f) -> p c f", c=8) for w in wslot]
    Wd = [w[:, 8192:12288].rearrange("p (c f) -> p c f", c=4) for w in wslot]
    Wo = wslot[1][:, 0:8192].rearrange("p (c f) -> p c f", c=8)
    YT = [ar.take(8 * 128).rearrange("p (c t) -> p c t", c=8) for _ in range(2)]
    xt = [ar.take(1024, F32) for _ in range(2)]
    v32s = [ar.take(1024, F32) for _ in range(2)]
    lnp = [ar.take(1024, F32) for _ in range(2)]
    X1T32s = [ar.take(8 * 128, F32).rearrange("p (c t) -> p c t", c=8) for _ in range(2)]
    hT = [ar.take(4 * TT).rearrange("p (c t) -> p c t", c=4) for _ in range(2)]
    sg = [ar.take(TT, F32) for _ in range(2)]
    gates = ar.take(NT * 16, F32).rearrange("p (t e) -> p t e", t=NT)
    Wr = ar.take(8 * 16, F32).rearrange("p (c e) -> p c e", c=8)
    br = ar.take(16, F32)
    ident = ar.take(128, F32)
    stats2 = [ar.take(16, F32) for _ in range(2)]
    mv2 = [ar.take(4, F32) for _ in range(2)]
    rt2 = [[ar.take(16, F32) for _ in range(6)] for _ in range(2)]
    rs2 = [[ar.take(4, F32) for _ in range(6)] for _ in range(2)]
    xstp = ar.take(8 * 512).rearrange("p (c t) -> p c t", c=8)

    def dma(eng, out, in_, reads, writes, grp):
        return s.add(eng, lambda e: e.dma_start(out=out, in_=in_), reads=reads, writes=writes, grp=grp)

    for i, nm in enumerate(("ln1g", "ln1b")):
        dma("sp", lnp[i], D[nm], [], [K("lnp")], K("lnp"))
    for dc in range(8):
        dma("sp", Wr[:, dc, :], D["wr"][dc * 128:(dc + 1) * 128, :], [], [K("c")], K("c"))
    dma("sp", br, D["br"], [], [K("c")], K("c"))
    dma("sp", ident, D["ident32"], [], [K("c")], K("c"))
    for ec in range(8):
        dma("pool", Wo[:, ec, :], D["wo"][ec * 128:(ec + 1) * 128, :], [], [K("wslot", 1)], K("wslot", 1))

    def load_unit(u):
        e, fh = u // 2, u % 2
        sl = u % 2
        key = K("wslot", sl)
        for dc in range(8):
            dma("pool", Wg[sl][:, dc, :], D["wg"][e, dc * 128:(dc + 1) * 128, fh * 512:(fh + 1) * 512], [], [key], key)
        for dc in range(8):
            dma("pool", Wu[sl][:, dc, :], D["wu"][e, dc * 128:(dc + 1) * 128, fh * 512:(fh + 1) * 512], [], [key], key)
        for fc in range(4):
            dma("pool", Wd[sl][:, fc, :], D["wd"][e, fh * 512 + fc * 128:fh * 512 + (fc + 1) * 128, :], [], [key], key)

    load_unit(0)

    if D.get("Gall") is not None:
        s.add("sp", lambda e: e.dma_start(out=D["Gloc"], in_=D["Gall"][bass.ds(e.snap(st["reg"]), 1024), :]),
              reads=[K("G", q_) for q_ in range(4)], writes=[K("Gloc")], grp=K("Gloc"))
    def ln_stats(src, sl):
        for hh in range(2):
            s.add("dve", (lambda hh: lambda e: e.bn_stats(out=stats2[sl][:, hh * 6:(hh + 1) * 6], in_=src[:, hh * 512:(hh + 1) * 512]))(hh),
                  reads=[K("lnsrc", sl)], writes=[K("stats", sl)])
        s.add("dve", lambda e: e.bn_aggr(out=mv2[sl][:, 0:2], in_=stats2[sl][:, 0:12]), reads=[K("stats", sl)], writes=[K("mv", sl)])
        s.add("act", lambda e: e.activation(out=mv2[sl][:, 2:3], in_=mv2[sl][:, 1:2], func=AF.Sqrt, bias=LN_EPS), reads=[K("mv", sl)], writes=[K("mvb", sl)])

    def ln_apply(src, dst, gi, bi, dkey, sl):
        s.add("dve", lambda e: e.reciprocal(out=mv2[sl][:, 3:4], in_=mv2[sl][:, 2:3]), reads=[K("mvb", sl)], writes=[K("mvc", sl)])
        s.add("dve", lambda e: e.tensor_scalar(out=src, in0=src, scalar1=mv2[sl][:, 0:1], scalar2=mv2[sl][:, 3:4], op0=ALU.subtract, op1=ALU.mult),
              reads=[K("mv", sl), K("mvc", sl), K("lnsrc", sl)], writes=[K("lnsrc", sl)])
        s.add("dve", lambda e: e.tensor_tensor(out=src, in0=src, in1=lnp[gi], op=ALU.mult), reads=[K("lnsrc", sl), K("lnp")], writes=[K("lnsrc", sl)])
        s.add("dve", lambda e: e.tensor_tensor(out=dst, in0=src, in1=lnp[bi], op=ALU.add), reads=[K("lnsrc", sl), K("lnp")], writes=[dkey])

    def loads(ti):
        sl = ti % 2
        for ec in range(8):
            dma("sp", YT[sl][:, ec, :], D["Gloc"][ec * 128:(ec + 1) * 128, ti * 128:(ti + 1) * 128], [K("Gloc")], [K("YT", sl)], K("YT", sl))
        dma("sp", xt[sl], D["x"][ti * 128:(ti + 1) * 128, :], [("x2s",)], [K("xt", sl)], K("xt", sl))

    def phase1_gen(ti):
        sl = ti % 2
        pb = 4 * sl
        v32 = v32s[sl]
        X1T32 = X1T32s[sl]
        loads(ti)
        yield
        for hh in range(2):
            for ec in range(8):
                s.add("pe", (lambda hh, ec: lambda e: e.matmul(ps[pb + hh][:, :], YT[sl][:, ec, :], Wo[:, ec, hh * 512:(hh + 1) * 512],
                                                               start=(ec == 0), stop=(ec == 7)))(hh, ec),
                      reads=[K("YT", sl), K("wslot", 1)], writes=[("ps", pb + hh)])
            s.add("dve", (lambda hh: lambda e: e.scalar_tensor_tensor(out=v32[:, hh * 512:(hh + 1) * 512], in0=xt[sl][:, hh * 512:(hh + 1) * 512],
                                                                      scalar=float(ALPHA), in1=ps[pb + hh][:, :], op0=ALU.mult, op1=ALU.add))(hh),
                  reads=[K("xt", sl), ("ps", pb + hh)], writes=[K("lnsrc", sl)])
        yield
        ln_stats(v32, sl)
        yield
        ln_apply(v32, xt[sl], 0, 1, K("xt", sl), sl)
        s.add("act", lambda e: e.activation(out=yacc[:, ti, :], in_=xt[sl], func=AF.Copy, scale=float(ALPHA)),
              reads=[K("xt", sl)], writes=[K("yacc", ti)])
        yield
        for q in range(2):
            for kq in range(4):
                dc = q * 4 + kq
                s.add("pe", (lambda q, kq, dc: lambda e: e.transpose(out=ps[pb + 2 + q][:, kq * 128:(kq + 1) * 128], in_=xt[sl][:, dc * 128:(dc + 1) * 128], identity=ident))(q, kq, dc),
                      reads=[K("xt", sl), K("c")], writes=[("ps", pb + 2 + q)])
            yield
            s.add("dve", (lambda q: lambda e: e.tensor_copy(out=X1T32[:, q * 4:(q + 1) * 4, :], in_=ps[pb + 2 + q][:, :].rearrange("p (c t) -> p c t", c=4)))(q),
                  reads=[("ps", pb + 2 + q)], writes=[K("X1T32", sl, q)])
            s.add("act", (lambda q: lambda e: e.activation(out=X1T[:, q * 4:(q + 1) * 4, ti * 128:(ti + 1) * 128], in_=X1T32[:, q * 4:(q + 1) * 4, :], func=AF.Copy))(q),
                  reads=[K("X1T32", sl, q)], writes=[K("X1T", ti)])
        yield
        for dc in range(8):
            s.add("pe", (lambda dc: lambda e: e.matmul(ps[pb][:, 0:16], X1T32[:, dc, :], Wr[:, dc, :], start=(dc == 0), stop=(dc == 7)))(dc),
                  reads=[K("X1T32", sl, dc // 4), K("c")], writes=[("ps", pb)])
        yield
        lg, ex, eq, p2, selm, msk = rt2[sl]
        v1, v2, gs, gsel, gmx, den = rs2[sl]
        R = K("rt", sl)
        s.add("dve", lambda e: e.tensor_tensor(out=lg, in0=ps[pb][:, 0:16], in1=br, op=ALU.add), reads=[("ps", pb), K("c")], writes=[R])
        s.add("dve", lambda e: e.tensor_reduce(out=gmx[:, 0:1], in_=lg, axis=AX.X, op=ALU.max), reads=[R], writes=[R])
        s.add("dve", lambda e: e.tensor_scalar(out=lg, in0=lg, scalar1=gmx[:, 0:1], scalar2=None, op0=ALU.subtract), reads=[R], writes=[R])
        s.add("act", lambda e: e.activation(out=ex, in_=lg, func=AF.Exp), reads=[R], writes=[K("rtb", sl)])
        yield
        R2 = K("rtb", sl)
        ex3 = ex.rearrange("p (g k) -> p g k", g=4)
        eq3 = eq.rearrange("p (g k) -> p g k", g=4)
        p23 = p2.rearrange("p (g k) -> p g k", g=4)
        sel3 = selm.rearrange("p (g k) -> p g k", g=4)
        msk3 = msk.rearrange("p (g k) -> p g k", g=4)
        s.add("dve", lambda e: e.tensor_reduce(out=v1, in_=ex3, axis=AX.X, op=ALU.max), reads=[R2], writes=[R2])
        s.add("dve", lambda e: e.tensor_tensor(out=eq3, in0=ex3, in1=v1.unsqueeze(2).to_broadcast([128, 4, 4]), op=ALU.is_equal), reads=[R2], writes=[R2])
        s.add("dve", lambda e: e.scalar_tensor_tensor(out=p2, in0=eq, scalar=-2.0, in1=ex, op0=ALU.mult, op1=ALU.add), reads=[R2], writes=[R2])
        s.add("dve", lambda e: e.tensor_reduce(out=v2, in_=p23, axis=AX.X, op=ALU.max), reads=[R2], writes=[R2])
        s.add("dve", lambda e: e.tensor_tensor(out=gs, in0=v1, in1=v2, op=ALU.add), reads=[R2], writes=[R2])
        s.add("dve", lambda e: e.tensor_reduce(out=gmx[:, 1:2], in_=gs, axis=AX.X, op=ALU.max), reads=[R2], writes=[R2])
        yield
        s.add("dve", lambda e: e.tensor_scalar(out=gsel, in0=gs, scalar1=gmx[:, 1:2], scalar2=None, op0=ALU.is_equal), reads=[R2], writes=[R2])
        s.add("dve", lambda e: e.tensor_tensor(out=sel3, in0=ex3, in1=v2.unsqueeze(2).to_broadcast([128, 4, 4]), op=ALU.is_ge), reads=[R2], writes=[R2])
        s.add("dve", lambda e: e.tensor_tensor(out=msk3, in0=sel3, in1=gsel.unsqueeze(2).to_broadcast([128, 4, 4]), op=ALU.mult), reads=[R2], writes=[R2])
        s.add("dve", lambda e: e.tensor_tensor(out=v1, in0=gs, in1=gsel, op=ALU.mult), reads=[R2], writes=[R2])
        s.add("dve", lambda e: e.tensor_reduce(out=den[:, 0:1], in_=v1, axis=AX.X, op=ALU.add), reads=[R2], writes=[R2])
        s.add("dve", lambda e: e.reciprocal(out=den[:, 1:2], in_=den[:, 0:1]), reads=[R2], writes=[R2])
        s.add("dve", lambda e: e.tensor_tensor(out=msk, in0=msk, in1=ex, op=ALU.mult), reads=[R2], writes=[R2])
        s.add("dve", lambda e: e.tensor_scalar(out=gates[:, ti, :], in0=msk, scalar1=den[:, 1:2], scalar2=None, op0=ALU.mult),
              reads=[R2], writes=[K("gates", ti)])

    def run_two(gens):
        gens = list(gens)
        active = [None, None]
        nxt = [0]

        def refill(slot):
            for j in range(nxt[0], len(gens)):
                if gens[j] is not None and gens[j][0] % 2 == slot:
                    g = gens[j][1]
                    gens[j] = None
                    return g
            return None
        active[0] = refill(0)
        active[1] = refill(1)
        for _ in range(3):
            if active[0] is not None:
                try:
                    next(active[0])
                except StopIteration:
                    active[0] = refill(0)
        while active[0] is not None or active[1] is not None:
            for slot in (0, 1):
                if active[slot] is None:
                    continue
                try:
                    next(active[slot])
                except StopIteration:
                    active[slot] = refill(slot)

    run_two([(ti, phase1_gen(ti)) for ti in range(NT)])
    dma("sp", lnp[0], D["ln2g"], [], [K("lnp")], K("lnp"))
    dma("sp", lnp[1], D["ln2b"], [], [K("lnp")], K("lnp"))

    NU = NE * 2
    hcnt = [0]
    for u in range(NU):
        e_, fh = u // 2, u % 2
        sl = u % 2
        if u + 1 < NU:
            load_unit(u + 1)
        wkey = K("wslot", sl)
        for t4 in range(NT4):
            hs = hcnt[0] % 2
            hcnt[0] += 1
            tok = slice(t4 * TT, (t4 + 1) * TT)
            for fc in range(4):
                gi = 0 + (fc % 2)
                ui = 2 + (fc % 2)
                for dc in range(8):
                    s.add("pe", (lambda sl, dc, fc, gi, tok: lambda e: e.matmul(ps[gi][:, 0:TT], Wg[sl][:, dc, fc * 128:(fc + 1) * 128], X1T[:, dc, tok],
                                                                               start=(dc == 0), stop=(dc == 7)))(sl, dc, fc, gi, tok),
                          reads=[wkey] + [K("X1T", t4 * NSUB + i) for i in range(NSUB)], writes=[("ps", gi)])
                for dc in range(8):
                    s.add("pe", (lambda sl, dc, fc, ui, tok: lambda e: e.matmul(ps[ui][:, 0:TT], Wu[sl][:, dc, fc * 128:(fc + 1) * 128], X1T[:, dc, tok],
                                                                               start=(dc == 0), stop=(dc == 7)))(sl, dc, fc, ui, tok),
                          reads=[wkey] + [K("X1T", t4 * NSUB + i) for i in range(NSUB)], writes=[("ps", ui)])
                si = fc % 2
                s.add("act", (lambda gi, si: lambda e: e.activation(out=sg[si][:, 0:TT], in_=ps[gi][:, 0:TT], func=AF.Silu))(gi, si),
                      reads=[("ps", gi)], writes=[K("sg", si)])
                s.add("dve", (lambda hs, fc, si, ui: lambda e: e.tensor_tensor(out=hT[hs][:, fc, :], in0=sg[si][:, 0:TT], in1=ps[ui][:, 0:TT], op=ALU.mult))(hs, fc, si, ui),
                      reads=[K("sg", si), ("ps", ui)], writes=[K("hT", hs, fc)])
            for sub in range(NSUB):
                ti = t4 * NSUB + sub
                for hh in range(2):
                    di = 4 + ((sub * 2 + hh) % 2)
                    for fc in range(4):
                        s.add("pe", (lambda sl, hs, fc, sub, hh, di: lambda e: e.matmul(ps[di][:, :], hT[hs][:, fc, sub * 128:(sub + 1) * 128], Wd[sl][:, fc, hh * 512:(hh + 1) * 512],
                                                                                         start=(fc == 0), stop=(fc == 3)))(sl, hs, fc, sub, hh, di),
                              reads=[wkey, K("hT", hs, fc)], writes=[("ps", di)])
                    s.add("dve", (lambda ti, hh, di, e_: lambda e: e.scalar_tensor_tensor(out=yacc[:, ti, hh * 512:(hh + 1) * 512], in0=ps[di][:, :], scalar=gates[:, ti, e_:e_ + 1],
                                                                                          in1=yacc[:, ti, hh * 512:(hh + 1) * 512], op0=ALU.mult, op1=ALU.add))(ti, hh, di, e_),
                          reads=[("ps", di), K("gates", ti), K("yacc", ti)], writes=[K("yacc", ti)])

    outs = []

    def ln2_gen(ti):
        sl = ti % 2
        pb = 4 * sl
        v32 = v32s[sl]
        s.add("dve", lambda e: e.tensor_copy(out=v32, in_=yacc[:, ti, :]), reads=[K("yacc", ti)], writes=[K("lnsrc", sl)])
        ln_stats(v32, sl)
        yield
        ln_apply(v32, xt[sl], 0, 1, K("xt", sl), sl)
        yield
        if last:
            outs.append(dma("sp", D["x2"][ti * 128:(ti + 1) * 128, :], xt[sl], [K("xt", sl)], [], K("x2out")))
        else:
            outs.append(dma("sp", D["x2s"][ti * 128:(ti + 1) * 128, :], xt[sl], [K("xt", sl)], [("x2s",)], K("x2out")))
            for q in range(2):
                for kq in range(4):
                    dc = q * 4 + kq
                    s.add("pe", (lambda q, kq, dc: lambda e: e.transpose(out=ps[pb + 2 + q][:, kq * 128:(kq + 1) * 128], in_=xt[sl][:, dc * 128:(dc + 1) * 128], identity=ident))(q, kq, dc),
                          reads=[K("xt", sl), K("c")], writes=[("ps", pb + 2 + q)])
            yield
            tq = ti % 4
            for q in range(2):
                s.add("dve", (lambda q: lambda e: e.tensor_copy(out=xstp[:, q * 4:(q + 1) * 4, tq * 128:(tq + 1) * 128], in_=ps[pb + 2 + q][:, :].rearrange("p (c t) -> p c t", c=4)))(q),
                      reads=[("ps", pb + 2 + q)], writes=[K("xstp", ti)])

    if last:
        run_two([(ti, ln2_gen(ti)) for ti in range(NT)])
    else:
        for p in range(NT // 4):
            run_two([(ti, ln2_gen(ti)) for ti in range(4 * p, 4 * p + 4)])
            for dc in range(8):
                outs.append(dma("sp", D["X2T"][p][dc * 128:(dc + 1) * 128, :], xstp[:, dc, :], [K("xstp", 4 * p + i) for i in range(4)],
                                [("X2T", p)] + [K("xstp", 4 * p + 4 + i) for i in range(4)], K("x2out")))
            if cc2 is not None:
                cc2(p)
    return outs

I32 = mybir.dt.int32
_GROUPS = [[0, 1, 2, 3], [4, 5, 6, 7]]


def _build_fused(S=8192, T=2048, NE=16, depth=2):
    nc = bass.Bass("TRN2", target_bir_lowering=False, num_devices=8)
    D = {}

    def inp(name, shape, dt=F32):
        D[name] = nc.dram_tensor(name, shape, dt, kind="ExternalInput").ap()
    inp("xT", [1024, S])
    inp("xres", [T, 1024])
    inp("qi", [1, 1], I32)
    inp("wA", [depth * 1024, 768])
    inp("gA", [depth * 256, 1])
    inp("biasm", [3, 2, 128, 256])
    C = consts_A()
    for k, v in C.items():
        inp(k, list(v.shape), BF16 if v.dtype == ml_dtypes.bfloat16 else F32)
    inp("wo", [depth * 1024, 1024])
    inp("lnp", [depth * 4 * 128, 1024])
    inp("wr", [1024, 16])
    inp("br", [128, 16])
    inp("ident32", [128, 128])
    for nm in ("wg", "wu", "wd"):
        inp(nm, [depth * NE, 1024, 1024])
    D["out"] = nc.dram_tensor("out", [T, 1024], F32, kind="ExternalOutput").ap()
    yA = [[nc.dram_tensor("yA%d_%d" % (l, p), [256, T], BF16).ap() for p in range(4)] for l in range(depth)]
    Gall = [nc.dram_tensor("Gall%d" % l, [4096, T], BF16).ap() for l in range(depth)]
    Gloc = [nc.dram_tensor("Gloc%d" % l, [1024, T], BF16).ap() for l in range(depth)]
    X2T = [nc.dram_tensor("X2T%d" % p, [1024, 512], BF16).ap() for p in range(4)]
    GX = [nc.dram_tensor("GX%d" % p, [4096, 512], BF16).ap() for p in range(4)]
    x2s = nc.dram_tensor("x2s", [T, 1024], F32).ap()
    with ExitStack() as es:
        arena = es.enter_context(nc.sbuf_tensor("arena", [128, 103 * 1024], BF16))
        qs = es.enter_context(nc.sbuf_tensor("qs", [1, 1], I32))
        reg = es.enter_context(nc.sync.register("qreg"))
        ps = [es.enter_context(nc.psum_tensor("ps%d" % i, [128, 512], F32)) for i in range(8)]
        s = Sched(nc)
        st = {}
        s.add("sp", lambda e: e.dma_start(out=qs[:, :], in_=D["qi"]), writes=[("qs",)], grp=("qs",))

        def ld(e):
            ins = e.reg_load(reg, qs[0:1, 0:1])
            st["reg"] = reg
            return ins
        s.add("sp", ld, reads=[("qs",)], writes=[("qreg",)])
        outs = []
        ccn = [0]
        for l in range(depth):
            last = (l == depth - 1)
            DA = dict(D)
            DA["w"] = D["wA"][l * 1024:(l + 1) * 1024, :]
            DA["g"] = D["gA"][l * 256:(l + 1) * 256, :]
            DA["yA"] = yA[l]
            DA["GX"] = GX

            def cc1(p, l=l):
                ccn[0] += 1
                s.add("pool", lambda e: e.collective_compute("AllGather", ALU.bypass, replica_groups=_GROUPS, ins=[yA[l][p]],
                                                             outs=[Gall[l][p * 1024:(p + 1) * 1024, :]]),
                      reads=[("A%d" % l, "yA", p)], writes=[("B%d" % l, "G", p)], grp=("cc", ccn[0]), inc=1)
            ar = Arena(arena)
            emit_A(nc, s, ar, ps, DA, S, l == 0, "A%d" % l, cc=cc1)
            s.fence()
            DB = dict(D)
            DB["Gall"] = Gall[l]
            DB["Gloc"] = Gloc[l]
            DB["x"] = D["xres"] if l == 0 else x2s
            DB["wo"] = D["wo"][l * 1024:(l + 1) * 1024, :]
            for i, nm in enumerate(("ln1g", "ln1b", "ln2g", "ln2b")):
                DB[nm] = D["lnp"][(l * 4 + i) * 128:(l * 4 + i + 1) * 128, :]
            for nm in ("wg", "wu", "wd"):
                DB[nm] = D[nm][l * NE:(l + 1) * NE]
            DB["x2"] = D["out"]
            DB["x2s"] = x2s
            DB["X2T"] = X2T

            def cc2(p):
                ccn[0] += 1
                s.add("pool", lambda e: e.collective_compute("AllGather", ALU.bypass, replica_groups=_GROUPS, ins=[X2T[p]], outs=[GX[p]]),
                      reads=[("X2T", p)], writes=[("GX", p)], grp=("cc", ccn[0]), inc=1)
            ar = Arena(arena)
            o = emit_B(nc, s, ar, ps, DB, T, "B%d" % l, NE=NE, st=st, last=last, cc2=None if last else cc2)
            if last:
                outs = o
            else:
                s.fence()
        keys = s.emit(final_wait_ops=outs)
        sems = {k: es.enter_context(nc.semaphore("s%d" % i)) for i, k in enumerate(keys)}
        s.run(sems, final_wait_ops=outs)
    return nc, C, len(s.ops), len(keys)


def kernel(x, w_in, g_sb, g_dil, w_out, ln1_g, ln1_b, ln2_g, ln2_b, rel_bias, w_router, b_router, w_gate, w_up, w_down):
    x = np.asarray(x, np.float32)
    B, S, Dm = x.shape
    depth = w_in.shape[0]
    T = 2048
    NE = w_gate.shape[1]
    nc, C, _, _ = _build_fused(S, T, NE, depth)
    f32 = lambda v: np.asarray(v, np.float32)
    rep = lambda v: np.broadcast_to(f32(v)[None, :], (128, v.shape[0]))
    rel_bias = f32(rel_bias)
    perm = np.concatenate([np.concatenate([np.arange(128 * j, 128 * j + 128), 512 + np.arange(128 * j, 128 * j + 128)]) for j in range(4)])
    wo = np.ascontiguousarray(np.concatenate([f32(w_out[l])[perm, :] for l in range(depth)], axis=0))
    lnp = np.ascontiguousarray(np.concatenate([rep(p[l]) for l in range(depth) for p in (ln1_g, ln1_b, ln2_g, ln2_b)], axis=0))
    shared = {"wo": wo, "lnp": lnp, "wr": np.ascontiguousarray(f32(w_router)), "br": np.ascontiguousarray(rep(b_router)),
              "ident32": np.eye(128, dtype=np.float32),
              "wg": f32(w_gate).reshape(depth * NE, 1024, 1024), "wu": f32(w_up).reshape(depth * NE, 1024, 1024),
              "wd": f32(w_down).reshape(depth * NE, 1024, 1024)}
    shared.update(C)
    xTs = [np.ascontiguousarray(x[b].T) for b in range(B)]
    in_maps = []
    for c in range(8):
        b, j = c // 4, c % 4
        wA = []
        gA = []
        for l in range(depth):
            wl = f32(w_in[l])
            wA.append(np.concatenate([wl[:, o + 128 * j:o + 128 * j + 128] for o in (0, 512, 1536, 2048, 1024, 2560)], axis=1))
            gA.append(np.concatenate([f32(g_sb[l])[128 * j:128 * j + 128], f32(g_dil[l])[128 * j:128 * j + 128]])[:, None])
        m = {"xT": xTs[b], "xres": np.ascontiguousarray(x[b, j * T:(j + 1) * T, :]), "qi": np.array([[1024 * j]], np.int32),
             "wA": np.ascontiguousarray(np.concatenate(wA, axis=0)), "gA": np.ascontiguousarray(np.concatenate(gA, axis=0)),
             "biasm": dil_bias_tables(rel_bias[:, 2 * j:2 * j + 2])}
        m.update(shared)
        in_maps.append(m)
    res = run_bass_kernel_spmd(nc, in_maps, core_ids=list(range(8)))
    out = np.zeros((B, S, Dm), np.float32)
    for c in range(8):
        b, j = c // 4, c % 4
        out[b, j * T:(j + 1) * T, :] = np.asarray(res.results[c]["out"], np.float32)
    return out
```

```python
from contextlib import ExitStack
from concourse.bass_utils import run_bass_kernel_spmd
import numpy as np
import concourse.bass as bass
import concourse.mybir as mybir

F32 = mybir.dt.float32
BF16 = mybir.dt.bfloat16
AF = mybir.ActivationFunctionType
ALU = mybir.AluOpType

COMPUTE = ("pe", "act", "dve", "pool")


class Sched:
    def __init__(self, nc):
        self.nc = nc
        self.ops = []
        self.last_writer = {}
        self.readers = {}
        self.dma_groups = {}
        self.fence_deps = set()
        self.fence_pending = set()

    def fence(self):
        last = {}
        for i, op in enumerate(self.ops):
            k = ("dma", op["grp"]) if op["dma"] else ("eng", op["eng"])
            last[k] = i
        self.fence_deps = set(last.values())
        self.fence_pending = {"pe", "act", "dve", "pool", "sp"}

    def add(self, eng, fn, reads=(), writes=(), grp=None, inc=16):
        idx = len(self.ops)
        deps = set()
        if eng in self.fence_pending:
            deps |= self.fence_deps
            self.fence_pending.discard(eng)
        for b in reads:
            w = self.last_writer.get(b)
            if w is not None:
                deps.add(w)
        for b in writes:
            w = self.last_writer.get(b)
            if w is not None:
                deps.add(w)
            for r in self.readers.get(b, ()):
                deps.add(r)
        deps.discard(idx)
        is_dma = grp is not None
        if eng == "pe":
            deps = {d for d in deps if not (self.ops[d]["eng"] == "pe" and not self.ops[d]["dma"])}
        op = dict(eng=eng, fn=fn, deps=deps, dma=is_dma, grp=grp, needed=False, inc=(inc if is_dma else 1))
        self.ops.append(op)
        for b in reads:
            self.readers.setdefault(b, []).append(idx)
        for b in writes:
            self.last_writer[b] = idx
            self.readers[b] = []
        return idx

    def emit(self, final_wait_ops=()):
        nc = self.nc
        ops = self.ops
        for op in ops:
            for d in op["deps"]:
                ops[d]["needed"] = True
        for d in final_wait_ops:
            ops[d]["needed"] = True
        sem_names = []
        counters = {}
        for i, op in enumerate(ops):
            if not op["needed"]:
                continue
            key = ("dma", op["grp"]) if op["dma"] else ("eng", op["eng"])
            if key not in counters:
                counters[key] = 0
                sem_names.append(key)
            counters[key] += op["inc"]
            op["sem"] = key
            op["val"] = counters[key]
        self.sem_keys = sem_names
        return sem_names

    def run(self, sems, final_wait_ops=()):
        nc = self.nc
        ops = self.ops
        per_eng = {}
        for i, op in enumerate(ops):
            per_eng.setdefault(op["eng"], []).append(i)

        def body(engname, eng):
            known = {}
            for i in per_eng.get(engname, []):
                op = ops[i]
                need = {}
                for d in op["deps"]:
                    p = ops[d]
                    k = p["sem"]
                    need[k] = max(need.get(k, 0), p["val"])
                for k, v in need.items():
                    if known.get(k, 0) >= v:
                        continue
                    eng.wait_ge(sems[k], v)
                    known[k] = v
                ins = op["fn"](eng)
                if op["needed"]:
                    ins.then_inc(sems[op["sem"]], op["inc"])
            if engname == "sp":
                fin = {}
                for d in final_wait_ops:
                    p = ops[d]
                    fin[p["sem"]] = max(fin.get(p["sem"], 0), p["val"])
                for k, v in fin.items():
                    if known.get(k, 0) < v:
                        eng.wait_ge(sems[k], v)
                        known[k] = v

        with nc.Block() as block:
            @block.sync
            def _(e):
                body("sp", e)

            @block.tensor
            def _(e):
                body("pe", e)

            @block.scalar
            def _(e):
                body("act", e)

            @block.vector
            def _(e):
                body("dve", e)

            @block.gpsimd
            def _(e):
                body("pool", e)


import math
import numpy as np
import ml_dtypes
import concourse.bass as bass
import concourse.mybir as mybir

NEG = -30000.0
SKIP = ['dveonly']
SC = 2048


def t5_bucket_np(dist):
    max_exact = 16
    d = np.maximum(dist, 0)
    large = max_exact + (np.log(np.maximum(d, 1).astype(np.float32) / np.float32(max_exact))
                         / np.float32(math.log(2048 / max_exact)) * np.float32(32 - max_exact)).astype(np.int32)
    large = np.minimum(large, 31)
    return np.where(d < max_exact, d, large)


def consts_A():
    j = np.arange(128)[:, None]
    s = np.arange(128)[None, :]
    negtri = np.where(j >= s, -1.0, 0.0).astype(ml_dtypes.bfloat16)
    negones = np.full((128, 128), -1.0, dtype=ml_dtypes.bfloat16)
    ident = np.eye(128, dtype=ml_dtypes.bfloat16)
    t = np.arange(512)[None, :]
    sbmask = np.stack([np.where(128 * a + j >= t, NEG, 0.0) for a in range(4)], 0).astype(ml_dtypes.bfloat16)
    onesblk = np.zeros((128, 128), np.float32)
    onesblk[:64, :64] = 1.0
    onesblk[64:, 64:] = 1.0
    sel65 = np.zeros((65, 64), np.float32)
    sel65[64, :] = 1.0
    ones65 = np.zeros((65, 64), np.float32)
    ones65[:64, :] = 1.0
    return dict(negtri=negtri, negones=negones, ident=ident, sbmask=sbmask, onesblk=onesblk, sel65=sel65, ones65=ones65)


def dil_bias_tables(rel_bias_heads):
    kj = np.arange(128)[:, None]
    qi = np.arange(128)[None, :]
    out = np.zeros((3, 2, 128, 256), np.float32)
    for ri, r in enumerate((1, 4, 16)):
        steps_cur = qi - kj
        steps_prev = qi - kj + 128
        b_cur = t5_bucket_np(np.maximum(steps_cur, 0) * r)
        b_prev = t5_bucket_np(np.maximum(steps_prev, 0) * r)
        for h in range(2):
            cur = np.where(steps_cur >= 0, rel_bias_heads[b_cur, h], np.float32(NEG))
            prev = np.where(steps_prev <= 128, rel_bias_heads[b_prev, h], np.float32(NEG))
            out[ri, h, :, :128] = prev
            out[ri, h, :, 128:] = cur
    return out


class Arena:
    def __init__(self, ap_bf16):
        self.ap = ap_bf16
        self.off = 0
        self.total = ap_bf16.shape[1]

    def take(self, nelem, dtype=BF16):
        nb = nelem * (4 if dtype == F32 else 2)
        nb = (nb + 63) // 64 * 64
        n16 = nb // 2
        assert self.off + n16 <= self.total, ("arena overflow", self.off, n16, self.total)
        v = self.ap[:, self.off:self.off + n16]
        self.off += n16
        if dtype == F32:
            return v.bitcast(F32)[:, :nelem]
        return v[:, :nelem]


def emit_A(nc, s, ar, ps, D, S, src_is_f32, lname, stop=99, cc=None):
    NSC = S // SC
    NB = S // 128
    L = lname
    K = lambda *a: (L,) + a

    Wb = ar.take(8 * 768).rearrange("p (f c) -> p f c", f=8)
    XT = ar.take(8 * SC).rearrange("p (f t) -> p f t", f=8)
    QTs = ar.take(SC)
    QTsB = ar.take(SC)
    QTd = ar.take(SC)
    QTdB = ar.take(SC)
    KTs = ar.take(S)
    KTd = ar.take(S)
    Vs = ar.take(NB * 128).rearrange("p (b c) -> p b c", c=128)
    NW1 = NB
    Vd1 = ar.take(NW1 * 132).rearrange("p (b h c) -> p b h c", h=2, c=66)
    Vd4 = ar.take(32 * 132).rearrange("p (b h c) -> p b h c", h=2, c=66)
    Vd16 = ar.take(32 * 132).rearrange("p (b h c) -> p b h c", h=2, c=66)
    negtri = ar.take(128)
    negones = ar.take(128)
    ident = ar.take(128)
    sbmask = ar.take(4 * 512).rearrange("p (a t) -> p a t", a=4)
    onesblk = ar.take(128, F32)
    sel65 = ar.take(64, F32)
    ones65 = ar.take(64, F32)
    gvec = ar.take(4, F32)
    biasm16 = ar.take(6 * 256).rearrange("p (r h c) -> p r h c", r=3, h=2)
    E32 = [ar.take(512, F32) for _ in range(2)]
    Lp16 = [ar.take(512) for _ in range(4)]
    LA32 = [ar.take(512, F32) for _ in range(2)]
    A16 = [ar.take(512) for _ in range(4)]
    ncar = [ar.take(512, F32) for _ in range(2)]
    P16 = [ar.take(512) for _ in range(6)]
    acc = [ar.take(SC, F32) for _ in range(2)]
    o32 = ar.take(512, F32)
    sq32 = ar.take(512, F32)
    r32 = ar.take(512, F32)
    Yb = [ar.take(512) for _ in range(2)]

    def dma(eng, out, in_, reads, writes, grp):
        return s.add(eng, lambda e: e.dma_start(out=out, in_=in_), reads=reads, writes=writes, grp=grp)

    for fc in range(8):
        dma("pool", Wb[:, fc, :], D["w"][fc * 128:(fc + 1) * 128, :], [], [K("W")], K("W"))
    dma("sp", negtri, D["negtri"], [], [K("c")], K("c"))
    dma("sp", negones, D["negones"], [], [K("c")], K("c"))
    dma("sp", ident, D["ident"], [], [K("c")], K("c"))
    for a in range(4):
        dma("sp", sbmask[:, a, :], D["sbmask"][a], [], [K("c")], K("c"))
    dma("sp", onesblk, D["onesblk"], [], [K("c")], K("c"))
    dma("sp", sel65[0:65, :], D["sel65"], [], [K("c")], K("c"))
    dma("sp", ones65[0:65, :], D["ones65"], [], [K("c")], K("c"))
    dma("sp", gvec[:, 0:1], D["g"][0:128, :], [], [K("c")], K("c"))
    dma("sp", gvec[0:64, 1:2], D["g"][128:192, :], [], [K("c")], K("c"))
    dma("sp", gvec[0:64, 2:3], D["g"][192:256, :], [], [K("c")], K("c"))
    for ri in range(3):
        for h in range(2):
            dma("pool", biasm16[:, ri, h, :], D["biasm"][ri, h], [], [K("c")], K("c"))
    for vb in (Vd1, Vd4, Vd16):
        s.add("dve", (lambda vb: lambda e: e.memset(vb[:, :, :, 64:65], 1.0))(vb), writes=[K("vones")])

    psX = [ps[6], ps[7]]
    def dummy_out():
        return [dma("sp", D["yT"][0:128, 0:512], QTs[:, 0:512], [K("c"), K("W"), K("XT"), K("vones"), K("QTs"), K("QTd"), K("Vd4", 0), K("Vd16", 0), K("Vs", 0)], [], K("yout"))]
    if stop == 0:
        return dummy_out()
    xcnt = [0]

    def nextX():
        i = xcnt[0] % 2
        xcnt[0] += 1
        return i

    evac_cnt = [0]

    def evac(out, in_, reads, writes, scale=None):
        evac_cnt[0] += 1
        if evac_cnt[0] % 2 == 0 or 'dveonly' in SKIP:
            if scale is None:
                return s.add("dve", lambda e: e.tensor_copy(out=out, in_=in_), reads=reads, writes=writes)
            return s.add("dve", lambda e: e.tensor_scalar(out=out, in0=in_, scalar1=float(scale), scalar2=None, op0=ALU.mult),
                         reads=reads, writes=writes)
        sc = 1.0 if scale is None else float(scale)
        return s.add("act", lambda e: e.activation(out=out, in_=in_, func=AF.Copy, scale=sc), reads=reads, writes=writes)

    out_dmas = []
    ycnt = [0]

    def ss(start, n, step):
        return slice(start, start + (n - 1) * step + 1, step)

    QTs2 = [QTs, QTsB]
    QTd2 = [QTd, QTdB]

    def make(sc_i):
        t0 = sc_i * SC

        def load_xt(sci):
            for fc in range(8):
                if src_is_f32:
                    dma("pool", XT[:, fc, :], D["xT"][fc * 128:(fc + 1) * 128, sci * SC:(sci + 1) * SC], [], [K("XT")], K("XT"))
                else:
                    for p in range(4):
                        dma("sp", XT[:, fc, p * 512:(p + 1) * 512], D["GX"][p][sci * 1024 + fc * 128:sci * 1024 + (fc + 1) * 128, :], [("GX", p)], [K("XT")], K("XT"))

        def proj_gen():
            if sc_i == 0:
                load_xt(0)
            for c in range(4):
                tl = c * 512
                for kind, col0, dst, scale in (("qs", 0, QTs2[sc_i % 2][:, tl:tl + 512], 0.125), ("ks", 128, KTs[:, t0 + tl:t0 + tl + 512], None),
                                               ("qd", 256, QTd2[sc_i % 2][:, tl:tl + 512], 0.125), ("kd", 384, KTd[:, t0 + tl:t0 + tl + 512], None)):
                    yield
                    xi = nextX()
                    for fc in range(8):
                        s.add("pe", (lambda xi, fc, col0, tl: lambda e: e.matmul(psX[xi][:, :], Wb[:, fc, col0:col0 + 128], XT[:, fc, tl:tl + 512],
                                                                                 start=(fc == 0), stop=(fc == 7)))(xi, fc, col0, tl),
                              reads=[K("W"), K("XT")], writes=[("ps", 6 + xi)])
                    wkey = {"qs": K("QTs", sc_i % 2), "ks": K("KTs", sc_i), "qd": K("QTd", sc_i % 2), "kd": K("KTd", sc_i)}[kind]
                    evac(dst, psX[xi][:, :], [("ps", 6 + xi)], [wkey], scale)
                for sub in range(4 if 'vnat' not in SKIP else 0):
                    tt = tl + sub * 128
                    blk = (t0 + tt) // 128
                    yield
                    xi = nextX()
                    for fc in range(8):
                        s.add("pe", (lambda xi, fc, tt: lambda e: e.matmul(psX[xi][:, 0:256], XT[:, fc, tt:tt + 128], Wb[:, fc, 512:768],
                                                                           start=(fc == 0), stop=(fc == 7)))(xi, fc, tt),
                              reads=[K("W"), K("XT")], writes=[("ps", 6 + xi)])
                    if 'evs' not in SKIP: evac(Vs[:, blk, :], psX[xi][:, 0:128], [("ps", 6 + xi)], [K("Vs", blk)])
                    for hh in range(2 if 'evd1' not in SKIP else 0):
                        evac(Vd1[:, blk, hh, 0:64], psX[xi][:, 128 + 64 * hh:192 + 64 * hh], [("ps", 6 + xi)], [K("Vd1", blk)])
                n4 = 4 * sc_i + c
                yield
                xi = nextX()
                for cls in range(4 if 'v4' not in SKIP else 0):
                    for fc in range(8):
                        s.add("pe", (lambda xi, fc, cls, tl: lambda e: e.matmul(psX[xi][:, cls * 128:(cls + 1) * 128],
                                                                                XT[:, fc, ss(tl + cls, 128, 4)], Wb[:, fc, 640:768],
                                                                                start=(fc == 0), stop=(fc == 7)))(xi, fc, cls, tl),
                              reads=[K("W"), K("XT")], writes=[("ps", 6 + xi)])
                slot0 = (n4 % 8) * 4
                for kk_ in range(4 if 'v4' not in SKIP else 0):
                    for hh in range(2):
                        evac(Vd4[:, slot0 + kk_, hh, 0:64], psX[xi][:, kk_ * 128 + 64 * hh:kk_ * 128 + 64 * hh + 64], [("ps", 6 + xi)], [K("Vd4", n4 % 8)])
            for c4 in range(4 if 'v16' not in SKIP else 0):
                yield
                xi = nextX()
                for k in range(4):
                    cls = c4 * 4 + k
                    for fc in range(8):
                        s.add("pe", (lambda xi, fc, cls, k: lambda e: e.matmul(psX[xi][:, k * 128:(k + 1) * 128],
                                                                               XT[:, fc, ss(cls, 128, 16)], Wb[:, fc, 640:768],
                                                                               start=(fc == 0), stop=(fc == 7)))(xi, fc, cls, k),
                              reads=[K("W"), K("XT")], writes=[("ps", 6 + xi)])
                slot0 = (sc_i % 2) * 16 + c4 * 4
                for kk_ in range(4 if 'v16' not in SKIP else 0):
                    for hh in range(2):
                        evac(Vd16[:, slot0 + kk_, hh, 0:64], psX[xi][:, kk_ * 128 + 64 * hh:kk_ * 128 + 64 * hh + 64], [("ps", 6 + xi)], [K("Vd16", sc_i % 2)])

            yield
            if sc_i + 1 < NSC:
                load_xt(sc_i + 1)
            yield

        def dil_gen():
            groups = []
            for h in range(2):
                for ri, r in enumerate((1, 4, 16)):
                    nblk_sc = SC // (128 * r)
                    for c in range(r):
                        n0 = sc_i * nblk_sc
                        for g0 in range(n0, n0 + nblk_sc, 4):
                            blks = list(range(g0, min(g0 + 4, n0 + nblk_sc)))
                            groups.append(dict(h=h, ri=ri, r=r, c=c, g0=g0, blks=blks, gi=len(groups)))
            SBANKS = [(3, 6), (3, 6), (3, 6)]

            def d_s1(g):
                h, ri, r, c, blks, gi = g["h"], g["ri"], g["r"], g["c"], g["blks"], g["gi"]
                hp = slice(64 * h, 64 * h + 64)
                units = []
                for n in blks:
                    units += [(n - 1, n), (n, n)]
                g["uinfo"] = []
                g["tls"] = []
                for t_i in range(0, len(units), 4):
                    tu = units[t_i:t_i + 4]
                    bank = SBANKS[gi % 3][t_i // 4]
                    pslot = (gi % 3) * 2 + t_i // 4
                    lo = None
                    for u, (kb, n) in enumerate(tu):
                        co = u * 128
                        g["uinfo"].append((pslot, co, kb))
                        if kb < 0:
                            continue
                        if lo is None:
                            lo = co
                        qstart = c + r * 128 * n - t0
                        kstart = c + r * 128 * kb
                        role = u % 2
                        s.add("pe", (lambda bank, co, kstart, qstart, r, hp: lambda e: e.matmul(
                            ps[bank][:, co:co + 128], KTd[hp, ss(kstart, 128, r)], QTd2[sc_i % 2][hp, ss(qstart, 128, r)],
                            start=True, stop=False))(bank, co, kstart, qstart, r, hp),
                            reads=[K("KTd", kstart // SC), K("QTd", sc_i % 2)], writes=[("ps", bank)])
                        s.add("pe", (lambda bank, co, ri, h, role: lambda e: e.matmul(
                            ps[bank][:, co:co + 128], ident, biasm16[:, ri, h, role * 128:(role + 1) * 128],
                            start=False, stop=True))(bank, co, ri, h, role),
                            reads=[K("c")], writes=[("ps", bank)])
                    g["tls"].append((bank, pslot, lo, len(tu) * 128))

            def d_s2(g):
                for (bank, pslot, lo, hi) in g["tls"]:
                    s.add("act", (lambda bank, pslot, lo, hi: lambda e: e.activation(out=P16[pslot][:, lo:hi], in_=ps[bank][:, lo:hi], func=AF.Exp))(bank, pslot, lo, hi),
                          reads=[("ps", bank)], writes=[K("P", pslot)])

            def d_s3(g):
                h, r, c, blks, gi = g["h"], g["r"], g["c"], g["blks"], g["gi"]
                ob = 7
                vkey = {1: "Vd1", 4: "Vd4", 16: "Vd16"}[r]
                for bi, n in enumerate(blks):
                    first = True
                    for uu in (2 * bi, 2 * bi + 1):
                        pslot, co, kb = g["uinfo"][uu]
                        if kb < 0:
                            continue
                        if r == 1:
                            vap = Vd1[:, kb, h, 0:65]
                        elif r == 4:
                            vap = Vd4[:, (kb % 8) * 4 + c, h, 0:65]
                        else:
                            vap = Vd16[:, (kb % 2) * 16 + c, h, 0:65]
                        last = (uu == 2 * bi + 1)
                        s.add("pe", (lambda ob, bi, vap, pslot, co, first, last: lambda e: e.matmul(
                            ps[ob][0:65, bi * 128:(bi + 1) * 128], vap, P16[pslot][:, co:co + 128],
                            start=first, stop=last))(ob, bi, vap, pslot, co, first, last),
                            reads=[K("P", pslot), K(vkey, kb if r == 1 else (kb % 8 if r == 4 else kb % 2)), K("vones")], writes=[("ps", ob)])
                        first = False

            def d_s4(g):
                h, r, c, blks, gi, g0 = g["h"], g["r"], g["c"], g["blks"], g["gi"], g["g0"]
                ob = 7
                nb_ = len(blks)
                col = c + r * (128 * g0) - t0
                if r == 1:
                    s.add("dve", (lambda h, col, nb_, ob: lambda e: e.tensor_copy(out=acc[h][0:65, col:col + 128 * nb_], in_=ps[ob][0:65, 0:128 * nb_]))(h, col, nb_, ob),
                          reads=[("ps", ob)], writes=[K("acc", h)])
                else:
                    s.add("dve", (lambda h, col, nb_, r, ob: lambda e: e.tensor_tensor(
                        out=acc[h][0:65, ss(col, 128 * nb_, r)], in0=ps[ob][0:65, 0:128 * nb_], in1=acc[h][0:65, ss(col, 128 * nb_, r)], op=ALU.add))(h, col, nb_, r, ob),
                        reads=[("ps", ob), K("acc", h)], writes=[K("acc", h)])

            for g in groups:
                d_s1(g)
                yield
                d_s2(g)
                yield
                d_s3(g)
                yield
                d_s4(g)
                yield
            for h in range(2):
                for tq in range(4):
                    yield
                    cs = slice(tq * 512, tq * 512 + 512)
                    xi = nextX()
                    s.add("pe", (lambda xi, h, cs: lambda e: e.matmul(psX[xi][0:64, :], sel65[0:65, :], acc[h][0:65, cs], start=True, stop=True))(xi, h, cs),
                          reads=[K("acc", h), K("c")], writes=[("ps", 6 + xi)])
                    s.add("dve", (lambda xi: lambda e: e.reciprocal(out=r32[0:64, :], in_=psX[xi][0:64, :]))(xi), reads=[("ps", 6 + xi)], writes=[K("r32")])
                    s.add("dve", (lambda h, cs: lambda e: e.tensor_tensor(out=o32[0:64, :], in0=acc[h][0:64, cs], in1=r32[0:64, :], op=ALU.mult))(h, cs),
                          reads=[K("acc", h), K("r32")], writes=[K("o32")])
                    s.add("act", lambda e: e.activation(out=sq32[0:64, :], in_=o32[0:64, :], func=AF.Square), reads=[K("o32")], writes=[K("sq32")])
                    xi2 = nextX()
                    s.add("pe", (lambda xi2: lambda e: e.matmul(psX[xi2][0:64, :], ones65[0:64, :], sq32[0:64, :], start=True, stop=True))(xi2),
                          reads=[K("sq32"), K("c")], writes=[("ps", 6 + xi2)])
                    s.add("act", (lambda xi2: lambda e: e.activation(out=r32[0:64, :], in_=psX[xi2][0:64, :], func=AF.Sqrt, scale=1.0 / 64, bias=1e-6))(xi2),
                          reads=[("ps", 6 + xi2)], writes=[K("r32")])
                    s.add("dve", lambda e: e.reciprocal(out=sq32[0:64, :], in_=r32[0:64, :]), reads=[K("r32")], writes=[K("sq32")])
                    yi = ycnt[0] % 2
                    ycnt[0] += 1
                    s.add("dve", (lambda yi, h: lambda e: e.scalar_tensor_tensor(out=Yb[yi][0:64, :], in0=o32[0:64, :], scalar=gvec[0:64, 1 + h:2 + h],
                                                                                  in1=sq32[0:64, :], op0=ALU.mult, op1=ALU.mult))(yi, h),
                          reads=[K("o32"), K("sq32"), K("c")], writes=[K("Yb", yi)])
                    od = dma("sp", D["yA"][sc_i][128 + 64 * h:128 + 64 * h + 64, tq * 512:tq * 512 + 512], Yb[yi][0:64, :], [K("Yb", yi)], [K("yA", sc_i)], K("yout"))
                    out_dmas.append(od)


        def run_sb(drip):
            jobs = []
            for qt in range(4 * sc_i, 4 * sc_i + 4):
                kbl = list(range(4 * qt + 3, -1, -1))
                for ki, kb in enumerate(kbl):
                    for h in range(2):
                        jobs.append(dict(qt=qt, kb=kb, h=h, first=(ki == 0), last=(ki == len(kbl) - 1), idx=len(jobs)))

            def stage1(j):
                qt, kb, h, i = j["qt"], j["kb"], j["h"], j["idx"]
                hp = slice(64 * h, 64 * h + 64)
                a = kb - 4 * qt
                bb, se, sl = i % 3, i % 2, i % 4
                ql = (qt - 4 * sc_i) * 512
                diag = a >= 0
                kk = K("KTs", (kb * 128) // SC)
                s.add("pe", lambda e: e.matmul(ps[bb][:, :], KTs[hp, kb * 128:(kb + 1) * 128], QTs2[sc_i % 2][hp, ql:ql + 512], start=True, stop=False),
                      reads=[kk, K("QTs", sc_i % 2)], writes=[("ps", bb)])
                if diag:
                    s.add("pe", lambda e: e.matmul(ps[bb][:, :], ident, sbmask[:, a, :], start=False, stop=False), reads=[K("c")], writes=[("ps", bb)])
                s.add("act", lambda e: e.activation(out=E32[se], in_=ps[bb][:, :], func=AF.Exp), reads=[("ps", bb)], writes=[K("E", se)])
                s.add("act", lambda e: e.activation(out=Lp16[sl], in_=E32[se], func=AF.Ln, bias=1.0), reads=[K("E", se)], writes=[K("Lp", sl)])

            def stage2(j):
                qt, kb, h, i = j["qt"], j["kb"], j["h"], j["idx"]
                bb, sl, sla, sa16 = i % 3, i % 4, i % 2, i % 4
                cb = 5
                s.add("pe", lambda e: e.matmul(ps[bb][:, :], negtri, Lp16[sl], start=False, stop=True), reads=[K("Lp", sl), K("c")], writes=[("ps", bb)])
                if not j["last"]:
                    s.add("pe", lambda e: e.matmul(ps[cb][:, :], negones, Lp16[sl], start=True, stop=True), reads=[K("Lp", sl), K("c")], writes=[("ps", cb)])
                if j["first"]:
                    s.add("act", lambda e: e.activation(out=A16[sa16], in_=ps[bb][:, :], func=AF.Exp), reads=[("ps", bb)], writes=[K("A", sa16)])
                    if not j["last"]:
                        s.add("dve", lambda e: e.tensor_copy(out=ncar[h], in_=ps[cb][:, :]), reads=[("ps", cb)], writes=[K("ncar", h)])
                else:
                    s.add("dve", lambda e: e.tensor_tensor(out=LA32[sla], in0=ps[bb][:, :], in1=ncar[h], op=ALU.add),
                          reads=[("ps", bb), K("ncar", h)], writes=[K("LA", sla)])
                    if not j["last"]:
                        s.add("dve", lambda e: e.tensor_tensor(out=ncar[h], in0=ps[cb][:, :], in1=ncar[h], op=ALU.add),
                              reads=[("ps", cb), K("ncar", h)], writes=[K("ncar", h)])
                    s.add("act", lambda e: e.activation(out=A16[sa16], in_=LA32[sla], func=AF.Exp), reads=[K("LA", sla)], writes=[K("A", sa16)])

            def stage3(j):
                qt, kb, h, i = j["qt"], j["kb"], j["h"], j["idx"]
                sa16 = i % 4
                s.add("pe", lambda e: e.matmul(ps[4][64 * h:64 * h + 64, :], Vs[:, kb, 64 * h:64 * h + 64], A16[sa16],
                                               start=j["first"], stop=j["last"]),
                      reads=[K("A", sa16), K("Vs", kb)], writes=[("ps", 4)])
                if j["last"] and h == 1:
                    finalize_sb(qt)

            def finalize_sb(qt):
                s.add("dve", lambda e: e.tensor_copy(out=o32, in_=ps[4][:, :]), reads=[("ps", 4)], writes=[K("o32")])
                s.add("act", lambda e: e.activation(out=sq32, in_=o32, func=AF.Square), reads=[K("o32")], writes=[K("sq32")])
                s.add("pe", lambda e: e.matmul(ps[5][:, :], onesblk, sq32, start=True, stop=True), reads=[K("sq32"), K("c")], writes=[("ps", 5)])
                s.add("act", lambda e: e.activation(out=r32, in_=ps[5][:, :], func=AF.Sqrt, scale=1.0 / 64, bias=1e-6), reads=[("ps", 5)], writes=[K("r32")])
                s.add("dve", lambda e: e.reciprocal(out=sq32, in_=r32), reads=[K("r32")], writes=[K("sq32")])
                yi = ycnt[0] % 2
                ycnt[0] += 1
                s.add("dve", lambda e: e.scalar_tensor_tensor(out=Yb[yi], in0=o32, scalar=gvec[:, 0:1], in1=sq32, op0=ALU.mult, op1=ALU.mult),
                      reads=[K("o32"), K("sq32"), K("c")], writes=[K("Yb", yi)])
                od = dma("sp", D["yA"][sc_i][0:128, (qt % 4) * 512:(qt % 4) * 512 + 512], Yb[yi], [K("Yb", yi)], [K("yA", sc_i)], K("yout"))
                out_dmas.append(od)

            n = len(jobs)
            for step in range(n + 2):
                if step < n:
                    stage1(jobs[step])
                if 0 <= step - 1 < n:
                    stage2(jobs[step - 1])
                if 0 <= step - 2 < n:
                    stage3(jobs[step - 2])
                drip(n)
        return proj_gen, dil_gen, run_sb

    mk = [make(i) for i in range(NSC)]
    for _ in mk[0][0]():
        pass
    for sc_i in range(NSC):
        tasks = [mk[sc_i][1]()]
        nsteps = 205
        if sc_i + 1 < NSC:
            tasks.append(mk[sc_i + 1][0]())
            nsteps += 45
        state = [tasks, 0.0, nsteps]

        def drip(njobs, state=state):
            state[1] += state[2] / float(njobs)
            while state[1] >= 1.0 and state[0]:
                state[1] -= 1.0
                try:
                    next(state[0][0])
                except StopIteration:
                    state[0].pop(0)
        mk[sc_i][2](drip)
        for t in tasks:
            for _ in t:
                pass
        if cc is not None:
            cc(sc_i)
    return out_dmas


import numpy as np
import ml_dtypes
import concourse.bass as bass
import concourse.mybir as mybir

ALPHA = (2.0 * 2) ** 0.25
LN_EPS = 1e-5
AX = mybir.AxisListType


def emit_B(nc, s, ar, ps, D, T, lname, NE=16, st=None, last=True, cc2=None):
    L = lname
    K = lambda *a: (L,) + a
    NT = T // 128
    TT = min(512, T)
    NT4 = T // TT
    NSUB = TT // 128

    yacc = ar.take(NT * 1024, F32).rearrange("p (t d) -> p t d", t=NT)
    X1T = ar.take(8 * T).rearrange("p (c t) -> p c t", c=8)
    wslot = [ar.take(3 * 4096) for _ in range(2)]
    Wg = [w[:, 0:4096].rearrange("p (c f) -> p c f", c=8) for w in wslot]
    Wu = [w[:, 4096:8192].rearrange("p (c f) -> p c f", c=8) for w in wslot]
    Wd = [w[:, 8192:12288].rearrange("p (c f) -> p c f", c=4) for w in wslot]
    Wo = wslot[1][:, 0:8192].rearrange("p (c f) -> p c f", c=8)
    YT = [ar.take(8 * 128).rearrange("p (c t) -> p c t", c=8) for _ in range(2)]
    xt = [ar.take(1024, F32) for _ in range(2)]
    v32s = [ar.take(1024, F32) for _ in range(2)]
    lnp = [ar.take(1024, F32) for _ in range(2)]
    X1T32s = [ar.take(8 * 128, F32).rearrange("p (c t) -> p c t", c=8) for _ in range(2)]
    hT = [ar.take(4 * TT).rearrange("p (c t) -> p c t", c=4) for _ in range(2)]
    sg = [ar.take(TT, F32) for _ in range(2)]
    gates = ar.take(NT * 16, F32).rearrange("p (t e) -> p t e", t=NT)
    Wr = ar.take(8 * 16, F32).rearrange("p (c e) -> p c e", c=8)
    br = ar.take(16, F32)
    ident = ar.take(128, F32)
    stats2 = [ar.take(16, F32) for _ in range(2)]
    mv2 = [ar.take(4, F32) for _ in range(2)]
    rt2 = [[ar.take(16, F32) for _ in range(6)] for _ in range(2)]
    rs2 = [[ar.take(4, F32) for _ in range(6)] for _ in range(2)]
    xstp = ar.take(8 * 512).rearrange("p (c t) -> p c t", c=8)

    def dma(eng, out, in_, reads, writes, grp):
        return s.add(eng, lambda e: e.dma_start(out=out, in_=in_), reads=reads, writes=writes, grp=grp)

    for i, nm in enumerate(("ln1g", "ln1b")):
        dma("sp", lnp[i], D[nm], [], [K("lnp")], K("lnp"))
    for dc in range(8):
        dma("sp", Wr[:, dc, :], D["wr"][dc * 128:(dc + 1) * 128, :], [], [K("c")], K("c"))
    dma("sp", br, D["br"], [], [K("c")], K("c"))
    dma("sp", ident, D["ident32"], [], [K("c")], K("c"))
    for ec in range(8):
        dma("pool", Wo[:, ec, :], D["wo"][ec * 128:(ec + 1) * 128, :], [], [K("wslot", 1)], K("wslot", 1))

    def load_unit(u):
        e, fh = u // 2, u % 2
        sl = u % 2
        key = K("wslot", sl)
        for dc in range(8):
            dma("pool", Wg[sl][:, dc, :], D["wg"][e, dc * 128:(dc + 1) * 128, fh * 512:(fh + 1) * 512], [], [key], key)
        for dc in range(8):
            dma("pool", Wu[sl][:, dc, :], D["wu"][e, dc * 128:(dc + 1) * 128, fh * 512:(fh + 1) * 512], [], [key], key)
        for fc in range(4):
            dma("pool", Wd[sl][:, fc, :], D["wd"][e, fh * 512 + fc * 128:fh * 512 + (fc + 1) * 128, :], [], [key], key)

    load_unit(0)

    if D.get("Gall") is not None:
        s.add("sp", lambda e: e.dma_start(out=D["Gloc"], in_=D["Gall"][bass.ds(e.snap(st["reg"]), 1024), :]),
              reads=[K("G", q_) for q_ in range(4)], writes=[K("Gloc")], grp=K("Gloc"))
    def ln_stats(src, sl):
        for hh in range(2):
            s.add("dve", (lambda hh: lambda e: e.bn_stats(out=stats2[sl][:, hh * 6:(hh + 1) * 6], in_=src[:, hh * 512:(hh + 1) * 512]))(hh),
                  reads=[K("lnsrc", sl)], writes=[K("stats", sl)])
        s.add("dve", lambda e: e.bn_aggr(out=mv2[sl][:, 0:2], in_=stats2[sl][:, 0:12]), reads=[K("stats", sl)], writes=[K("mv", sl)])
        s.add("act", lambda e: e.activation(out=mv2[sl][:, 2:3], in_=mv2[sl][:, 1:2], func=AF.Sqrt, bias=LN_EPS), reads=[K("mv", sl)], writes=[K("mvb", sl)])

    def ln_apply(src, dst, gi, bi, dkey, sl):
        s.add("dve", lambda e: e.reciprocal(out=mv2[sl][:, 3:4], in_=mv2[sl][:, 2:3]), reads=[K("mvb", sl)], writes=[K("mvc", sl)])
        s.add("dve", lambda e: e.tensor_scalar(out=src, in0=src, scalar1=mv2[sl][:, 0:1], scalar2=mv2[sl][:, 3:4], op0=ALU.subtract, op1=ALU.mult),
              reads=[K("mv", sl), K("mvc", sl), K("lnsrc", sl)], writes=[K("lnsrc", sl)])
        s.add("dve", lambda e: e.tensor_tensor(out=src, in0=src, in1=lnp[gi], op=ALU.mult), reads=[K("lnsrc", sl), K("lnp")], writes=[K("lnsrc", sl)])
        s.add("dve", lambda e: e.tensor_tensor(out=dst, in0=src, in1=lnp[bi], op=ALU.add), reads=[K("lnsrc", sl), K("lnp")], writes=[dkey])

    def loads(ti):
        sl = ti % 2
        for ec in range(8):
            dma("sp", YT[sl][:, ec, :], D["Gloc"][ec * 128:(ec + 1) * 128, ti * 128:(ti + 1) * 128], [K("Gloc")], [K("YT", sl)], K("YT", sl))
        dma("sp", xt[sl], D["x"][ti * 128:(ti + 1) * 128, :], [("x2s",)], [K("xt", sl)], K("xt", sl))

    def phase1_gen(ti):
        sl = ti % 2
        pb = 4 * sl
        v32 = v32s[sl]
        X1T32 = X1T32s[sl]
        loads(ti)
        yield
        for hh in range(2):
            for ec in range(8):
                s.add("pe", (lambda hh, ec: lambda e: e.matmul(ps[pb + hh][:, :], YT[sl][:, ec, :], Wo[:, ec, hh * 512:(hh + 1) * 512],
                                                               start=(ec == 0), stop=(ec == 7)))(hh, ec),
                      reads=[K("YT", sl), K("wslot", 1)], writes=[("ps", pb + hh)])
            s.add("dve", (lambda hh: lambda e: e.scalar_tensor_tensor(out=v32[:, hh * 512:(hh + 1) * 512], in0=xt[sl][:, hh * 512:(hh + 1) * 512],
                                                                      scalar=float(ALPHA), in1=ps[pb + hh][:, :], op0=ALU.mult, op1=ALU.add))(hh),
                  reads=[K("xt", sl), ("ps", pb + hh)], writes=[K("lnsrc", sl)])
        yield
        ln_stats(v32, sl)
        yield
        ln_apply(v32, xt[sl], 0, 1, K("xt", sl), sl)
        s.add("act", lambda e: e.activation(out=yacc[:, ti, :], in_=xt[sl], func=AF.Copy, scale=float(ALPHA)),
              reads=[K("xt", sl)], writes=[K("yacc", ti)])
        yield
        for q in range(2):
            for kq in range(4):
                dc = q * 4 + kq
                s.add("pe", (lambda q, kq, dc: lambda e: e.transpose(out=ps[pb + 2 + q][:, kq * 128:(kq + 1) * 128], in_=xt[sl][:, dc * 128:(dc + 1) * 128], identity=ident))(q, kq, dc),
                      reads=[K("xt", sl), K("c")], writes=[("ps", pb + 2 + q)])
            yield
            s.add("dve", (lambda q: lambda e: e.tensor_copy(out=X1T32[:, q * 4:(q + 1) * 4, :], in_=ps[pb + 2 + q][:, :].rearrange("p (c t) -> p c t", c=4)))(q),
                  reads=[("ps", pb + 2 + q)], writes=[K("X1T32", sl, q)])
            s.add("act", (lambda q: lambda e: e.activation(out=X1T[:, q * 4:(q + 1) * 4, ti * 128:(ti + 1) * 128], in_=X1T32[:, q * 4:(q + 1) * 4, :], func=AF.Copy))(q),
                  reads=[K("X1T32", sl, q)], writes=[K("X1T", ti)])
        yield
        for dc in range(8):
            s.add("pe", (lambda dc: lambda e: e.matmul(ps[pb][:, 0:16], X1T32[:, dc, :], Wr[:, dc, :], start=(dc == 0), stop=(dc == 7)))(dc),
                  reads=[K("X1T32", sl, dc // 4), K("c")], writes=[("ps", pb)])
        yield
        lg, ex, eq, p2, selm, msk = rt2[sl]
        v1, v2, gs, gsel, gmx, den = rs2[sl]
        R = K("rt", sl)
        s.add("dve", lambda e: e.tensor_tensor(out=lg, in0=ps[pb][:, 0:16], in1=br, op=ALU.add), reads=[("ps", pb), K("c")], writes=[R])
        s.add("dve", lambda e: e.tensor_reduce(out=gmx[:, 0:1], in_=lg, axis=AX.X, op=ALU.max), reads=[R], writes=[R])
        s.add("dve", lambda e: e.tensor_scalar(out=lg, in0=lg, scalar1=gmx[:, 0:1], scalar2=None, op0=ALU.subtract), reads=[R], writes=[R])
        s.add("act", lambda e: e.activation(out=ex, in_=lg, func=AF.Exp), reads=[R], writes=[K("rtb", sl)])
        yield
        R2 = K("rtb", sl)
        ex3 = ex.rearrange("p (g k) -> p g k", g=4)
        eq3 = eq.rearrange("p (g k) -> p g k", g=4)
        p23 = p2.rearrange("p (g k) -> p g k", g=4)
        sel3 = selm.rearrange("p (g k) -> p g k", g=4)
        msk3 = msk.rearrange("p (g k) -> p g k", g=4)
        s.add("dve", lambda e: e.tensor_reduce(out=v1, in_=ex3, axis=AX.X, op=ALU.max), reads=[R2], writes=[R2])
        s.add("dve", lambda e: e.tensor_tensor(out=eq3, in0=ex3, in1=v1.unsqueeze(2).to_broadcast([128, 4, 4]), op=ALU.is_equal), reads=[R2], writes=[R2])
        s.add("dve", lambda e: e.scalar_tensor_tensor(out=p2, in0=eq, scalar=-2.0, in1=ex, op0=ALU.mult, op1=ALU.add), reads=[R2], writes=[R2])
        s.add("dve", lambda e: e.tensor_reduce(out=v2, in_=p23, axis=AX.X, op=ALU.max), reads=[R2], writes=[R2])
        s.add("dve", lambda e: e.tensor_tensor(out=gs, in0=v1, in1=v2, op=ALU.add), reads=[R2], writes=[R2])
        s.add("dve", lambda e: e.tensor_reduce(out=gmx[:, 1:2], in_=gs, axis=AX.X, op=ALU.max), reads=[R2], writes=[R2])
        yield
        s.add("dve", lambda e: e.tensor_scalar(out=gsel, in0=gs, scalar1=gmx[:, 1:2], scalar2=None, op0=ALU.is_equal), reads=[R2], writes=[R2])
        s.add("dve", lambda e: e.tensor_tensor(out=sel3, in0=ex3, in1=v2.unsqueeze(2).to_broadcast([128, 4, 4]), op=ALU.is_ge), reads=[R2], writes=[R2])
        s.add("dve", lambda e: e.tensor_tensor(out=msk3, in0=sel3, in1=gsel.unsqueeze(2).to_broadcast([128, 4, 4]), op=ALU.mult), reads=[R2], writes=[R2])
        s.add("dve", lambda e: e.tensor_tensor(out=v1, in0=gs, in1=gsel, op=ALU.mult), reads=[R2], writes=[R2])
        s.add("dve", lambda e: e.tensor_reduce(out=den[:, 0:1], in_=v1, axis=AX.X, op=ALU.add), reads=[R2], writes=[R2])
        s.add("dve", lambda e: e.reciprocal(out=den[:, 1:2], in_=den[:, 0:1]), reads=[R2], writes=[R2])
        s.add("dve", lambda e: e.tensor_tensor(out=msk, in0=msk, in1=ex, op=ALU.mult), reads=[R2], writes=[R2])
        s.add("dve", lambda e: e.tensor_scalar(out=gates[:, ti, :], in0=msk, scalar1=den[:, 1:2], scalar2=None, op0=ALU.mult),
              reads=[R2], writes=[K("gates", ti)])

    def run_two(gens):
        gens = list(gens)
        active = [None, None]
        nxt = [0]

        def refill(slot):
            for j in range(nxt[0], len(gens)):
                if gens[j] is not None and gens[j][0] % 2 == slot:
                    g = gens[j][1]
                    gens[j] = None
                    return g
            return None
        active[0] = refill(0)
        active[1] = refill(1)
        for _ in range(3):
            if active[0] is not None:
                try:
                    next(active[0])
                except StopIteration:
                    active[0] = refill(0)
        while active[0] is not None or active[1] is not None:
            for slot in (0, 1):
                if active[slot] is None:
                    continue
                try:
                    next(active[slot])
                except StopIteration:
                    active[slot] = refill(slot)

    run_two([(ti, phase1_gen(ti)) for ti in range(NT)])
    dma("sp", lnp[0], D["ln2g"], [], [K("lnp")], K("lnp"))
    dma("sp", lnp[1], D["ln2b"], [], [K("lnp")], K("lnp"))

    outs = []

    def ln2_gen(ti):
        sl = ti % 2
        pb = 4 * sl
        v32 = v32s[sl]
        s.add("dve", lambda e: e.tensor_copy(out=v32, in_=yacc[:, ti, :]), reads=[K("yacc", ti)], writes=[K("lnsrc", sl)])
        ln_stats(v32, sl)
        yield
        ln_apply(v32, xt[sl], 0, 1, K("xt", sl), sl)
        yield
        if last:
            outs.append(dma("sp", D["x2"][ti * 128:(ti + 1) * 128, :], xt[sl], [K("xt", sl)], [], K("x2out")))
        else:
            outs.append(dma("sp", D["x2s"][ti * 128:(ti + 1) * 128, :], xt[sl], [K("xt", sl)], [("x2s",)], K("x2out")))
            for q in range(2):
                for kq in range(4):
                    dc = q * 4 + kq
                    s.add("pe", (lambda q, kq, dc: lambda e: e.transpose(out=ps[6 + q][:, kq * 128:(kq + 1) * 128], in_=xt[sl][:, dc * 128:(dc + 1) * 128], identity=ident))(q, kq, dc),
                          reads=[K("xt", sl), K("c")], writes=[("ps", 6 + q)])
            tq = ti % 4
            for q in range(2):
                s.add("dve", (lambda q: lambda e: e.tensor_copy(out=xstp[:, q * 4:(q + 1) * 4, tq * 128:(tq + 1) * 128], in_=ps[6 + q][:, :].rearrange("p (c t) -> p c t", c=4)))(q),
                      reads=[("ps", 6 + q)], writes=[K("xstp", ti)])

    def emit_ln2_piece(p):
        run_two([(ti, ln2_gen(ti)) for ti in range(NSUB * p, NSUB * p + NSUB)])
        if not last:
            for dc in range(8):
                outs.append(dma("sp", D["X2T"][p][dc * 128:(dc + 1) * 128, :], xstp[:, dc, :], [K("xstp", 4 * p + i) for i in range(4)],
                                [("X2T", p)] + [K("xstp", 4 * p + 4 + i) for i in range(4)], K("x2out")))
            if cc2 is not None:
                cc2(p)

    NU = NE * 2
    hcnt = [0]
    for u in range(NU):
        e_, fh = u // 2, u % 2
        sl = u % 2
        if u + 1 < NU:
            load_unit(u + 1)
        wkey = K("wslot", sl)
        for t4 in range(NT4):
            hs = hcnt[0] % 2
            hcnt[0] += 1
            tok = slice(t4 * TT, (t4 + 1) * TT)
            for fc in range(4):
                gi = 0 + (fc % 2)
                ui = 2 + (fc % 2)
                for dc in range(8):
                    s.add("pe", (lambda sl, dc, fc, gi, tok: lambda e: e.matmul(ps[gi][:, 0:TT], Wg[sl][:, dc, fc * 128:(fc + 1) * 128], X1T[:, dc, tok],
                                                                               start=(dc == 0), stop=(dc == 7)))(sl, dc, fc, gi, tok),
                          reads=[wkey] + [K("X1T", t4 * NSUB + i) for i in range(NSUB)], writes=[("ps", gi)])
                for dc in range(8):
                    s.add("pe", (lambda sl, dc, fc, ui, tok: lambda e: e.matmul(ps[ui][:, 0:TT], Wu[sl][:, dc, fc * 128:(fc + 1) * 128], X1T[:, dc, tok],
                                                                               start=(dc == 0), stop=(dc == 7)))(sl, dc, fc, ui, tok),
                          reads=[wkey] + [K("X1T", t4 * NSUB + i) for i in range(NSUB)], writes=[("ps", ui)])
                si = fc % 2
                s.add("act", (lambda gi, si: lambda e: e.activation(out=sg[si][:, 0:TT], in_=ps[gi][:, 0:TT], func=AF.Silu))(gi, si),
                      reads=[("ps", gi)], writes=[K("sg", si)])
                s.add("dve", (lambda hs, fc, si, ui: lambda e: e.tensor_tensor(out=hT[hs][:, fc, :], in0=sg[si][:, 0:TT], in1=ps[ui][:, 0:TT], op=ALU.mult))(hs, fc, si, ui),
                      reads=[K("sg", si), ("ps", ui)], writes=[K("hT", hs, fc)])
            for sub in range(NSUB):
                ti = t4 * NSUB + sub
                for hh in range(2):
                    di = 4 + ((sub * 2 + hh) % 2)
                    for fc in range(4):
                        s.add("pe", (lambda sl, hs, fc, sub, hh, di: lambda e: e.matmul(ps[di][:, :], hT[hs][:, fc, sub * 128:(sub + 1) * 128], Wd[sl][:, fc, hh * 512:(hh + 1) * 512],
                                                                                         start=(fc == 0), stop=(fc == 3)))(sl, hs, fc, sub, hh, di),
                              reads=[wkey, K("hT", hs, fc)], writes=[("ps", di)])
                    s.add("dve", (lambda ti, hh, di, e_: lambda e: e.scalar_tensor_tensor(out=yacc[:, ti, hh * 512:(hh + 1) * 512], in0=ps[di][:, :], scalar=gates[:, ti, e_:e_ + 1],
                                                                                          in1=yacc[:, ti, hh * 512:(hh + 1) * 512], op0=ALU.mult, op1=ALU.add))(ti, hh, di, e_),
                          reads=[("ps", di), K("gates", ti), K("yacc", ti)], writes=[K("yacc", ti)])
            if u == NU - 1:
                emit_ln2_piece(t4)

    return outs

I32 = mybir.dt.int32
_GROUPS = [[0, 1, 2, 3], [4, 5, 6, 7]]


def _build_fused(S=8192, T=2048, NE=16, depth=2):
    nc = bass.Bass("TRN2", target_bir_lowering=False, num_devices=8)
    D = {}

    def inp(name, shape, dt=F32):
        D[name] = nc.dram_tensor(name, shape, dt, kind="ExternalInput").ap()
    inp("xT", [1024, S])
    inp("xres", [T, 1024])
    inp("qi", [1, 1], I32)
    inp("wA", [depth * 1024, 768])
    inp("gA", [depth * 256, 1])
    inp("biasm", [3, 2, 128, 256])
    C = consts_A()
    for k, v in C.items():
        inp(k, list(v.shape), BF16 if v.dtype == ml_dtypes.bfloat16 else F32)
    inp("wo", [depth * 1024, 1024])
    inp("lnp", [depth * 4 * 128, 1024])
    inp("wr", [1024, 16])
    inp("br", [128, 16])
    inp("ident32", [128, 128])
    for nm in ("wg", "wu", "wd"):
        inp(nm, [depth * NE, 1024, 1024])
    D["out"] = nc.dram_tensor("out", [T, 1024], F32, kind="ExternalOutput").ap()
    yA = [[nc.dram_tensor("yA%d_%d" % (l, p), [256, T], BF16).ap() for p in range(4)] for l in range(depth)]
    Gall = [nc.dram_tensor("Gall%d" % l, [4096, T], BF16).ap() for l in range(depth)]
    Gloc = [nc.dram_tensor("Gloc%d" % l, [1024, T], BF16).ap() for l in range(depth)]
    X2T = [nc.dram_tensor("X2T%d" % p, [1024, 512], BF16).ap() for p in range(4)]
    GX = [nc.dram_tensor("GX%d" % p, [4096, 512], BF16).ap() for p in range(4)]
    x2s = nc.dram_tensor("x2s", [T, 1024], F32).ap()
    with ExitStack() as es:
        arena = es.enter_context(nc.sbuf_tensor("arena", [128, 103 * 1024], BF16))
        qs = es.enter_context(nc.sbuf_tensor("qs", [1, 1], I32))
        reg = es.enter_context(nc.sync.register("qreg"))
        ps = [es.enter_context(nc.psum_tensor("ps%d" % i, [128, 512], F32)) for i in range(8)]
        s = Sched(nc)
        st = {}
        s.add("sp", lambda e: e.dma_start(out=qs[:, :], in_=D["qi"]), writes=[("qs",)], grp=("qs",))

        def ld(e):
            ins = e.reg_load(reg, qs[0:1, 0:1])
            st["reg"] = reg
            return ins
        s.add("sp", ld, reads=[("qs",)], writes=[("qreg",)])
        outs = []
        ccn = [0]
        for l in range(depth):
            last = (l == depth - 1)
            DA = dict(D)
            DA["w"] = D["wA"][l * 1024:(l + 1) * 1024, :]
            DA["g"] = D["gA"][l * 256:(l + 1) * 256, :]
            DA["yA"] = yA[l]
            DA["GX"] = GX

            def cc1(p, l=l):
                ccn[0] += 1
                s.add("pool", lambda e: e.collective_compute("AllGather", ALU.bypass, replica_groups=_GROUPS, ins=[yA[l][p]],
                                                             outs=[Gall[l][p * 1024:(p + 1) * 1024, :]]),
                      reads=[("A%d" % l, "yA", p)], writes=[("B%d" % l, "G", p)], grp=("cc", ccn[0]), inc=1)
            ar = Arena(arena)
            emit_A(nc, s, ar, ps, DA, S, l == 0, "A%d" % l, cc=cc1)
            s.fence()
            DB = dict(D)
            DB["Gall"] = Gall[l]
            DB["Gloc"] = Gloc[l]
            DB["x"] = D["xres"] if l == 0 else x2s
            DB["wo"] = D["wo"][l * 1024:(l + 1) * 1024, :]
            for i, nm in enumerate(("ln1g", "ln1b", "ln2g", "ln2b")):
                DB[nm] = D["lnp"][(l * 4 + i) * 128:(l * 4 + i + 1) * 128, :]
            for nm in ("wg", "wu", "wd"):
                DB[nm] = D[nm][l * NE:(l + 1) * NE]
            DB["x2"] = D["out"]
            DB["x2s"] = x2s
            DB["X2T"] = X2T

            def cc2(p):
                ccn[0] += 1
                s.add("pool", lambda e: e.collective_compute("AllGather", ALU.bypass, replica_groups=_GROUPS, ins=[X2T[p]], outs=[GX[p]]),
                      reads=[("X2T", p)], writes=[("GX", p)], grp=("cc", ccn[0]), inc=1)
            ar = Arena(arena)
            o = emit_B(nc, s, ar, ps, DB, T, "B%d" % l, NE=NE, st=st, last=last, cc2=None if last else cc2)
            if last:
                outs = o
            else:
                s.fence()
        keys = s.emit(final_wait_ops=outs)
        sems = {k: es.enter_context(nc.semaphore("s%d" % i)) for i, k in enumerate(keys)}
        s.run(sems, final_wait_ops=outs)
    return nc, C, len(s.ops), len(keys)


def kernel(x, w_in, g_sb, g_dil, w_out, ln1_g, ln1_b, ln2_g, ln2_b, rel_bias, w_router, b_router, w_gate, w_up, w_down):
    x = np.asarray(x, np.float32)
    B, S, Dm = x.shape
    depth = w_in.shape[0]
    T = 2048
    NE = w_gate.shape[1]
    nc, C, _, _ = _build_fused(S, T, NE, depth)
    f32 = lambda v: np.asarray(v, np.float32)
    rep = lambda v: np.broadcast_to(f32(v)[None, :], (128, v.shape[0]))
    rel_bias = f32(rel_bias)
    perm = np.concatenate([np.concatenate([np.arange(128 * j, 128 * j + 128), 512 + np.arange(128 * j, 128 * j + 128)]) for j in range(4)])
    wo = np.ascontiguousarray(np.concatenate([f32(w_out[l])[perm, :] for l in range(depth)], axis=0))
    lnp = np.ascontiguousarray(np.concatenate([rep(p[l]) for l in range(depth) for p in (ln1_g, ln1_b, ln2_g, ln2_b)], axis=0))
    shared = {"wo": wo, "lnp": lnp, "wr": np.ascontiguousarray(f32(w_router)), "br": np.ascontiguousarray(rep(b_router)),
              "ident32": np.eye(128, dtype=np.float32),
              "wg": f32(w_gate).reshape(depth * NE, 1024, 1024), "wu": f32(w_up).reshape(depth * NE, 1024, 1024),
              "wd": f32(w_down).reshape(depth * NE, 1024, 1024)}
    shared.update(C)
    xTs = [np.ascontiguousarray(x[b].T) for b in range(B)]
    in_maps = []
    for c in range(8):
        b, j = c // 4, c % 4
        wA = []
        gA = []
        for l in range(depth):
            wl = f32(w_in[l])
            wA.append(np.concatenate([wl[:, o + 128 * j:o + 128 * j + 128] for o in (0, 512, 1536, 2048, 1024, 2560)], axis=1))
            gA.append(np.concatenate([f32(g_sb[l])[128 * j:128 * j + 128], f32(g_dil[l])[128 * j:128 * j + 128]])[:, None])
        m = {"xT": xTs[b], "xres": np.ascontiguousarray(x[b, j * T:(j + 1) * T, :]), "qi": np.array([[1024 * j]], np.int32),
             "wA": np.ascontiguousarray(np.concatenate(wA, axis=0)), "gA": np.ascontiguousarray(np.concatenate(gA, axis=0)),
             "biasm": dil_bias_tables(rel_bias[:, 2 * j:2 * j + 2])}
        m.update(shared)
        in_maps.append(m)
    res = run_bass_kernel_spmd(nc, in_maps, core_ids=list(range(8)))
    out = np.zeros((B, S, Dm), np.float32)
    for c in range(8):
        b, j = c // 4, c % 4
        out[b, j * T:(j + 1) * T, :] = np.asarray(res.results[c]["out"], np.float32)
    return out
```

```python
from contextlib import ExitStack
from concourse.bass_utils import run_bass_kernel_spmd
import numpy as np
import concourse.bass as bass
import concourse.mybir as mybir

F32 = mybir.dt.float32
BF16 = mybir.dt.bfloat16
AF = mybir.ActivationFunctionType
ALU = mybir.AluOpType

COMPUTE = ("pe", "act", "dve", "pool")


class Sched:
    def __init__(self, nc):
        self.nc = nc
        self.ops = []
        self.last_writer = {}
        self.readers = {}
        self.dma_groups = {}
        self.fence_deps = set()
        self.fence_pending = set()

    def fence(self):
        last = {}
        for i, op in enumerate(self.ops):
            k = ("dma", op["grp"]) if op["dma"] else ("eng", op["eng"])
            last[k] = i
        self.fence_deps = set(last.values())
        self.fence_pending = {"pe", "act", "dve", "pool", "sp"}

    def add(self, eng, fn, reads=(), writes=(), grp=None, inc=16):
        idx = len(self.ops)
        deps = set()
        if eng in self.fence_pending:
            deps |= self.fence_deps
            self.fence_pending.discard(eng)
        for b in reads:
            w = self.last_writer.get(b)
            if w is not None:
                deps.add(w)
        for b in writes:
            w = self.last_writer.get(b)
            if w is not None:
                deps.add(w)
            for r in self.readers.get(b, ()):
                deps.add(r)
        deps.discard(idx)
        is_dma = grp is not None
        if eng == "pe":
            deps = {d for d in deps if not (self.ops[d]["eng"] == "pe" and not self.ops[d]["dma"])}
        op = dict(eng=eng, fn=fn, deps=deps, dma=is_dma, grp=grp, needed=False, inc=(inc if is_dma else 1))
        self.ops.append(op)
        for b in reads:
            self.readers.setdefault(b, []).append(idx)
        for b in writes:
            self.last_writer[b] = idx
            self.readers[b] = []
        return idx

    def emit(self, final_wait_ops=()):
        nc = self.nc
        ops = self.ops
        for op in ops:
            for d in op["deps"]:
                ops[d]["needed"] = True
        for d in final_wait_ops:
            ops[d]["needed"] = True
        sem_names = []
        counters = {}
        for i, op in enumerate(ops):
            if not op["needed"]:
                continue
            key = ("dma", op["grp"]) if op["dma"] else ("eng", op["eng"])
            if key not in counters:
                counters[key] = 0
                sem_names.append(key)
            counters[key] += op["inc"]
            op["sem"] = key
            op["val"] = counters[key]
        self.sem_keys = sem_names
        return sem_names

    def run(self, sems, final_wait_ops=()):
        nc = self.nc
        ops = self.ops
        per_eng = {}
        for i, op in enumerate(ops):
            per_eng.setdefault(op["eng"], []).append(i)

        def body(engname, eng):
            known = {}
            for i in per_eng.get(engname, []):
                op = ops[i]
                need = {}
                for d in op["deps"]:
                    p = ops[d]
                    k = p["sem"]
                    need[k] = max(need.get(k, 0), p["val"])
                for k, v in need.items():
                    if known.get(k, 0) >= v:
                        continue
                    eng.wait_ge(sems[k], v)
                    known[k] = v
                ins = op["fn"](eng)
                if op["needed"]:
                    ins.then_inc(sems[op["sem"]], op["inc"])
            if engname == "sp":
                fin = {}
                for d in final_wait_ops:
                    p = ops[d]
                    fin[p["sem"]] = max(fin.get(p["sem"], 0), p["val"])
                for k, v in fin.items():
                    if known.get(k, 0) < v:
                        eng.wait_ge(sems[k], v)
                        known[k] = v

        with nc.Block() as block:
            @block.sync
            def _(e):
                body("sp", e)

            @block.tensor
            def _(e):
                body("pe", e)

            @block.scalar
            def _(e):
                body("act", e)

            @block.vector
            def _(e):
                body("dve", e)

            @block.gpsimd
            def _(e):
                body("pool", e)


import math
import numpy as np
import ml_dtypes
import concourse.bass as bass
import concourse.mybir as mybir

NEG = -30000.0
SKIP = ['dveonly']
SC = 2048


def t5_bucket_np(dist):
    max_exact = 16
    d = np.maximum(dist, 0)
    large = max_exact + (np.log(np.maximum(d, 1).astype(np.float32) / np.float32(max_exact))
                         / np.float32(math.log(2048 / max_exact)) * np.float32(32 - max_exact)).astype(np.int32)
    large = np.minimum(large, 31)
    return np.where(d < max_exact, d, large)


def consts_A():
    j = np.arange(128)[:, None]
    s = np.arange(128)[None, :]
    negtri = np.where(j >= s, -1.0, 0.0).astype(ml_dtypes.bfloat16)
    negones = np.full((128, 128), -1.0, dtype=ml_dtypes.bfloat16)
    ident = np.eye(128, dtype=ml_dtypes.bfloat16)
    t = np.arange(512)[None, :]
    sbmask = np.stack([np.where(128 * a + j >= t, NEG, 0.0) for a in range(4)], 0).astype(ml_dtypes.bfloat16)
    onesblk = np.zeros((128, 128), np.float32)
    onesblk[:64, :64] = 1.0
    onesblk[64:, 64:] = 1.0
    sel65 = np.zeros((65, 64), np.float32)
    sel65[64, :] = 1.0
    ones65 = np.zeros((65, 64), np.float32)
    ones65[:64, :] = 1.0
    return dict(negtri=negtri, negones=negones, ident=ident, sbmask=sbmask, onesblk=onesblk, sel65=sel65, ones65=ones65)


def dil_bias_tables(rel_bias_heads):
    kj = np.arange(128)[:, None]
    qi = np.arange(128)[None, :]
    out = np.zeros((3, 2, 128, 256), np.float32)
    for ri, r in enumerate((1, 4, 16)):
        steps_cur = qi - kj
        steps_prev = qi - kj + 128
        b_cur = t5_bucket_np(np.maximum(steps_cur, 0) * r)
        b_prev = t5_bucket_np(np.maximum(steps_prev, 0) * r)
        for h in range(2):
            cur = np.where(steps_cur >= 0, rel_bias_heads[b_cur, h], np.float32(NEG))
            prev = np.where(steps_prev <= 128, rel_bias_heads[b_prev, h], np.float32(NEG))
            out[ri, h, :, :128] = prev
            out[ri, h, :, 128:] = cur
    return out


class Arena:
    def __init__(self, ap_bf16):
        self.ap = ap_bf16
        self.off = 0
        self.total = ap_bf16.shape[1]

    def take(self, nelem, dtype=BF16):
        nb = nelem * (4 if dtype == F32 else 2)
        nb = (nb + 63) // 64 * 64
        n16 = nb // 2
        assert self.off + n16 <= self.total, ("arena overflow", self.off, n16, self.total)
        v = self.ap[:, self.off:self.off + n16]
        self.off += n16
        if dtype == F32:
            return v.bitcast(F32)[:, :nelem]
        return v[:, :nelem]


def emit_A(nc, s, ar, ps, D, S, src_is_f32, lname, stop=99, cc=None):
    NSC = S // SC
    NB = S // 128
    L = lname
    K = lambda *a: (L,) + a

    Wb = ar.take(8 * 768).rearrange("p (f c) -> p f c", f=8)
    XT = ar.take(8 * SC).rearrange("p (f t) -> p f t", f=8)
    QTs = ar.take(SC)
    QTsB = ar.take(SC)
    QTd = ar.take(SC)
    KTs = ar.take(S)
    KTd = ar.take(S)
    Vs = ar.take(NB * 128).rearrange("p (b c) -> p b c", c=128)
    NW1 = NB
    Vd1 = ar.take(NW1 * 132).rearrange("p (b h c) -> p b h c", h=2, c=66)
    Vd4 = ar.take(32 * 132).rearrange("p (b h c) -> p b h c", h=2, c=66)
    Vd16 = ar.take(32 * 132).rearrange("p (b h c) -> p b h c", h=2, c=66)
    negtri = ar.take(128)
    negones = ar.take(128)
    ident = ar.take(128)
    sbmask = ar.take(4 * 512).rearrange("p (a t) -> p a t", a=4)
    onesblk = ar.take(128, F32)
    sel65 = ar.take(64, F32)
    ones65 = ar.take(64, F32)
    gvec = ar.take(4, F32)
    biasm16 = ar.take(6 * 256).rearrange("p (r h c) -> p r h c", r=3, h=2)
    E32 = [ar.take(512, F32) for _ in range(2)]
    Lp16 = [ar.take(512) for _ in range(4)]
    LA32 = [ar.take(512, F32) for _ in range(2)]
    A16 = [ar.take(512) for _ in range(4)]
    ncar = [ar.take(512, F32) for _ in range(2)]
    P16 = [ar.take(512) for _ in range(6)]
    acc = [ar.take(SC, F32) for _ in range(2)]
    o32 = ar.take(512, F32)
    sq32 = ar.take(512, F32)
    r32 = ar.take(512, F32)
    Yb = [ar.take(512) for _ in range(2)]

    def dma(eng, out, in_, reads, writes, grp):
        return s.add(eng, lambda e: e.dma_start(out=out, in_=in_), reads=reads, writes=writes, grp=grp)

    for fc in range(8):
        dma("pool", Wb[:, fc, :], D["w"][fc * 128:(fc + 1) * 128, :], [], [K("W")], K("W"))
    dma("sp", negtri, D["negtri"], [], [K("c")], K("c"))
    dma("sp", negones, D["negones"], [], [K("c")], K("c"))
    dma("sp", ident, D["ident"], [], [K("c")], K("c"))
    for a in range(4):
        dma("sp", sbmask[:, a, :], D["sbmask"][a], [], [K("c")], K("c"))
    dma("sp", onesblk, D["onesblk"], [], [K("c")], K("c"))
    dma("sp", sel65[0:65, :], D["sel65"], [], [K("c")], K("c"))
    dma("sp", ones65[0:65, :], D["ones65"], [], [K("c")], K("c"))
    dma("sp", gvec[:, 0:1], D["g"][0:128, :], [], [K("c")], K("c"))
    dma("sp", gvec[0:64, 1:2], D["g"][128:192, :], [], [K("c")], K("c"))
    dma("sp", gvec[0:64, 2:3], D["g"][192:256, :], [], [K("c")], K("c"))
    for ri in range(3):
        for h in range(2):
            dma("sp", acc[0][:, (ri * 2 + h) * 256:(ri * 2 + h + 1) * 256], D["biasm"][ri, h], [], [K("acc", 0)], K("bm"))
    s.add("act", lambda e: e.activation(out=biasm16.rearrange("p r h c -> p (r h c)"), in_=acc[0][:, 0:1536], func=AF.Exp), reads=[K("acc", 0)], writes=[K("ebias")])
    for vb in (Vd1, Vd4, Vd16):
        s.add("dve", (lambda vb: lambda e: e.memset(vb[:, :, :, 64:65], 1.0))(vb), writes=[K("vones")])

    psX = [ps[6], ps[7]]
    def dummy_out():
        return [dma("sp", D["yT"][0:128, 0:512], QTs[:, 0:512], [K("c"), K("W"), K("XT"), K("vones"), K("QTs"), K("QTd"), K("Vd4", 0), K("Vd16", 0), K("Vs", 0)], [], K("yout"))]
    if stop == 0:
        return dummy_out()
    xcnt = [0]

    def nextX():
        i = xcnt[0] % 2
        xcnt[0] += 1
        return i

    evac_cnt = [0]

    def evac(out, in_, reads, writes, scale=None):
        evac_cnt[0] += 1
        if evac_cnt[0] % 2 == 0 or 'dveonly' in SKIP:
            if scale is None:
                return s.add("dve", lambda e: e.tensor_copy(out=out, in_=in_), reads=reads, writes=writes)
            return s.add("dve", lambda e: e.tensor_scalar(out=out, in0=in_, scalar1=float(scale), scalar2=None, op0=ALU.mult),
                         reads=reads, writes=writes)
        sc = 1.0 if scale is None else float(scale)
        return s.add("act", lambda e: e.activation(out=out, in_=in_, func=AF.Copy, scale=sc), reads=reads, writes=writes)

    out_dmas = []
    ycnt = [0]

    def ss(start, n, step):
        return slice(start, start + (n - 1) * step + 1, step)

    QTs2 = [QTs, QTsB]

    def make(sc_i):
        t0 = sc_i * SC

        def load_xt(sci):
            for fc in range(8):
                if src_is_f32:
                    dma("pool", XT[:, fc, :], D["xT"][fc * 128:(fc + 1) * 128, sci * SC:(sci + 1) * SC], [], [K("XT")], K("XT"))
                else:
                    for p in range(4):
                        dma("sp", XT[:, fc, p * 512:(p + 1) * 512], D["GX"][p][sci * 1024 + fc * 128:sci * 1024 + (fc + 1) * 128, :], [("GX", p)], [K("XT")], K("XT"))

        def prep():
            if sc_i == 0:
                load_xt(0)
            for c in range(4):
                tl = c * 512
                for kind, col0, dst, scale in (("qs", 0, QTs2[sc_i % 2][:, tl:tl + 512], 0.125), ("ks", 128, KTs[:, t0 + tl:t0 + tl + 512], None),
                                               ("qd", 256, QTd[:, tl:tl + 512], 0.125), ("kd", 384, KTd[:, t0 + tl:t0 + tl + 512], None)):
                    yield
                    xi = nextX()
                    for fc in range(8):
                        s.add("pe", (lambda xi, fc, col0, tl: lambda e: e.matmul(psX[xi][:, :], Wb[:, fc, col0:col0 + 128], XT[:, fc, tl:tl + 512],
                                                                                 start=(fc == 0), stop=(fc == 7)))(xi, fc, col0, tl),
                              reads=[K("W"), K("XT")], writes=[("ps", 6 + xi)])
                    wkey = {"qs": K("QTs", sc_i % 2), "ks": K("KTs", sc_i), "qd": K("QTd"), "kd": K("KTd", sc_i)}[kind]
                    evac(dst, psX[xi][:, :], [("ps", 6 + xi)], [wkey], scale)
                for sub in range(4 if 'vnat' not in SKIP else 0):
                    tt = tl + sub * 128
                    blk = (t0 + tt) // 128
                    yield
                    xi = nextX()
                    for fc in range(8):
                        s.add("pe", (lambda xi, fc, tt: lambda e: e.matmul(psX[xi][:, 0:256], XT[:, fc, tt:tt + 128], Wb[:, fc, 512:768],
                                                                           start=(fc == 0), stop=(fc == 7)))(xi, fc, tt),
                              reads=[K("W"), K("XT")], writes=[("ps", 6 + xi)])
                    if 'evs' not in SKIP: evac(Vs[:, blk, :], psX[xi][:, 0:128], [("ps", 6 + xi)], [K("Vs", blk)])
                    for hh in range(2 if 'evd1' not in SKIP else 0):
                        evac(Vd1[:, blk, hh, 0:64], psX[xi][:, 128 + 64 * hh:192 + 64 * hh], [("ps", 6 + xi)], [K("Vd1", blk)])
                n4 = 4 * sc_i + c
                yield
                xi = nextX()
                for cls in range(4 if 'v4' not in SKIP else 0):
                    for fc in range(8):
                        s.add("pe", (lambda xi, fc, cls, tl: lambda e: e.matmul(psX[xi][:, cls * 128:(cls + 1) * 128],
                                                                                XT[:, fc, ss(tl + cls, 128, 4)], Wb[:, fc, 640:768],
                                                                                start=(fc == 0), stop=(fc == 7)))(xi, fc, cls, tl),
                              reads=[K("W"), K("XT")], writes=[("ps", 6 + xi)])
                slot0 = (n4 % 8) * 4
                for kk_ in range(4 if 'v4' not in SKIP else 0):
                    for hh in range(2):
                        evac(Vd4[:, slot0 + kk_, hh, 0:64], psX[xi][:, kk_ * 128 + 64 * hh:kk_ * 128 + 64 * hh + 64], [("ps", 6 + xi)], [K("Vd4", n4 % 8)])
            for c4 in range(4 if 'v16' not in SKIP else 0):
                yield
                xi = nextX()
                for k in range(4):
                    cls = c4 * 4 + k
                    for fc in range(8):
                        s.add("pe", (lambda xi, fc, cls, k: lambda e: e.matmul(psX[xi][:, k * 128:(k + 1) * 128],
                                                                               XT[:, fc, ss(cls, 128, 16)], Wb[:, fc, 640:768],
                                                                               start=(fc == 0), stop=(fc == 7)))(xi, fc, cls, k),
                              reads=[K("W"), K("XT")], writes=[("ps", 6 + xi)])
                slot0 = (sc_i % 2) * 16 + c4 * 4
                for kk_ in range(4 if 'v16' not in SKIP else 0):
                    for hh in range(2):
                        evac(Vd16[:, slot0 + kk_, hh, 0:64], psX[xi][:, kk_ * 128 + 64 * hh:kk_ * 128 + 64 * hh + 64], [("ps", 6 + xi)], [K("Vd16", sc_i % 2)])

            yield
            if sc_i + 1 < NSC:
                load_xt(sc_i + 1)
            yield
            groups = []
            for h in range(2):
                for ri, r in enumerate((1, 4, 16)):
                    nblk_sc = SC // (128 * r)
                    for c in range(r):
                        n0 = sc_i * nblk_sc
                        for g0 in range(n0, n0 + nblk_sc, 4):
                            blks = list(range(g0, min(g0 + 4, n0 + nblk_sc)))
                            groups.append(dict(h=h, ri=ri, r=r, c=c, g0=g0, blks=blks, gi=len(groups)))
            SBANKS = [(3, 6), (3, 6), (3, 6)]

            def d_s1(g):
                h, ri, r, c, blks, gi = g["h"], g["ri"], g["r"], g["c"], g["blks"], g["gi"]
                hp = slice(64 * h, 64 * h + 64)
                units = []
                for n in blks:
                    units += [(n - 1, n), (n, n)]
                g["uinfo"] = []
                g["tls"] = []
                for t_i in range(0, len(units), 4):
                    tu = units[t_i:t_i + 4]
                    bank = SBANKS[gi % 3][t_i // 4]
                    pslot = (gi % 3) * 2 + t_i // 4
                    lo = None
                    for u, (kb, n) in enumerate(tu):
                        co = u * 128
                        g["uinfo"].append((pslot, co, kb))
                        if kb < 0:
                            continue
                        if lo is None:
                            lo = co
                        qstart = c + r * 128 * n - t0
                        kstart = c + r * 128 * kb
                        role = u % 2
                        s.add("pe", (lambda bank, co, kstart, qstart, r, hp: lambda e: e.matmul(
                            ps[bank][:, co:co + 128], KTd[hp, ss(kstart, 128, r)], QTd[hp, ss(qstart, 128, r)],
                            start=True, stop=True))(bank, co, kstart, qstart, r, hp),
                            reads=[K("KTd", kstart // SC), K("QTd")], writes=[("ps", bank)])
                    g["tls"].append((bank, pslot, lo, len(tu) * 128))

            def d_s2(g):
                for (bank, pslot, lo, hi) in g["tls"]:
                    s.add("act", (lambda bank, pslot, lo, hi: lambda e: e.activation(out=P16[pslot][:, lo:hi], in_=ps[bank][:, lo:hi], func=AF.Exp))(bank, pslot, lo, hi),
                          reads=[("ps", bank)], writes=[K("P", pslot)])
                    for half in range(0, hi, 256):
                        a0 = max(half, lo)
                        a1 = half + 256
                        bo = a0 - half
                        s.add("pool", (lambda pslot, a0, a1, bo, ri, h: lambda e: e.tensor_tensor(out=P16[pslot][:, a0:a1], in0=P16[pslot][:, a0:a1], in1=biasm16[:, ri, h, bo:256], op=ALU.mult))(
                            pslot, a0, a1, bo, g["ri"], g["h"]),
                            reads=[K("P", pslot), K("ebias")], writes=[K("P", pslot)])

            def d_s3(g):
                h, r, c, blks, gi = g["h"], g["r"], g["c"], g["blks"], g["gi"]
                ob = 7
                vkey = {1: "Vd1", 4: "Vd4", 16: "Vd16"}[r]
                for bi, n in enumerate(blks):
                    first = True
                    for uu in (2 * bi, 2 * bi + 1):
                        pslot, co, kb = g["uinfo"][uu]
                        if kb < 0:
                            continue
                        if r == 1:
                            vap = Vd1[:, kb, h, 0:65]
                        elif r == 4:
                            vap = Vd4[:, (kb % 8) * 4 + c, h, 0:65]
                        else:
                            vap = Vd16[:, (kb % 2) * 16 + c, h, 0:65]
                        last = (uu == 2 * bi + 1)
                        s.add("pe", (lambda ob, bi, vap, pslot, co, first, last: lambda e: e.matmul(
                            ps[ob][0:65, bi * 128:(bi + 1) * 128], vap, P16[pslot][:, co:co + 128],
                            start=first, stop=last))(ob, bi, vap, pslot, co, first, last),
                            reads=[K("P", pslot), K(vkey, kb if r == 1 else (kb % 8 if r == 4 else kb % 2)), K("vones")], writes=[("ps", ob)])
                        first = False

            def d_s4(g):
                h, r, c, blks, gi, g0 = g["h"], g["r"], g["c"], g["blks"], g["gi"], g["g0"]
                ob = 7
                nb_ = len(blks)
                col = c + r * (128 * g0) - t0
                if r == 1:
                    s.add("dve", (lambda h, col, nb_, ob: lambda e: e.tensor_copy(out=acc[h][0:65, col:col + 128 * nb_], in_=ps[ob][0:65, 0:128 * nb_]))(h, col, nb_, ob),
                          reads=[("ps", ob)], writes=[K("acc", h)])
                else:
                    s.add("dve", (lambda h, col, nb_, r, ob: lambda e: e.tensor_tensor(
                        out=acc[h][0:65, ss(col, 128 * nb_, r)], in0=ps[ob][0:65, 0:128 * nb_], in1=acc[h][0:65, ss(col, 128 * nb_, r)], op=ALU.add))(h, col, nb_, r, ob),
                        reads=[("ps", ob), K("acc", h)], writes=[K("acc", h)])

            for g in groups:
                d_s1(g)
                yield
                d_s2(g)
                yield
                d_s3(g)
                yield
                d_s4(g)
                yield
            for h in range(2):
                for tq in range(4):
                    yield
                    cs = slice(tq * 512, tq * 512 + 512)
                    xi = nextX()
                    s.add("pe", (lambda xi, h, cs: lambda e: e.matmul(psX[xi][0:64, :], sel65[0:65, :], acc[h][0:65, cs], start=True, stop=True))(xi, h, cs),
                          reads=[K("acc", h), K("c")], writes=[("ps", 6 + xi)])
                    s.add("dve", (lambda xi: lambda e: e.reciprocal(out=r32[0:64, :], in_=psX[xi][0:64, :]))(xi), reads=[("ps", 6 + xi)], writes=[K("r32")])
                    s.add("dve", (lambda h, cs: lambda e: e.tensor_tensor(out=o32[0:64, :], in0=acc[h][0:64, cs], in1=r32[0:64, :], op=ALU.mult))(h, cs),
                          reads=[K("acc", h), K("r32")], writes=[K("o32")])
                    s.add("act", lambda e: e.activation(out=sq32[0:64, :], in_=o32[0:64, :], func=AF.Square), reads=[K("o32")], writes=[K("sq32")])
                    xi2 = nextX()
                    s.add("pe", (lambda xi2: lambda e: e.matmul(psX[xi2][0:64, :], ones65[0:64, :], sq32[0:64, :], start=True, stop=True))(xi2),
                          reads=[K("sq32"), K("c")], writes=[("ps", 6 + xi2)])
                    s.add("act", (lambda xi2: lambda e: e.activation(out=r32[0:64, :], in_=psX[xi2][0:64, :], func=AF.Sqrt, scale=1.0 / 64, bias=1e-6))(xi2),
                          reads=[("ps", 6 + xi2)], writes=[K("r32")])
                    s.add("dve", lambda e: e.reciprocal(out=sq32[0:64, :], in_=r32[0:64, :]), reads=[K("r32")], writes=[K("sq32")])
                    yi = ycnt[0] % 2
                    ycnt[0] += 1
                    s.add("dve", (lambda yi, h: lambda e: e.scalar_tensor_tensor(out=Yb[yi][0:64, :], in0=o32[0:64, :], scalar=gvec[0:64, 1 + h:2 + h],
                                                                                  in1=sq32[0:64, :], op0=ALU.mult, op1=ALU.mult))(yi, h),
                          reads=[K("o32"), K("sq32"), K("c")], writes=[K("Yb", yi)])
                    od = dma("sp", D["yA"][sc_i][128 + 64 * h:128 + 64 * h + 64, tq * 512:tq * 512 + 512], Yb[yi][0:64, :], [K("Yb", yi)], [K("yA", sc_i)], K("yout"))
                    out_dmas.append(od)


        def run_sb(drip):
            jobs = []
            for qt in range(4 * sc_i, 4 * sc_i + 4):
                kbl = list(range(4 * qt + 3, -1, -1))
                for ki, kb in enumerate(kbl):
                    for h in range(2):
                        jobs.append(dict(qt=qt, kb=kb, h=h, first=(ki == 0), last=(ki == len(kbl) - 1), idx=len(jobs)))

            def stage1(j):
                qt, kb, h, i = j["qt"], j["kb"], j["h"], j["idx"]
                hp = slice(64 * h, 64 * h + 64)
                a = kb - 4 * qt
                bb, se, sl = i % 3, i % 2, i % 4
                ql = (qt - 4 * sc_i) * 512
                diag = a >= 0
                kk = K("KTs", (kb * 128) // SC)
                s.add("pe", lambda e: e.matmul(ps[bb][:, :], KTs[hp, kb * 128:(kb + 1) * 128], QTs2[sc_i % 2][hp, ql:ql + 512], start=True, stop=False),
                      reads=[kk, K("QTs", sc_i % 2)], writes=[("ps", bb)])
                if diag:
                    s.add("pe", lambda e: e.matmul(ps[bb][:, :], ident, sbmask[:, a, :], start=False, stop=False), reads=[K("c")], writes=[("ps", bb)])
                s.add("act", lambda e: e.activation(out=E32[se], in_=ps[bb][:, :], func=AF.Exp), reads=[("ps", bb)], writes=[K("E", se)])
                s.add("act", lambda e: e.activation(out=Lp16[sl], in_=E32[se], func=AF.Ln, bias=1.0), reads=[K("E", se)], writes=[K("Lp", sl)])

            def stage2(j):
                qt, kb, h, i = j["qt"], j["kb"], j["h"], j["idx"]
                bb, sl, sla, sa16 = i % 3, i % 4, i % 2, i % 4
                cb = 5
                s.add("pe", lambda e: e.matmul(ps[bb][:, :], negtri, Lp16[sl], start=False, stop=True), reads=[K("Lp", sl), K("c")], writes=[("ps", bb)])
                if not j["last"]:
                    s.add("pe", lambda e: e.matmul(ps[cb][:, :], negones, Lp16[sl], start=True, stop=True), reads=[K("Lp", sl), K("c")], writes=[("ps", cb)])
                if j["first"]:
                    s.add("act", lambda e: e.activation(out=A16[sa16], in_=ps[bb][:, :], func=AF.Exp), reads=[("ps", bb)], writes=[K("A", sa16)])
                    if not j["last"]:
                        s.add("dve", lambda e: e.tensor_copy(out=ncar[h], in_=ps[cb][:, :]), reads=[("ps", cb)], writes=[K("ncar", h)])
                else:
                    s.add("dve", lambda e: e.tensor_tensor(out=LA32[sla], in0=ps[bb][:, :], in1=ncar[h], op=ALU.add),
                          reads=[("ps", bb), K("ncar", h)], writes=[K("LA", sla)])
                    if not j["last"]:
                        s.add("dve", lambda e: e.tensor_tensor(out=ncar[h], in0=ps[cb][:, :], in1=ncar[h], op=ALU.add),
                              reads=[("ps", cb), K("ncar", h)], writes=[K("ncar", h)])
                    s.add("act", lambda e: e.activation(out=A16[sa16], in_=LA32[sla], func=AF.Exp), reads=[K("LA", sla)], writes=[K("A", sa16)])

            def stage3(j):
                qt, kb, h, i = j["qt"], j["kb"], j["h"], j["idx"]
                sa16 = i % 4
                s.add("pe", lambda e: e.matmul(ps[4][64 * h:64 * h + 64, :], Vs[:, kb, 64 * h:64 * h + 64], A16[sa16],
                                               start=j["first"], stop=j["last"]),
                      reads=[K("A", sa16), K("Vs", kb)], writes=[("ps", 4)])
                if j["last"] and h == 1:
                    finalize_sb(qt)

            def finalize_sb(qt):
                s.add("dve", lambda e: e.tensor_copy(out=o32, in_=ps[4][:, :]), reads=[("ps", 4)], writes=[K("o32")])
                s.add("act", lambda e: e.activation(out=sq32, in_=o32, func=AF.Square), reads=[K("o32")], writes=[K("sq32")])
                s.add("pe", lambda e: e.matmul(ps[5][:, :], onesblk, sq32, start=True, stop=True), reads=[K("sq32"), K("c")], writes=[("ps", 5)])
                s.add("act", lambda e: e.activation(out=r32, in_=ps[5][:, :], func=AF.Sqrt, scale=1.0 / 64, bias=1e-6), reads=[("ps", 5)], writes=[K("r32")])
                s.add("dve", lambda e: e.reciprocal(out=sq32, in_=r32), reads=[K("r32")], writes=[K("sq32")])
                yi = ycnt[0] % 2
                ycnt[0] += 1
                s.add("dve", lambda e: e.scalar_tensor_tensor(out=Yb[yi], in0=o32, scalar=gvec[:, 0:1], in1=sq32, op0=ALU.mult, op1=ALU.mult),
                      reads=[K("o32"), K("sq32"), K("c")], writes=[K("Yb", yi)])
                od = dma("sp", D["yA"][sc_i][0:128, (qt % 4) * 512:(qt % 4) * 512 + 512], Yb[yi], [K("Yb", yi)], [K("yA", sc_i)], K("yout"))
                out_dmas.append(od)

            n = len(jobs)
            for step in range(n + 2):
                if step < n:
                    stage1(jobs[step])
                if 0 <= step - 1 < n:
                    stage2(jobs[step - 1])
                if 0 <= step - 2 < n:
                    stage3(jobs[step - 2])
                drip(n)
        return prep, run_sb

    PREP_STEPS = 260
    mk = [make(i) for i in range(NSC)]
    g0 = mk[0][0]()
    for _ in g0:
        pass
    for sc_i in range(NSC):
        nxt = mk[sc_i + 1][0]() if sc_i + 1 < NSC else None
        state = [nxt, 0.0]

        def drip(njobs, state=state):
            if state[0] is None:
                return
            state[1] += PREP_STEPS / float(njobs)
            while state[1] >= 1.0 and state[0] is not None:
                state[1] -= 1.0
                try:
                    next(state[0])
                except StopIteration:
                    state[0] = None
        mk[sc_i][1](drip)
        if nxt is not None:
            for _ in nxt:
                pass
        if cc is not None:
            cc(sc_i)
    return out_dmas


import numpy as np
import ml_dtypes
import concourse.bass as bass
import concourse.mybir as mybir

ALPHA = (2.0 * 2) ** 0.25
LN_EPS = 1e-5
AX = mybir.AxisListType


def emit_B(nc, s, ar, ps, D, T, lname, NE=16, st=None, last=True, cc2=None):
    L = lname
    K = lambda *a: (L,) + a
    NT = T // 128
    TT = min(512, T)
    NT4 = T // TT
    NSUB = TT // 128

    yacc = ar.take(NT * 1024, F32).rearrange("p (t d) -> p t d", t=NT)
    X1T = ar.take(8 * T).rearrange("p (c t) -> p c t", c=8)
    wslot = [ar.take(3 * 4096) for _ in range(2)]
    Wg = [w[:, 0:4096].rearrange("p (c f) -> p c f", c=8) for w in wslot]
    Wu = [w[:, 4096:8192].rearrange("p (c f) -> p c f", c=8) for w in wslot]
    Wd = [w[:, 8192:12288].rearrange("p (c f) -> p c f", c=4) for w in wslot]
    Wo = wslot[1][:, 0:8192].rearrange("p (c f) -> p c f", c=8)
    YT = [ar.take(8 * 128).rearrange("p (c t) -> p c t", c=8) for _ in range(2)]
    xt = [ar.take(1024, F32) for _ in range(2)]
    v32s = [ar.take(1024, F32) for _ in range(2)]
    lnp = [ar.take(1024, F32) for _ in range(2)]
    X1T32s = [ar.take(8 * 128, F32).rearrange("p (c t) -> p c t", c=8) for _ in range(2)]
    hT = [ar.take(4 * TT).rearrange("p (c t) -> p c t", c=4) for _ in range(2)]
    sg = [ar.take(TT, F32) for _ in range(2)]
    gates = ar.take(NT * 16, F32).rearrange("p (t e) -> p t e", t=NT)
    Wr = ar.take(8 * 16, F32).rearrange("p (c e) -> p c e", c=8)
    br = ar.take(16, F32)
    ident = ar.take(128, F32)
    stats2 = [ar.take(16, F32) for _ in range(2)]
    mv2 = [ar.take(4, F32) for _ in range(2)]
    rt2 = [[ar.take(16, F32) for _ in range(6)] for _ in range(2)]
    rs2 = [[ar.take(4, F32) for _ in range(6)] for _ in range(2)]
    xstp = ar.take(8 * 512).rearrange("p (c t) -> p c t", c=8)

    def dma(eng, out, in_, reads, writes, grp):
        return s.add(eng, lambda e: e.dma_start(out=out, in_=in_), reads=reads, writes=writes, grp=grp)

    for i, nm in enumerate(("ln1g", "ln1b")):
        dma("sp", lnp[i], D[nm], [], [K("lnp")], K("lnp"))
    for dc in range(8):
        dma("sp", Wr[:, dc, :], D["wr"][dc * 128:(dc + 1) * 128, :], [], [K("c")], K("c"))
    dma("sp", br, D["br"], [], [K("c")], K("c"))
    dma("sp", ident, D["ident32"], [], [K("c")], K("c"))
    for ec in range(8):
        dma("pool", Wo[:, ec, :], D["wo"][ec * 128:(ec + 1) * 128, :], [], [K("wslot", 1)], K("wslot", 1))

    def load_unit(u):
        e, fh = u // 2, u % 2
        sl = u % 2
        key = K("wslot", sl)
        for dc in range(8):
            dma("pool", Wg[sl][:, dc, :], D["wg"][e, dc * 128:(dc + 1) * 128, fh * 512:(fh + 1) * 512], [], [key], key)
        for dc in range(8):
            dma("pool", Wu[sl][:, dc, :], D["wu"][e, dc * 128:(dc + 1) * 128, fh * 512:(fh + 1) * 512], [], [key], key)
        for fc in range(4):
            dma("pool", Wd[sl][:, fc, :], D["wd"][e, fh * 512 + fc * 128:fh * 512 + (fc + 1) * 128, :], [], [key], key)

    load_unit(0)

    if D.get("Gall") is not None:
        s.add("sp", lambda e: e.dma_start(out=D["Gloc"], in_=D["Gall"][bass.ds(e.snap(st["reg"]), 1024), :]),
              reads=[K("G", q_) for q_ in range(4)], writes=[K("Gloc")], grp=K("Gloc"))
    def ln_stats(src, sl):
        for hh in range(2):
            s.add("dve", (lambda hh: lambda e: e.bn_stats(out=stats2[sl][:, hh * 6:(hh + 1) * 6], in_=src[:, hh * 512:(hh + 1) * 512]))(hh),
                  reads=[K("lnsrc", sl)], writes=[K("stats", sl)])
        s.add("dve", lambda e: e.bn_aggr(out=mv2[sl][:, 0:2], in_=stats2[sl][:, 0:12]), reads=[K("stats", sl)], writes=[K("mv", sl)])
        s.add("act", lambda e: e.activation(out=mv2[sl][:, 2:3], in_=mv2[sl][:, 1:2], func=AF.Sqrt, bias=LN_EPS), reads=[K("mv", sl)], writes=[K("mvb", sl)])

    def ln_apply(src, dst, gi, bi, dkey, sl):
        s.add("dve", lambda e: e.reciprocal(out=mv2[sl][:, 3:4], in_=mv2[sl][:, 2:3]), reads=[K("mvb", sl)], writes=[K("mvc", sl)])
        s.add("dve", lambda e: e.tensor_scalar(out=src, in0=src, scalar1=mv2[sl][:, 0:1], scalar2=mv2[sl][:, 3:4], op0=ALU.subtract, op1=ALU.mult),
              reads=[K("mv", sl), K("mvc", sl), K("lnsrc", sl)], writes=[K("lnsrc", sl)])
        s.add("dve", lambda e: e.tensor_tensor(out=src, in0=src, in1=lnp[gi], op=ALU.mult), reads=[K("lnsrc", sl), K("lnp")], writes=[K("lnsrc", sl)])
        s.add("dve", lambda e: e.tensor_tensor(out=dst, in0=src, in1=lnp[bi], op=ALU.add), reads=[K("lnsrc", sl), K("lnp")], writes=[dkey])

    def loads(ti):
        sl = ti % 2
        for ec in range(8):
            dma("sp", YT[sl][:, ec, :], D["Gloc"][ec * 128:(ec + 1) * 128, ti * 128:(ti + 1) * 128], [K("Gloc")], [K("YT", sl)], K("YT", sl))
        dma("sp", xt[sl], D["x"][ti * 128:(ti + 1) * 128, :], [("x2s",)], [K("xt", sl)], K("xt", sl))

    def phase1_gen(ti):
        sl = ti % 2
        pb = 4 * sl
        v32 = v32s[sl]
        X1T32 = X1T32s[sl]
        loads(ti)
        yield
        for hh in range(2):
            for ec in range(8):
                s.add("pe", (lambda hh, ec: lambda e: e.matmul(ps[pb + hh][:, :], YT[sl][:, ec, :], Wo[:, ec, hh * 512:(hh + 1) * 512],
                                                               start=(ec == 0), stop=(ec == 7)))(hh, ec),
                      reads=[K("YT", sl), K("wslot", 1)], writes=[("ps", pb + hh)])
            s.add("dve", (lambda hh: lambda e: e.scalar_tensor_tensor(out=v32[:, hh * 512:(hh + 1) * 512], in0=xt[sl][:, hh * 512:(hh + 1) * 512],
                                                                      scalar=float(ALPHA), in1=ps[pb + hh][:, :], op0=ALU.mult, op1=ALU.add))(hh),
                  reads=[K("xt", sl), ("ps", pb + hh)], writes=[K("lnsrc", sl)])
        yield
        ln_stats(v32, sl)
        yield
        ln_apply(v32, xt[sl], 0, 1, K("xt", sl), sl)
        s.add("act", lambda e: e.activation(out=yacc[:, ti, :], in_=xt[sl], func=AF.Copy, scale=float(ALPHA)),
              reads=[K("xt", sl)], writes=[K("yacc", ti)])
        yield
        for q in range(2):
            for kq in range(4):
                dc = q * 4 + kq
                s.add("pe", (lambda q, kq, dc: lambda e: e.transpose(out=ps[pb + 2 + q][:, kq * 128:(kq + 1) * 128], in_=xt[sl][:, dc * 128:(dc + 1) * 128], identity=ident))(q, kq, dc),
                      reads=[K("xt", sl), K("c")], writes=[("ps", pb + 2 + q)])
            yield
            s.add("dve", (lambda q: lambda e: e.tensor_copy(out=X1T32[:, q * 4:(q + 1) * 4, :], in_=ps[pb + 2 + q][:, :].rearrange("p (c t) -> p c t", c=4)))(q),
                  reads=[("ps", pb + 2 + q)], writes=[K("X1T32", sl, q)])
            s.add("act", (lambda q: lambda e: e.activation(out=X1T[:, q * 4:(q + 1) * 4, ti * 128:(ti + 1) * 128], in_=X1T32[:, q * 4:(q + 1) * 4, :], func=AF.Copy))(q),
                  reads=[K("X1T32", sl, q)], writes=[K("X1T", ti)])
        yield
        for dc in range(8):
            s.add("pe", (lambda dc: lambda e: e.matmul(ps[pb][:, 0:16], X1T32[:, dc, :], Wr[:, dc, :], start=(dc == 0), stop=(dc == 7)))(dc),
                  reads=[K("X1T32", sl, dc // 4), K("c")], writes=[("ps", pb)])
        yield
        lg, ex, eq, p2, selm, msk = rt2[sl]
        v1, v2, gs, gsel, gmx, den = rs2[sl]
        R = K("rt", sl)
        s.add("dve", lambda e: e.tensor_tensor(out=lg, in0=ps[pb][:, 0:16], in1=br, op=ALU.add), reads=[("ps", pb), K("c")], writes=[R])
        s.add("dve", lambda e: e.tensor_reduce(out=gmx[:, 0:1], in_=lg, axis=AX.X, op=ALU.max), reads=[R], writes=[R])
        s.add("dve", lambda e: e.tensor_scalar(out=lg, in0=lg, scalar1=gmx[:, 0:1], scalar2=None, op0=ALU.subtract), reads=[R], writes=[R])
        s.add("act", lambda e: e.activation(out=ex, in_=lg, func=AF.Exp), reads=[R], writes=[K("rtb", sl)])
        yield
        R2 = K("rtb", sl)
        ex3 = ex.rearrange("p (g k) -> p g k", g=4)
        eq3 = eq.rearrange("p (g k) -> p g k", g=4)
        p23 = p2.rearrange("p (g k) -> p g k", g=4)
        sel3 = selm.rearrange("p (g k) -> p g k", g=4)
        msk3 = msk.rearrange("p (g k) -> p g k", g=4)
        s.add("dve", lambda e: e.tensor_reduce(out=v1, in_=ex3, axis=AX.X, op=ALU.max), reads=[R2], writes=[R2])
        s.add("dve", lambda e: e.tensor_tensor(out=eq3, in0=ex3, in1=v1.unsqueeze(2).to_broadcast([128, 4, 4]), op=ALU.is_equal), reads=[R2], writes=[R2])
        s.add("dve", lambda e: e.scalar_tensor_tensor(out=p2, in0=eq, scalar=-2.0, in1=ex, op0=ALU.mult, op1=ALU.add), reads=[R2], writes=[R2])
        s.add("dve", lambda e: e.tensor_reduce(out=v2, in_=p23, axis=AX.X, op=ALU.max), reads=[R2], writes=[R2])
        s.add("dve", lambda e: e.tensor_tensor(out=gs, in0=v1, in1=v2, op=ALU.add), reads=[R2], writes=[R2])
        s.add("dve", lambda e: e.tensor_reduce(out=gmx[:, 1:2], in_=gs, axis=AX.X, op=ALU.max), reads=[R2], writes=[R2])
        yield
        s.add("dve", lambda e: e.tensor_scalar(out=gsel, in0=gs, scalar1=gmx[:, 1:2], scalar2=None, op0=ALU.is_equal), reads=[R2], writes=[R2])
        s.add("dve", lambda e: e.tensor_tensor(out=sel3, in0=ex3, in1=v2.unsqueeze(2).to_broadcast([128, 4, 4]), op=ALU.is_ge), reads=[R2], writes=[R2])
        s.add("dve", lambda e: e.tensor_tensor(out=msk3, in0=sel3, in1=gsel.unsqueeze(2).to_broadcast([128, 4, 4]), op=ALU.mult), reads=[R2], writes=[R2])
        s.add("dve", lambda e: e.tensor_tensor(out=v1, in0=gs, in1=gsel, op=ALU.mult), reads=[R2], writes=[R2])
        s.add("dve", lambda e: e.tensor_reduce(out=den[:, 0:1], in_=v1, axis=AX.X, op=ALU.add), reads=[R2], writes=[R2])
        s.add("dve", lambda e: e.reciprocal(out=den[:, 1:2], in_=den[:, 0:1]), reads=[R2], writes=[R2])
        s.add("dve", lambda e: e.tensor_tensor(out=msk, in0=msk, in1=ex, op=ALU.mult), reads=[R2], writes=[R2])
        s.add("dve", lambda e: e.tensor_scalar(out=gates[:, ti, :], in0=msk, scalar1=den[:, 1:2], scalar2=None, op0=ALU.mult),
              reads=[R2], writes=[K("gates", ti)])

    def run_two(gens):
        gens = list(gens)
        active = [None, None]
        nxt = [0]

        def refill(slot):
            for j in range(nxt[0], len(gens)):
                if gens[j] is not None and gens[j][0] % 2 == slot:
                    g = gens[j][1]
                    gens[j] = None
                    return g
            return None
        active[0] = refill(0)
        active[1] = refill(1)
        for _ in range(3):
            if active[0] is not None:
                try:
                    next(active[0])
                except StopIteration:
                    active[0] = refill(0)
        while active[0] is not None or active[1] is not None:
            for slot in (0, 1):
                if active[slot] is None:
                    continue
                try:
                    next(active[slot])
                except StopIteration:
                    active[slot] = refill(slot)

    run_two([(ti, phase1_gen(ti)) for ti in range(NT)])
    dma("sp", lnp[0], D["ln2g"], [], [K("lnp")], K("lnp"))
    dma("sp", lnp[1], D["ln2b"], [], [K("lnp")], K("lnp"))

    NU = NE * 2
    hcnt = [0]
    for u in range(NU):
        e_, fh = u // 2, u % 2
        sl = u % 2
        if u + 1 < NU:
            load_unit(u + 1)
        wkey = K("wslot", sl)
        for t4 in range(NT4):
            hs = hcnt[0] % 2
            hcnt[0] += 1
            tok = slice(t4 * TT, (t4 + 1) * TT)
            for fc in range(4):
                gi = 0 + (fc % 2)
                ui = 2 + (fc % 2)
                for dc in range(8):
                    s.add("pe", (lambda sl, dc, fc, gi, tok: lambda e: e.matmul(ps[gi][:, 0:TT], Wg[sl][:, dc, fc * 128:(fc + 1) * 128], X1T[:, dc, tok],
                                                                               start=(dc == 0), stop=(dc == 7)))(sl, dc, fc, gi, tok),
                          reads=[wkey] + [K("X1T", t4 * NSUB + i) for i in range(NSUB)], writes=[("ps", gi)])
                for dc in range(8):
                    s.add("pe", (lambda sl, dc, fc, ui, tok: lambda e: e.matmul(ps[ui][:, 0:TT], Wu[sl][:, dc, fc * 128:(fc + 1) * 128], X1T[:, dc, tok],
                                                                               start=(dc == 0), stop=(dc == 7)))(sl, dc, fc, ui, tok),
                          reads=[wkey] + [K("X1T", t4 * NSUB + i) for i in range(NSUB)], writes=[("ps", ui)])
                si = fc % 2
                s.add("act", (lambda gi, si: lambda e: e.activation(out=sg[si][:, 0:TT], in_=ps[gi][:, 0:TT], func=AF.Silu))(gi, si),
                      reads=[("ps", gi)], writes=[K("sg", si)])
                s.add("dve", (lambda hs, fc, si, ui: lambda e: e.tensor_tensor(out=hT[hs][:, fc, :], in0=sg[si][:, 0:TT], in1=ps[ui][:, 0:TT], op=ALU.mult))(hs, fc, si, ui),
                      reads=[K("sg", si), ("ps", ui)], writes=[K("hT", hs, fc)])
            for sub in range(NSUB):
                ti = t4 * NSUB + sub
                for hh in range(2):
                    di = 4 + ((sub * 2 + hh) % 2)
                    for fc in range(4):
                        s.add("pe", (lambda sl, hs, fc, sub, hh, di: lambda e: e.matmul(ps[di][:, :], hT[hs][:, fc, sub * 128:(sub + 1) * 128], Wd[sl][:, fc, hh * 512:(hh + 1) * 512],
                                                                                         start=(fc == 0), stop=(fc == 3)))(sl, hs, fc, sub, hh, di),
                              reads=[wkey, K("hT", hs, fc)], writes=[("ps", di)])
                    s.add("dve", (lambda ti, hh, di, e_: lambda e: e.scalar_tensor_tensor(out=yacc[:, ti, hh * 512:(hh + 1) * 512], in0=ps[di][:, :], scalar=gates[:, ti, e_:e_ + 1],
                                                                                          in1=yacc[:, ti, hh * 512:(hh + 1) * 512], op0=ALU.mult, op1=ALU.add))(ti, hh, di, e_),
                          reads=[("ps", di), K("gates", ti), K("yacc", ti)], writes=[K("yacc", ti)])

    outs = []

    def ln2_gen(ti):
        sl = ti % 2
        pb = 4 * sl
        v32 = v32s[sl]
        s.add("dve", lambda e: e.tensor_copy(out=v32, in_=yacc[:, ti, :]), reads=[K("yacc", ti)], writes=[K("lnsrc", sl)])
        ln_stats(v32, sl)
        yield
        ln_apply(v32, xt[sl], 0, 1, K("xt", sl), sl)
        yield
        if last:
            outs.append(dma("sp", D["x2"][ti * 128:(ti + 1) * 128, :], xt[sl], [K("xt", sl)], [], K("x2out")))
        else:
            outs.append(dma("sp", D["x2s"][ti * 128:(ti + 1) * 128, :], xt[sl], [K("xt", sl)], [("x2s",)], K("x2out")))
            for q in range(2):
                for kq in range(4):
                    dc = q * 4 + kq
                    s.add("pe", (lambda q, kq, dc: lambda e: e.transpose(out=ps[pb + 2 + q][:, kq * 128:(kq + 1) * 128], in_=xt[sl][:, dc * 128:(dc + 1) * 128], identity=ident))(q, kq, dc),
                          reads=[K("xt", sl), K("c")], writes=[("ps", pb + 2 + q)])
            yield
            tq = ti % 4
            for q in range(2):
                s.add("dve", (lambda q: lambda e: e.tensor_copy(out=xstp[:, q * 4:(q + 1) * 4, tq * 128:(tq + 1) * 128], in_=ps[pb + 2 + q][:, :].rearrange("p (c t) -> p c t", c=4)))(q),
                      reads=[("ps", pb + 2 + q)], writes=[K("xstp", ti)])

    if last:
        run_two([(ti, ln2_gen(ti)) for ti in range(NT)])
    else:
        for p in range(NT // 4):
            run_two([(ti, ln2_gen(ti)) for ti in range(4 * p, 4 * p + 4)])
            for dc in range(8):
                outs.append(dma("sp", D["X2T"][p][dc * 128:(dc + 1) * 128, :], xstp[:, dc, :], [K("xstp", 4 * p + i) for i in range(4)],
                                [("X2T", p)] + [K("xstp", 4 * p + 4 + i) for i in range(4)], K("x2out")))
            if cc2 is not None:
                cc2(p)
    return outs

I32 = mybir.dt.int32
_GROUPS = [[0, 1, 2, 3], [4, 5, 6, 7]]


def _build_fused(S=8192, T=2048, NE=16, depth=2):
    nc = bass.Bass("TRN2", target_bir_lowering=False, num_devices=8)
    D = {}

    def inp(name, shape, dt=F32):
        D[name] = nc.dram_tensor(name, shape, dt, kind="ExternalInput").ap()
    inp("xT", [1024, S])
    inp("xres", [T, 1024])
    inp("qi", [1, 1], I32)
    inp("wA", [depth * 1024, 768])
    inp("gA", [depth * 256, 1])
    inp("biasm", [3, 2, 128, 256])
    C = consts_A()
    for k, v in C.items():
        inp(k, list(v.shape), BF16 if v.dtype == ml_dtypes.bfloat16 else F32)
    inp("wo", [depth * 1024, 1024])
    inp("lnp", [depth * 4 * 128, 1024])
    inp("wr", [1024, 16])
    inp("br", [128, 16])
    inp("ident32", [128, 128])
    for nm in ("wg", "wu", "wd"):
        inp(nm, [depth * NE, 1024, 1024])
    D["out"] = nc.dram_tensor("out", [T, 1024], F32, kind="ExternalOutput").ap()
    yA = [[nc.dram_tensor("yA%d_%d" % (l, p), [256, T], BF16).ap() for p in range(4)] for l in range(depth)]
    Gall = [nc.dram_tensor("Gall%d" % l, [4096, T], BF16).ap() for l in range(depth)]
    Gloc = [nc.dram_tensor("Gloc%d" % l, [1024, T], BF16).ap() for l in range(depth)]
    X2T = [nc.dram_tensor("X2T%d" % p, [1024, 512], BF16).ap() for p in range(4)]
    GX = [nc.dram_tensor("GX%d" % p, [4096, 512], BF16).ap() for p in range(4)]
    x2s = nc.dram_tensor("x2s", [T, 1024], F32).ap()
    with ExitStack() as es:
        arena = es.enter_context(nc.sbuf_tensor("arena", [128, 103 * 1024], BF16))
        qs = es.enter_context(nc.sbuf_tensor("qs", [1, 1], I32))
        reg = es.enter_context(nc.sync.register("qreg"))
        ps = [es.enter_context(nc.psum_tensor("ps%d" % i, [128, 512], F32)) for i in range(8)]
        s = Sched(nc)
        st = {}
        s.add("sp", lambda e: e.dma_start(out=qs[:, :], in_=D["qi"]), writes=[("qs",)], grp=("qs",))

        def ld(e):
            ins = e.reg_load(reg, qs[0:1, 0:1])
            st["reg"] = reg
            return ins
        s.add("sp", ld, reads=[("qs",)], writes=[("qreg",)])
        outs = []
        ccn = [0]
        for l in range(depth):
            last = (l == depth - 1)
            DA = dict(D)
            DA["w"] = D["wA"][l * 1024:(l + 1) * 1024, :]
            DA["g"] = D["gA"][l * 256:(l + 1) * 256, :]
            DA["yA"] = yA[l]
            DA["GX"] = GX

            def cc1(p, l=l):
                ccn[0] += 1
                s.add("pool", lambda e: e.collective_compute("AllGather", ALU.bypass, replica_groups=_GROUPS, ins=[yA[l][p]],
                                                             outs=[Gall[l][p * 1024:(p + 1) * 1024, :]]),
                      reads=[("A%d" % l, "yA", p)], writes=[("B%d" % l, "G", p)], grp=("cc", ccn[0]), inc=1)
            ar = Arena(arena)
            emit_A(nc, s, ar, ps, DA, S, l == 0, "A%d" % l, cc=cc1)
            s.fence()
            DB = dict(D)
            DB["Gall"] = Gall[l]
            DB["Gloc"] = Gloc[l]
            DB["x"] = D["xres"] if l == 0 else x2s
            DB["wo"] = D["wo"][l * 1024:(l + 1) * 1024, :]
            for i, nm in enumerate(("ln1g", "ln1b", "ln2g", "ln2b")):
                DB[nm] = D["lnp"][(l * 4 + i) * 128:(l * 4 + i + 1) * 128, :]
            for nm in ("wg", "wu", "wd"):
                DB[nm] = D[nm][l * NE:(l + 1) * NE]
            DB["x2"] = D["out"]
            DB["x2s"] = x2s
            DB["X2T"] = X2T

            def cc2(p):
                ccn[0] += 1
                s.add("pool", lambda e: e.collective_compute("AllGather", ALU.bypass, replica_groups=_GROUPS, ins=[X2T[p]], outs=[GX[p]]),
                      reads=[("X2T", p)], writes=[("GX", p)], grp=("cc", ccn[0]), inc=1)
            ar = Arena(arena)
            o = emit_B(nc, s, ar, ps, DB, T, "B%d" % l, NE=NE, st=st, last=last, cc2=None if last else cc2)
            if last:
                outs = o
            else:
                s.fence()
        keys = s.emit(final_wait_ops=outs)
        sems = {k: es.enter_context(nc.semaphore("s%d" % i)) for i, k in enumerate(keys)}
        s.run(sems, final_wait_ops=outs)
    return nc, C, len(s.ops), len(keys)


def kernel(x, w_in, g_sb, g_dil, w_out, ln1_g, ln1_b, ln2_g, ln2_b, rel_bias, w_router, b_router, w_gate, w_up, w_down):
    x = np.asarray(x, np.float32)
    B, S, Dm = x.shape
    depth = w_in.shape[0]
    T = 2048
    NE = w_gate.shape[1]
    nc, C, _, _ = _build_fused(S, T, NE, depth)
    f32 = lambda v: np.asarray(v, np.float32)
    rep = lambda v: np.broadcast_to(f32(v)[None, :], (128, v.shape[0]))
    rel_bias = f32(rel_bias)
    perm = np.concatenate([np.concatenate([np.arange(128 * j, 128 * j + 128), 512 + np.arange(128 * j, 128 * j + 128)]) for j in range(4)])
    wo = np.ascontiguousarray(np.concatenate([f32(w_out[l])[perm, :] for l in range(depth)], axis=0))
    lnp = np.ascontiguousarray(np.concatenate([rep(p[l]) for l in range(depth) for p in (ln1_g, ln1_b, ln2_g, ln2_b)], axis=0))
    shared = {"wo": wo, "lnp": lnp, "wr": np.ascontiguousarray(f32(w_router)), "br": np.ascontiguousarray(rep(b_router)),
              "ident32": np.eye(128, dtype=np.float32),
              "wg": f32(w_gate).reshape(depth * NE, 1024, 1024), "wu": f32(w_up).reshape(depth * NE, 1024, 1024),
              "wd": f32(w_down).reshape(depth * NE, 1024, 1024)}
    shared.update(C)
    xTs = [np.ascontiguousarray(x[b].T) for b in range(B)]
    in_maps = []
    for c in range(8):
        b, j = c // 4, c % 4
        wA = []
        gA = []
        for l in range(depth):
            wl = f32(w_in[l])
            wA.append(np.concatenate([wl[:, o + 128 * j:o + 128 * j + 128] for o in (0, 512, 1536, 2048, 1024, 2560)], axis=1))
            gA.append(np.concatenate([f32(g_sb[l])[128 * j:128 * j + 128], f32(g_dil[l])[128 * j:128 * j + 128]])[:, None])
        m = {"xT": xTs[b], "xres": np.ascontiguousarray(x[b, j * T:(j + 1) * T, :]), "qi": np.array([[1024 * j]], np.int32),
             "wA": np.ascontiguousarray(np.concatenate(wA, axis=0)), "gA": np.ascontiguousarray(np.concatenate(gA, axis=0)),
             "biasm": dil_bias_tables(rel_bias[:, 2 * j:2 * j + 2])}
        m.update(shared)
        in_maps.append(m)
    res = run_bass_kernel_spmd(nc, in_maps, core_ids=list(range(8)))
    out = np.zeros((B, S, Dm), np.float32)
    for c in range(8):
        b, j = c // 4, c % 4
        out[b, j * T:(j + 1) * T, :] = np.asarray(res.results[c]["out"], np.float32)
    return out
```

```python
from contextlib import ExitStack
from concourse.bass_utils import run_bass_kernel_spmd
import numpy as np
import concourse.bass as bass
import concourse.mybir as mybir

F32 = mybir.dt.float32
BF16 = mybir.dt.bfloat16
AF = mybir.ActivationFunctionType
ALU = mybir.AluOpType

COMPUTE = ("pe", "act", "dve", "pool")


class Sched:
    def __init__(self, nc):
        self.nc = nc
        self.ops = []
        self.last_writer = {}
        self.readers = {}
        self.dma_groups = {}
        self.fence_deps = set()
        self.fence_pending = set()

    def fence(self):
        last = {}
        for i, op in enumerate(self.ops):
            k = ("dma", op["grp"]) if op["dma"] else ("eng", op["eng"])
            last[k] = i
        self.fence_deps = set(last.values())
        self.fence_pending = {"pe", "act", "dve", "pool", "sp"}

    def add(self, eng, fn, reads=(), writes=(), grp=None, inc=16):
        idx = len(self.ops)
        deps = set()
        if eng in self.fence_pending:
            deps |= self.fence_deps
            self.fence_pending.discard(eng)
        for b in reads:
            w = self.last_writer.get(b)
            if w is not None:
                deps.add(w)
        for b in writes:
            w = self.last_writer.get(b)
            if w is not None:
                deps.add(w)
            for r in self.readers.get(b, ()):
                deps.add(r)
        deps.discard(idx)
        is_dma = grp is not None
        if eng == "pe":
            deps = {d for d in deps if not (self.ops[d]["eng"] == "pe" and not self.ops[d]["dma"])}
        op = dict(eng=eng, fn=fn, deps=deps, dma=is_dma, grp=grp, needed=False, inc=(inc if is_dma else 1))
        self.ops.append(op)
        for b in reads:
            self.readers.setdefault(b, []).append(idx)
        for b in writes:
            self.last_writer[b] = idx
            self.readers[b] = []
        return idx

    def emit(self, final_wait_ops=()):
        nc = self.nc
        ops = self.ops
        for op in ops:
            for d in op["deps"]:
                ops[d]["needed"] = True
        for d in final_wait_ops:
            ops[d]["needed"] = True
        sem_names = []
        counters = {}
        for i, op in enumerate(ops):
            if not op["needed"]:
                continue
            key = ("dma", op["grp"]) if op["dma"] else ("eng", op["eng"])
            if key not in counters:
                counters[key] = 0
                sem_names.append(key)
            counters[key] += op["inc"]
            op["sem"] = key
            op["val"] = counters[key]
        self.sem_keys = sem_names
        return sem_names

    def run(self, sems, final_wait_ops=()):
        nc = self.nc
        ops = self.ops
        per_eng = {}
        for i, op in enumerate(ops):
            per_eng.setdefault(op["eng"], []).append(i)

        def body(engname, eng):
            known = {}
            for i in per_eng.get(engname, []):
                op = ops[i]
                need = {}
                for d in op["deps"]:
                    p = ops[d]
                    k = p["sem"]
                    need[k] = max(need.get(k, 0), p["val"])
                for k, v in need.items():
                    if known.get(k, 0) >= v:
                        continue
                    eng.wait_ge(sems[k], v)
                    known[k] = v
                ins = op["fn"](eng)
                if op["needed"]:
                    ins.then_inc(sems[op["sem"]], op["inc"])
            if engname == "sp":
                fin = {}
                for d in final_wait_ops:
                    p = ops[d]
                    fin[p["sem"]] = max(fin.get(p["sem"], 0), p["val"])
                for k, v in fin.items():
                    if known.get(k, 0) < v:
                        eng.wait_ge(sems[k], v)
                        known[k] = v

        with nc.Block() as block:
            @block.sync
            def _(e):
                body("sp", e)

            @block.tensor
            def _(e):
                body("pe", e)

            @block.scalar
            def _(e):
                body("act", e)

            @block.vector
            def _(e):
                body("dve", e)

            @block.gpsimd
            def _(e):
                body("pool", e)


import math
import numpy as np
import ml_dtypes
import concourse.bass as bass
import concourse.mybir as mybir

NEG = -30000.0
SKIP = ['dveonly']
SC = 2048


def t5_bucket_np(dist):
    max_exact = 16
    d = np.maximum(dist, 0)
    large = max_exact + (np.log(np.maximum(d, 1).astype(np.float32) / np.float32(max_exact))
                         / np.float32(math.log(2048 / max_exact)) * np.float32(32 - max_exact)).astype(np.int32)
    large = np.minimum(large, 31)
    return np.where(d < max_exact, d, large)


def consts_A():
    j = np.arange(128)[:, None]
    s = np.arange(128)[None, :]
    negtri = np.where(j >= s, -1.0, 0.0).astype(ml_dtypes.bfloat16)
    negones = np.full((128, 128), -1.0, dtype=ml_dtypes.bfloat16)
    ident = np.eye(128, dtype=ml_dtypes.bfloat16)
    t = np.arange(512)[None, :]
    sbmask = np.stack([np.where(128 * a + j >= t, NEG, 0.0) for a in range(4)], 0).astype(ml_dtypes.bfloat16)
    onesblk = np.zeros((128, 128), np.float32)
    onesblk[:64, :64] = 1.0
    onesblk[64:, 64:] = 1.0
    sel65 = np.zeros((65, 64), np.float32)
    sel65[64, :] = 1.0
    ones65 = np.zeros((65, 64), np.float32)
    ones65[:64, :] = 1.0
    return dict(negtri=negtri, negones=negones, ident=ident, sbmask=sbmask, onesblk=onesblk, sel65=sel65, ones65=ones65)


def dil_bias_tables(rel_bias_heads):
    kj = np.arange(128)[:, None]
    qi = np.arange(128)[None, :]
    out = np.zeros((3, 2, 128, 256), np.float32)
    for ri, r in enumerate((1, 4, 16)):
        steps_cur = qi - kj
        steps_prev = qi - kj + 128
        b_cur = t5_bucket_np(np.maximum(steps_cur, 0) * r)
        b_prev = t5_bucket_np(np.maximum(steps_prev, 0) * r)
        for h in range(2):
            cur = np.where(steps_cur >= 0, rel_bias_heads[b_cur, h], np.float32(NEG))
            prev = np.where(steps_prev <= 128, rel_bias_heads[b_prev, h], np.float32(NEG))
            out[ri, h, :, :128] = prev
            out[ri, h, :, 128:] = cur
    return out


class Arena:
    def __init__(self, ap_bf16):
        self.ap = ap_bf16
        self.off = 0
        self.total = ap_bf16.shape[1]

    def take(self, nelem, dtype=BF16):
        nb = nelem * (4 if dtype == F32 else 2)
        nb = (nb + 63) // 64 * 64
        n16 = nb // 2
        assert self.off + n16 <= self.total, ("arena overflow", self.off, n16, self.total)
        v = self.ap[:, self.off:self.off + n16]
        self.off += n16
        if dtype == F32:
            return v.bitcast(F32)[:, :nelem]
        return v[:, :nelem]


def emit_A(nc, s, ar, ps, D, S, src_is_f32, lname, stop=99, cc=None):
    NSC = S // SC
    NB = S // 128
    L = lname
    K = lambda *a: (L,) + a

    Wb = ar.take(8 * 768).rearrange("p (f c) -> p f c", f=8)
    XT = ar.take(8 * SC).rearrange("p (f t) -> p f t", f=8)
    QTs = ar.take(SC)
    QTsB = ar.take(SC)
    QTd = ar.take(SC)
    KTs = ar.take(S)
    KTd = ar.take(S)
    Vs = ar.take(NB * 128).rearrange("p (b c) -> p b c", c=128)
    NW1 = NB
    Vd1 = ar.take(NW1 * 132).rearrange("p (b h c) -> p b h c", h=2, c=66)
    Vd4 = ar.take(32 * 132).rearrange("p (b h c) -> p b h c", h=2, c=66)
    Vd16 = ar.take(32 * 132).rearrange("p (b h c) -> p b h c", h=2, c=66)
    negtri = ar.take(128)
    negones = ar.take(128)
    ident = ar.take(128)
    sbmask = ar.take(4 * 512).rearrange("p (a t) -> p a t", a=4)
    onesblk = ar.take(128, F32)
    sel65 = ar.take(64, F32)
    ones65 = ar.take(64, F32)
    gvec = ar.take(4, F32)
    biasm16 = ar.take(6 * 256).rearrange("p (r h c) -> p r h c", r=3, h=2)
    E32 = [ar.take(512, F32) for _ in range(2)]
    Lp16 = [ar.take(512) for _ in range(4)]
    LA32 = [ar.take(512, F32) for _ in range(2)]
    A16 = [ar.take(512) for _ in range(4)]
    ncar = [ar.take(512, F32) for _ in range(2)]
    P16 = [ar.take(512) for _ in range(6)]
    acc = [ar.take(SC, F32) for _ in range(2)]
    o32 = ar.take(512, F32)
    sq32 = ar.take(512, F32)
    r32 = ar.take(512, F32)
    Yb = [ar.take(512) for _ in range(2)]

    def dma(eng, out, in_, reads, writes, grp):
        return s.add(eng, lambda e: e.dma_start(out=out, in_=in_), reads=reads, writes=writes, grp=grp)

    for fc in range(8):
        dma("pool", Wb[:, fc, :], D["w"][fc * 128:(fc + 1) * 128, :], [], [K("W")], K("W"))
    dma("sp", negtri, D["negtri"], [], [K("c")], K("c"))
    dma("sp", negones, D["negones"], [], [K("c")], K("c"))
    dma("sp", ident, D["ident"], [], [K("c")], K("c"))
    for a in range(4):
        dma("sp", sbmask[:, a, :], D["sbmask"][a], [], [K("c")], K("c"))
    dma("sp", onesblk, D["onesblk"], [], [K("c")], K("c"))
    dma("sp", sel65[0:65, :], D["sel65"], [], [K("c")], K("c"))
    dma("sp", ones65[0:65, :], D["ones65"], [], [K("c")], K("c"))
    dma("sp", gvec[:, 0:1], D["g"][0:128, :], [], [K("c")], K("c"))
    dma("sp", gvec[0:64, 1:2], D["g"][128:192, :], [], [K("c")], K("c"))
    dma("sp", gvec[0:64, 2:3], D["g"][192:256, :], [], [K("c")], K("c"))
    for ri in range(3):
        for h in range(2):
            dma("sp", acc[0][:, (ri * 2 + h) * 256:(ri * 2 + h + 1) * 256], D["biasm"][ri, h], [], [K("acc", 0)], K("bm"))
    s.add("act", lambda e: e.activation(out=biasm16.rearrange("p r h c -> p (r h c)"), in_=acc[0][:, 0:1536], func=AF.Exp), reads=[K("acc", 0)], writes=[K("ebias")])
    for vb in (Vd1, Vd4, Vd16):
        s.add("dve", (lambda vb: lambda e: e.memset(vb[:, :, :, 64:65], 1.0))(vb), writes=[K("vones")])

    psX = [ps[6], ps[7]]
    def dummy_out():
        return [dma("sp", D["yT"][0:128, 0:512], QTs[:, 0:512], [K("c"), K("W"), K("XT"), K("vones"), K("QTs"), K("QTd"), K("Vd4", 0), K("Vd16", 0), K("Vs", 0)], [], K("yout"))]
    if stop == 0:
        return dummy_out()
    xcnt = [0]

    def nextX():
        i = xcnt[0] % 2
        xcnt[0] += 1
        return i

    evac_cnt = [0]

    def evac(out, in_, reads, writes, scale=None):
        evac_cnt[0] += 1
        if evac_cnt[0] % 2 == 0 or 'dveonly' in SKIP:
            if scale is None:
                return s.add("dve", lambda e: e.tensor_copy(out=out, in_=in_), reads=reads, writes=writes)
            return s.add("dve", lambda e: e.tensor_scalar(out=out, in0=in_, scalar1=float(scale), scalar2=None, op0=ALU.mult),
                         reads=reads, writes=writes)
        sc = 1.0 if scale is None else float(scale)
        return s.add("act", lambda e: e.activation(out=out, in_=in_, func=AF.Copy, scale=sc), reads=reads, writes=writes)

    out_dmas = []
    ycnt = [0]

    def ss(start, n, step):
        return slice(start, start + (n - 1) * step + 1, step)

    QTs2 = [QTs, QTsB]

    def make(sc_i):
        t0 = sc_i * SC

        def load_xt(sci):
            for fc in range(8):
                if src_is_f32:
                    dma("pool", XT[:, fc, :], D["xT"][fc * 128:(fc + 1) * 128, sci * SC:(sci + 1) * SC], [], [K("XT")], K("XT"))
                else:
                    for p in range(4):
                        dma("sp", XT[:, fc, p * 512:(p + 1) * 512], D["GX"][p][sci * 1024 + fc * 128:sci * 1024 + (fc + 1) * 128, :], [("GX", p)], [K("XT")], K("XT"))

        def prep():
            if sc_i == 0:
                load_xt(0)
            for c in range(4):
                tl = c * 512
                for kind, col0, dst, scale in (("qs", 0, QTs2[sc_i % 2][:, tl:tl + 512], 0.125), ("ks", 128, KTs[:, t0 + tl:t0 + tl + 512], None),
                                               ("qd", 256, QTd[:, tl:tl + 512], 0.125), ("kd", 384, KTd[:, t0 + tl:t0 + tl + 512], None)):
                    yield
                    xi = nextX()
                    for fc in range(8):
                        s.add("pe", (lambda xi, fc, col0, tl: lambda e: e.matmul(psX[xi][:, :], Wb[:, fc, col0:col0 + 128], XT[:, fc, tl:tl + 512],
                                                                                 start=(fc == 0), stop=(fc == 7)))(xi, fc, col0, tl),
                              reads=[K("W"), K("XT")], writes=[("ps", 6 + xi)])
                    wkey = {"qs": K("QTs", sc_i % 2), "ks": K("KTs", sc_i), "qd": K("QTd"), "kd": K("KTd", sc_i)}[kind]
                    evac(dst, psX[xi][:, :], [("ps", 6 + xi)], [wkey], scale)
                for sub in range(4 if 'vnat' not in SKIP else 0):
                    tt = tl + sub * 128
                    blk = (t0 + tt) // 128
                    yield
                    xi = nextX()
                    for fc in range(8):
                        s.add("pe", (lambda xi, fc, tt: lambda e: e.matmul(psX[xi][:, 0:256], XT[:, fc, tt:tt + 128], Wb[:, fc, 512:768],
                                                                           start=(fc == 0), stop=(fc == 7)))(xi, fc, tt),
                              reads=[K("W"), K("XT")], writes=[("ps", 6 + xi)])
                    if 'evs' not in SKIP: evac(Vs[:, blk, :], psX[xi][:, 0:128], [("ps", 6 + xi)], [K("Vs", blk)])
                    for hh in range(2 if 'evd1' not in SKIP else 0):
                        evac(Vd1[:, blk, hh, 0:64], psX[xi][:, 128 + 64 * hh:192 + 64 * hh], [("ps", 6 + xi)], [K("Vd1", blk)])
                n4 = 4 * sc_i + c
                yield
                xi = nextX()
                for cls in range(4 if 'v4' not in SKIP else 0):
                    for fc in range(8):
                        s.add("pe", (lambda xi, fc, cls, tl: lambda e: e.matmul(psX[xi][:, cls * 128:(cls + 1) * 128],
                                                                                XT[:, fc, ss(tl + cls, 128, 4)], Wb[:, fc, 640:768],
                                                                                start=(fc == 0), stop=(fc == 7)))(xi, fc, cls, tl),
                              reads=[K("W"), K("XT")], writes=[("ps", 6 + xi)])
                slot0 = (n4 % 8) * 4
                for kk_ in range(4 if 'v4' not in SKIP else 0):
                    for hh in range(2):
                        evac(Vd4[:, slot0 + kk_, hh, 0:64], psX[xi][:, kk_ * 128 + 64 * hh:kk_ * 128 + 64 * hh + 64], [("ps", 6 + xi)], [K("Vd4", n4 % 8)])
            for c4 in range(4 if 'v16' not in SKIP else 0):
                yield
                xi = nextX()
                for k in range(4):
                    cls = c4 * 4 + k
                    for fc in range(8):
                        s.add("pe", (lambda xi, fc, cls, k: lambda e: e.matmul(psX[xi][:, k * 128:(k + 1) * 128],
                                                                               XT[:, fc, ss(cls, 128, 16)], Wb[:, fc, 640:768],
                                                                               start=(fc == 0), stop=(fc == 7)))(xi, fc, cls, k),
                              reads=[K("W"), K("XT")], writes=[("ps", 6 + xi)])
                slot0 = (sc_i % 2) * 16 + c4 * 4
                for kk_ in range(4 if 'v16' not in SKIP else 0):
                    for hh in range(2):
                        evac(Vd16[:, slot0 + kk_, hh, 0:64], psX[xi][:, kk_ * 128 + 64 * hh:kk_ * 128 + 64 * hh + 64], [("ps", 6 + xi)], [K("Vd16", sc_i % 2)])

            yield
            if sc_i + 1 < NSC:
                load_xt(sc_i + 1)
            yield
            groups = []
            for h in range(2):
                for ri, r in enumerate((1, 4, 16)):
                    nblk_sc = SC // (128 * r)
                    for c in range(r):
                        n0 = sc_i * nblk_sc
                        for g0 in range(n0, n0 + nblk_sc, 4):
                            blks = list(range(g0, min(g0 + 4, n0 + nblk_sc)))
                            groups.append(dict(h=h, ri=ri, r=r, c=c, g0=g0, blks=blks, gi=len(groups)))
            SBANKS = [(3, 6), (3, 6), (3, 6)]

            def d_s1(g):
                h, ri, r, c, blks, gi = g["h"], g["ri"], g["r"], g["c"], g["blks"], g["gi"]
                hp = slice(64 * h, 64 * h + 64)
                units = []
                for n in blks:
                    units += [(n - 1, n), (n, n)]
                g["uinfo"] = []
                g["tls"] = []
                for t_i in range(0, len(units), 4):
                    tu = units[t_i:t_i + 4]
                    bank = SBANKS[gi % 3][t_i // 4]
                    pslot = (gi % 3) * 2 + t_i // 4
                    lo = None
                    for u, (kb, n) in enumerate(tu):
                        co = u * 128
                        g["uinfo"].append((pslot, co, kb))
                        if kb < 0:
                            continue
                        if lo is None:
                            lo = co
                        qstart = c + r * 128 * n - t0
                        kstart = c + r * 128 * kb
                        role = u % 2
                        s.add("pe", (lambda bank, co, kstart, qstart, r, hp: lambda e: e.matmul(
                            ps[bank][:, co:co + 128], KTd[hp, ss(kstart, 128, r)], QTd[hp, ss(qstart, 128, r)],
                            start=True, stop=True))(bank, co, kstart, qstart, r, hp),
                            reads=[K("KTd", kstart // SC), K("QTd")], writes=[("ps", bank)])
                    g["tls"].append((bank, pslot, lo, len(tu) * 128))

            def d_s2(g):
                for (bank, pslot, lo, hi) in g["tls"]:
                    s.add("act", (lambda bank, pslot, lo, hi: lambda e: e.activation(out=P16[pslot][:, lo:hi], in_=ps[bank][:, lo:hi], func=AF.Exp))(bank, pslot, lo, hi),
                          reads=[("ps", bank)], writes=[K("P", pslot)])
                    for half in range(0, hi, 256):
                        a0 = max(half, lo)
                        a1 = half + 256
                        bo = a0 - half
                        s.add("pool", (lambda pslot, a0, a1, bo, ri, h: lambda e: e.tensor_tensor(out=P16[pslot][:, a0:a1], in0=P16[pslot][:, a0:a1], in1=biasm16[:, ri, h, bo:256], op=ALU.mult))(
                            pslot, a0, a1, bo, g["ri"], g["h"]),
                            reads=[K("P", pslot), K("ebias")], writes=[K("P", pslot)])

            def d_s3(g):
                h, r, c, blks, gi = g["h"], g["r"], g["c"], g["blks"], g["gi"]
                ob = 7
                vkey = {1: "Vd1", 4: "Vd4", 16: "Vd16"}[r]
                for bi, n in enumerate(blks):
                    first = True
                    for uu in (2 * bi, 2 * bi + 1):
                        pslot, co, kb = g["uinfo"][uu]
                        if kb < 0:
                            continue
                        if r == 1:
                            vap = Vd1[:, kb, h, 0:65]
                        elif r == 4:
                            vap = Vd4[:, (kb % 8) * 4 + c, h, 0:65]
                        else:
                            vap = Vd16[:, (kb % 2) * 16 + c, h, 0:65]
                        last = (uu == 2 * bi + 1)
                        s.add("pe", (lambda ob, bi, vap, pslot, co, first, last: lambda e: e.matmul(
                            ps[ob][0:65, bi * 128:(bi + 1) * 128], vap, P16[pslot][:, co:co + 128],
                            start=first, stop=last))(ob, bi, vap, pslot, co, first, last),
                            reads=[K("P", pslot), K(vkey, kb if r == 1 else (kb % 8 if r == 4 else kb % 2)), K("vones")], writes=[("ps", ob)])
                        first = False

            def d_s4(g):
                h, r, c, blks, gi, g0 = g["h"], g["r"], g["c"], g["blks"], g["gi"], g["g0"]
                ob = 7
                nb_ = len(blks)
                col = c + r * (128 * g0) - t0
                if r == 1:
                    s.add("dve", (lambda h, col, nb_, ob: lambda e: e.tensor_copy(out=acc[h][0:65, col:col + 128 * nb_], in_=ps[ob][0:65, 0:128 * nb_]))(h, col, nb_, ob),
                          reads=[("ps", ob)], writes=[K("acc", h)])
                else:
                    s.add("dve", (lambda h, col, nb_, r, ob: lambda e: e.tensor_tensor(
                        out=acc[h][0:65, ss(col, 128 * nb_, r)], in0=ps[ob][0:65, 0:128 * nb_], in1=acc[h][0:65, ss(col, 128 * nb_, r)], op=ALU.add))(h, col, nb_, r, ob),
                        reads=[("ps", ob), K("acc", h)], writes=[K("acc", h)])

            for g in groups:
                d_s1(g)
                yield
                d_s2(g)
                yield
                d_s3(g)
                yield
                d_s4(g)
                yield
            for h in range(2):
                for tq in range(4):
                    yield
                    cs = slice(tq * 512, tq * 512 + 512)
                    xi = nextX()
                    s.add("pe", (lambda xi, h, cs: lambda e: e.matmul(psX[xi][0:64, :], sel65[0:65, :], acc[h][0:65, cs], start=True, stop=True))(xi, h, cs),
                          reads=[K("acc", h), K("c")], writes=[("ps", 6 + xi)])
                    s.add("dve", (lambda xi: lambda e: e.reciprocal(out=r32[0:64, :], in_=psX[xi][0:64, :]))(xi), reads=[("ps", 6 + xi)], writes=[K("r32")])
                    s.add("dve", (lambda h, cs: lambda e: e.tensor_tensor(out=o32[0:64, :], in0=acc[h][0:64, cs], in1=r32[0:64, :], op=ALU.mult))(h, cs),
                          reads=[K("acc", h), K("r32")], writes=[K("o32")])
                    s.add("act", lambda e: e.activation(out=sq32[0:64, :], in_=o32[0:64, :], func=AF.Square), reads=[K("o32")], writes=[K("sq32")])
                    xi2 = nextX()
                    s.add("pe", (lambda xi2: lambda e: e.matmul(psX[xi2][0:64, :], ones65[0:64, :], sq32[0:64, :], start=True, stop=True))(xi2),
                          reads=[K("sq32"), K("c")], writes=[("ps", 6 + xi2)])
                    s.add("act", (lambda xi2: lambda e: e.activation(out=r32[0:64, :], in_=psX[xi2][0:64, :], func=AF.Sqrt, scale=1.0 / 64, bias=1e-6))(xi2),
                          reads=[("ps", 6 + xi2)], writes=[K("r32")])
                    s.add("dve", lambda e: e.reciprocal(out=sq32[0:64, :], in_=r32[0:64, :]), reads=[K("r32")], writes=[K("sq32")])
                    yi = ycnt[0] % 2
                    ycnt[0] += 1
                    s.add("dve", (lambda yi, h: lambda e: e.scalar_tensor_tensor(out=Yb[yi][0:64, :], in0=o32[0:64, :], scalar=gvec[0:64, 1 + h:2 + h],
                                                                                  in1=sq32[0:64, :], op0=ALU.mult, op1=ALU.mult))(yi, h),
                          reads=[K("o32"), K("sq32"), K("c")], writes=[K("Yb", yi)])
                    od = dma("sp", D["yA"][sc_i][128 + 64 * h:128 + 64 * h + 64, tq * 512:tq * 512 + 512], Yb[yi][0:64, :], [K("Yb", yi)], [K("yA", sc_i)], K("yout"))
                    out_dmas.append(od)


        def run_sb(drip):
            jobs = []
            for qt in range(4 * sc_i, 4 * sc_i + 4):
                kbl = list(range(4 * qt + 3, -1, -1))
                for ki, kb in enumerate(kbl):
                    for h in range(2):
                        jobs.append(dict(qt=qt, kb=kb, h=h, first=(ki == 0), last=(ki == len(kbl) - 1), idx=len(jobs)))

            def stage1(j):
                qt, kb, h, i = j["qt"], j["kb"], j["h"], j["idx"]
                hp = slice(64 * h, 64 * h + 64)
                a = kb - 4 * qt
                bb, se, sl = i % 3, i % 2, i % 4
                ql = (qt - 4 * sc_i) * 512
                diag = a >= 0
                kk = K("KTs", (kb * 128) // SC)
                s.add("pe", lambda e: e.matmul(ps[bb][:, :], KTs[hp, kb * 128:(kb + 1) * 128], QTs2[sc_i % 2][hp, ql:ql + 512], start=True, stop=not diag),
                      reads=[kk, K("QTs", sc_i % 2)], writes=[("ps", bb)])
                if diag:
                    s.add("pe", lambda e: e.matmul(ps[bb][:, :], ident, sbmask[:, a, :], start=False, stop=True), reads=[K("c")], writes=[("ps", bb)])
                s.add("act", lambda e: e.activation(out=E32[se], in_=ps[bb][:, :], func=AF.Exp), reads=[("ps", bb)], writes=[K("E", se)])
                s.add("act", lambda e: e.activation(out=Lp16[sl], in_=E32[se], func=AF.Ln, bias=1.0), reads=[K("E", se)], writes=[K("Lp", sl)])

            def stage2(j):
                qt, kb, h, i = j["qt"], j["kb"], j["h"], j["idx"]
                bb, sl, sla, sa16 = i % 3, i % 4, i % 2, i % 4
                cb = 5
                s.add("pe", lambda e: e.matmul(ps[bb][:, :], negtri, Lp16[sl], start=False, stop=True, skip_group_check=True), reads=[K("Lp", sl), K("c")], writes=[("ps", bb)])
                if not j["last"]:
                    s.add("pe", lambda e: e.matmul(ps[cb][:, :], negones, Lp16[sl], start=True, stop=True), reads=[K("Lp", sl), K("c")], writes=[("ps", cb)])
                if j["first"]:
                    s.add("act", lambda e: e.activation(out=A16[sa16], in_=ps[bb][:, :], func=AF.Exp), reads=[("ps", bb)], writes=[K("A", sa16)])
                    if not j["last"]:
                        s.add("dve", lambda e: e.tensor_copy(out=ncar[h], in_=ps[cb][:, :]), reads=[("ps", cb)], writes=[K("ncar", h)])
                else:
                    s.add("dve", lambda e: e.tensor_tensor(out=LA32[sla], in0=ps[bb][:, :], in1=ncar[h], op=ALU.add),
                          reads=[("ps", bb), K("ncar", h)], writes=[K("LA", sla)])
                    if not j["last"]:
                        s.add("dve", lambda e: e.tensor_tensor(out=ncar[h], in0=ps[cb][:, :], in1=ncar[h], op=ALU.add),
                              reads=[("ps", cb), K("ncar", h)], writes=[K("ncar", h)])
                    s.add("act", lambda e: e.activation(out=A16[sa16], in_=LA32[sla], func=AF.Exp), reads=[K("LA", sla)], writes=[K("A", sa16)])

            def stage3(j):
                qt, kb, h, i = j["qt"], j["kb"], j["h"], j["idx"]
                sa16 = i % 4
                s.add("pe", lambda e: e.matmul(ps[4][64 * h:64 * h + 64, :], Vs[:, kb, 64 * h:64 * h + 64], A16[sa16],
                                               start=j["first"], stop=j["last"]),
                      reads=[K("A", sa16), K("Vs", kb)], writes=[("ps", 4)])
                if j["last"] and h == 1:
                    finalize_sb(qt)

            def finalize_sb(qt):
                s.add("dve", lambda e: e.tensor_copy(out=o32, in_=ps[4][:, :]), reads=[("ps", 4)], writes=[K("o32")])
                s.add("act", lambda e: e.activation(out=sq32, in_=o32, func=AF.Square), reads=[K("o32")], writes=[K("sq32")])
                s.add("pe", lambda e: e.matmul(ps[5][:, :], onesblk, sq32, start=True, stop=True), reads=[K("sq32"), K("c")], writes=[("ps", 5)])
                s.add("act", lambda e: e.activation(out=r32, in_=ps[5][:, :], func=AF.Sqrt, scale=1.0 / 64, bias=1e-6), reads=[("ps", 5)], writes=[K("r32")])
                s.add("dve", lambda e: e.reciprocal(out=sq32, in_=r32), reads=[K("r32")], writes=[K("sq32")])
                yi = ycnt[0] % 2
                ycnt[0] += 1
                s.add("dve", lambda e: e.scalar_tensor_tensor(out=Yb[yi], in0=o32, scalar=gvec[:, 0:1], in1=sq32, op0=ALU.mult, op1=ALU.mult),
                      reads=[K("o32"), K("sq32"), K("c")], writes=[K("Yb", yi)])
                od = dma("sp", D["yA"][sc_i][0:128, (qt % 4) * 512:(qt % 4) * 512 + 512], Yb[yi], [K("Yb", yi)], [K("yA", sc_i)], K("yout"))
                out_dmas.append(od)

            n = len(jobs)
            for step in range(n + 2):
                if step < n:
                    stage1(jobs[step])
                if 0 <= step - 1 < n:
                    stage2(jobs[step - 1])
                if 0 <= step - 2 < n:
                    stage3(jobs[step - 2])
                drip(n)
        return prep, run_sb

    PREP_STEPS = 260
    mk = [make(i) for i in range(NSC)]
    g0 = mk[0][0]()
    for _ in g0:
        pass
    for sc_i in range(NSC):
        nxt = mk[sc_i + 1][0]() if sc_i + 1 < NSC else None
        state = [nxt, 0.0]

        def drip(njobs, state=state):
            if state[0] is None:
                return
            state[1] += PREP_STEPS / float(njobs)
            while state[1] >= 1.0 and state[0] is not None:
                state[1] -= 1.0
                try:
                    next(state[0])
                except StopIteration:
                    state[0] = None
        mk[sc_i][1](drip)
        if nxt is not None:
            for _ in nxt:
                pass
        if cc is not None:
            cc(sc_i)
    return out_dmas


import numpy as np
import ml_dtypes
import concourse.bass as bass
import concourse.mybir as mybir

ALPHA = (2.0 * 2) ** 0.25
LN_EPS = 1e-5
AX = mybir.AxisListType


def emit_B(nc, s, ar, ps, D, T, lname, NE=16, st=None, last=True, cc2=None):
    L = lname
    K = lambda *a: (L,) + a
    NT = T // 128
    TT = min(512, T)
    NT4 = T // TT
    NSUB = TT // 128

    yacc = ar.take(NT * 1024, F32).rearrange("p (t d) -> p t d", t=NT)
    X1T = ar.take(8 * T).rearrange("p (c t) -> p c t", c=8)
    wslot = [ar.take(3 * 4096) for _ in range(2)]
    Wg = [w[:, 0:4096].rearrange("p (c f) -> p c f", c=8) for w in wslot]
    Wu = [w[:, 4096:8192].rearrange("p (c f) -> p c f", c=8) for w in wslot]
    Wd = [w[:, 8192:12288].rearrange("p (c f) -> p c f", c=4) for w in wslot]
    Wo = wslot[1][:, 0:8192].rearrange("p (c f) -> p c f", c=8)
    YT = [ar.take(8 * 128).rearrange("p (c t) -> p c t", c=8) for _ in range(2)]
    xt = [ar.take(1024, F32) for _ in range(2)]
    v32s = [ar.take(1024, F32) for _ in range(2)]
    lnp = [ar.take(1024, F32) for _ in range(2)]
    X1T32s = [ar.take(8 * 128, F32).rearrange("p (c t) -> p c t", c=8) for _ in range(2)]
    hT = [ar.take(4 * TT).rearrange("p (c t) -> p c t", c=4) for _ in range(2)]
    sg = [ar.take(TT, F32) for _ in range(2)]
    gates = ar.take(NT * 16, F32).rearrange("p (t e) -> p t e", t=NT)
    Wr = ar.take(8 * 16, F32).rearrange("p (c e) -> p c e", c=8)
    br = ar.take(16, F32)
    ident = ar.take(128, F32)
    stats2 = [ar.take(16, F32) for _ in range(2)]
    mv2 = [ar.take(4, F32) for _ in range(2)]
    rt2 = [[ar.take(16, F32) for _ in range(6)] for _ in range(2)]
    rs2 = [[ar.take(4, F32) for _ in range(6)] for _ in range(2)]
    xstp = ar.take(8 * 512).rearrange("p (c t) -> p c t", c=8)

    def dma(eng, out, in_, reads, writes, grp):
        return s.add(eng, lambda e: e.dma_start(out=out, in_=in_), reads=reads, writes=writes, grp=grp)

    for i, nm in enumerate(("ln1g", "ln1b")):
        dma("sp", lnp[i], D[nm], [], [K("lnp")], K("lnp"))
    for dc in range(8):
        dma("sp", Wr[:, dc, :], D["wr"][dc * 128:(dc + 1) * 128, :], [], [K("c")], K("c"))
    dma("sp", br, D["br"], [], [K("c")], K("c"))
    dma("sp", ident, D["ident32"], [], [K("c")], K("c"))
    for ec in range(8):
        dma("pool", Wo[:, ec, :], D["wo"][ec * 128:(ec + 1) * 128, :], [], [K("wslot", 1)], K("wslot", 1))

    def load_unit(u):
        e, fh = u // 2, u % 2
        sl = u % 2
        key = K("wslot", sl)
        for dc in range(8):
            dma("pool", Wg[sl][:, dc, :], D["wg"][e, dc * 128:(dc + 1) * 128, fh * 512:(fh + 1) * 512], [], [key], key)
        for dc in range(8):
            dma("pool", Wu[sl][:, dc, :], D["wu"][e, dc * 128:(dc + 1) * 128, fh * 512:(fh + 1) * 512], [], [key], key)
        for fc in range(4):
            dma("pool", Wd[sl][:, fc, :], D["wd"][e, fh * 512 + fc * 128:fh * 512 + (fc + 1) * 128, :], [], [key], key)

    load_unit(0)

    if D.get("Gall") is not None:
        s.add("sp", lambda e: e.dma_start(out=D["Gloc"], in_=D["Gall"][bass.ds(e.snap(st["reg"]), 1024), :]),
              reads=[K("G", q_) for q_ in range(4)], writes=[K("Gloc")], grp=K("Gloc"))
    def ln_stats(src, sl):
        for hh in range(2):
            s.add("dve", (lambda hh: lambda e: e.bn_stats(out=stats2[sl][:, hh * 6:(hh + 1) * 6], in_=src[:, hh * 512:(hh + 1) * 512]))(hh),
                  reads=[K("lnsrc", sl)], writes=[K("stats", sl)])
        s.add("dve", lambda e: e.bn_aggr(out=mv2[sl][:, 0:2], in_=stats2[sl][:, 0:12]), reads=[K("stats", sl)], writes=[K("mv", sl)])
        s.add("act", lambda e: e.activation(out=mv2[sl][:, 2:3], in_=mv2[sl][:, 1:2], func=AF.Sqrt, bias=LN_EPS), reads=[K("mv", sl)], writes=[K("mvb", sl)])

    def ln_apply(src, dst, gi, bi, dkey, sl):
        s.add("dve", lambda e: e.reciprocal(out=mv2[sl][:, 3:4], in_=mv2[sl][:, 2:3]), reads=[K("mvb", sl)], writes=[K("mvc", sl)])
        s.add("dve", lambda e: e.tensor_scalar(out=src, in0=src, scalar1=mv2[sl][:, 0:1], scalar2=mv2[sl][:, 3:4], op0=ALU.subtract, op1=ALU.mult),
              reads=[K("mv", sl), K("mvc", sl), K("lnsrc", sl)], writes=[K("lnsrc", sl)])
        s.add("dve", lambda e: e.tensor_tensor(out=src, in0=src, in1=lnp[gi], op=ALU.mult), reads=[K("lnsrc", sl), K("lnp")], writes=[K("lnsrc", sl)])
        s.add("dve", lambda e: e.tensor_tensor(out=dst, in0=src, in1=lnp[bi], op=ALU.add), reads=[K("lnsrc", sl), K("lnp")], writes=[dkey])

    def loads(ti):
        sl = ti % 2
        for ec in range(8):
            dma("sp", YT[sl][:, ec, :], D["Gloc"][ec * 128:(ec + 1) * 128, ti * 128:(ti + 1) * 128], [K("Gloc")], [K("YT", sl)], K("YT", sl))
        dma("sp", xt[sl], D["x"][ti * 128:(ti + 1) * 128, :], [("x2s",)], [K("xt", sl)], K("xt", sl))

    def phase1_gen(ti):
        sl = ti % 2
        pb = 4 * sl
        v32 = v32s[sl]
        X1T32 = X1T32s[sl]
        loads(ti)
        yield
        for hh in range(2):
            for ec in range(8):
                s.add("pe", (lambda hh, ec: lambda e: e.matmul(ps[pb + hh][:, :], YT[sl][:, ec, :], Wo[:, ec, hh * 512:(hh + 1) * 512],
                                                               start=(ec == 0), stop=(ec == 7)))(hh, ec),
                      reads=[K("YT", sl), K("wslot", 1)], writes=[("ps", pb + hh)])
            s.add("dve", (lambda hh: lambda e: e.scalar_tensor_tensor(out=v32[:, hh * 512:(hh + 1) * 512], in0=xt[sl][:, hh * 512:(hh + 1) * 512],
                                                                      scalar=float(ALPHA), in1=ps[pb + hh][:, :], op0=ALU.mult, op1=ALU.add))(hh),
                  reads=[K("xt", sl), ("ps", pb + hh)], writes=[K("lnsrc", sl)])
        yield
        ln_stats(v32, sl)
        yield
        ln_apply(v32, xt[sl], 0, 1, K("xt", sl), sl)
        s.add("act", lambda e: e.activation(out=yacc[:, ti, :], in_=xt[sl], func=AF.Copy, scale=float(ALPHA)),
              reads=[K("xt", sl)], writes=[K("yacc", ti)])
        yield
        for q in range(2):
            for kq in range(4):
                dc = q * 4 + kq
                s.add("pe", (lambda q, kq, dc: lambda e: e.transpose(out=ps[pb + 2 + q][:, kq * 128:(kq + 1) * 128], in_=xt[sl][:, dc * 128:(dc + 1) * 128], identity=ident))(q, kq, dc),
                      reads=[K("xt", sl), K("c")], writes=[("ps", pb + 2 + q)])
            yield
            s.add("dve", (lambda q: lambda e: e.tensor_copy(out=X1T32[:, q * 4:(q + 1) * 4, :], in_=ps[pb + 2 + q][:, :].rearrange("p (c t) -> p c t", c=4)))(q),
                  reads=[("ps", pb + 2 + q)], writes=[K("X1T32", sl, q)])
            s.add("act", (lambda q: lambda e: e.activation(out=X1T[:, q * 4:(q + 1) * 4, ti * 128:(ti + 1) * 128], in_=X1T32[:, q * 4:(q + 1) * 4, :], func=AF.Copy))(q),
                  reads=[K("X1T32", sl, q)], writes=[K("X1T", ti)])
        yield
        for dc in range(8):
            s.add("pe", (lambda dc: lambda e: e.matmul(ps[pb][:, 0:16], X1T32[:, dc, :], Wr[:, dc, :], start=(dc == 0), stop=(dc == 7)))(dc),
                  reads=[K("X1T32", sl, dc // 4), K("c")], writes=[("ps", pb)])
        yield
        lg, ex, eq, p2, selm, msk = rt2[sl]
        v1, v2, gs, gsel, gmx, den = rs2[sl]
        R = K("rt", sl)
        s.add("dve", lambda e: e.tensor_tensor(out=lg, in0=ps[pb][:, 0:16], in1=br, op=ALU.add), reads=[("ps", pb), K("c")], writes=[R])
        s.add("dve", lambda e: e.tensor_reduce(out=gmx[:, 0:1], in_=lg, axis=AX.X, op=ALU.max), reads=[R], writes=[R])
        s.add("dve", lambda e: e.tensor_scalar(out=lg, in0=lg, scalar1=gmx[:, 0:1], scalar2=None, op0=ALU.subtract), reads=[R], writes=[R])
        s.add("act", lambda e: e.activation(out=ex, in_=lg, func=AF.Exp), reads=[R], writes=[K("rtb", sl)])
        yield
        R2 = K("rtb", sl)
        ex3 = ex.rearrange("p (g k) -> p g k", g=4)
        eq3 = eq.rearrange("p (g k) -> p g k", g=4)
        p23 = p2.rearrange("p (g k) -> p g k", g=4)
        sel3 = selm.rearrange("p (g k) -> p g k", g=4)
        msk3 = msk.rearrange("p (g k) -> p g k", g=4)
        s.add("dve", lambda e: e.tensor_reduce(out=v1, in_=ex3, axis=AX.X, op=ALU.max), reads=[R2], writes=[R2])
        s.add("dve", lambda e: e.tensor_tensor(out=eq3, in0=ex3, in1=v1.unsqueeze(2).to_broadcast([128, 4, 4]), op=ALU.is_equal), reads=[R2], writes=[R2])
        s.add("dve", lambda e: e.scalar_tensor_tensor(out=p2, in0=eq, scalar=-2.0, in1=ex, op0=ALU.mult, op1=ALU.add), reads=[R2], writes=[R2])
        s.add("dve", lambda e: e.tensor_reduce(out=v2, in_=p23, axis=AX.X, op=ALU.max), reads=[R2], writes=[R2])
        s.add("dve", lambda e: e.tensor_tensor(out=gs, in0=v1, in1=v2, op=ALU.add), reads=[R2], writes=[R2])
        s.add("dve", lambda e: e.tensor_reduce(out=gmx[:, 1:2], in_=gs, axis=AX.X, op=ALU.max), reads=[R2], writes=[R2])
        yield
        s.add("dve", lambda e: e.tensor_scalar(out=gsel, in0=gs, scalar1=gmx[:, 1:2], scalar2=None, op0=ALU.is_equal), reads=[R2], writes=[R2])
        s.add("dve", lambda e: e.tensor_tensor(out=sel3, in0=ex3, in1=v2.unsqueeze(2).to_broadcast([128, 4, 4]), op=ALU.is_ge), reads=[R2], writes=[R2])
        s.add("dve", lambda e: e.tensor_tensor(out=msk3, in0=sel3, in1=gsel.unsqueeze(2).to_broadcast([128, 4, 4]), op=ALU.mult), reads=[R2], writes=[R2])
        s.add("dve", lambda e: e.tensor_tensor(out=v1, in0=gs, in1=gsel, op=ALU.mult), reads=[R2], writes=[R2])
        s.add("dve", lambda e: e.tensor_reduce(out=den[:, 0:1], in_=v1, axis=AX.X, op=ALU.add), reads=[R2], writes=[R2])
        s.add("dve", lambda e: e.reciprocal(out=den[:, 1:2], in_=den[:, 0:1]), reads=[R2], writes=[R2])
        s.add("dve", lambda e: e.tensor_tensor(out=msk, in0=msk, in1=ex, op=ALU.mult), reads=[R2], writes=[R2])
        s.add("dve", lambda e: e.tensor_scalar(out=gates[:, ti, :], in0=msk, scalar1=den[:, 1:2], scalar2=None, op0=ALU.mult),
              reads=[R2], writes=[K("gates", ti)])

    def run_two(gens):
        gens = list(gens)
        active = [None, None]
        nxt = [0]

        def refill(slot):
            for j in range(nxt[0], len(gens)):
                if gens[j] is not None and gens[j][0] % 2 == slot:
                    g = gens[j][1]
                    gens[j] = None
                    return g
            return None
        active[0] = refill(0)
        active[1] = refill(1)
        for _ in range(3):
            if active[0] is not None:
                try:
                    next(active[0])
                except StopIteration:
                    active[0] = refill(0)
        while active[0] is not None or active[1] is not None:
            for slot in (0, 1):
                if active[slot] is None:
                    continue
                try:
                    next(active[slot])
                except StopIteration:
                    active[slot] = refill(slot)

    run_two([(ti, phase1_gen(ti)) for ti in range(NT)])
    dma("sp", lnp[0], D["ln2g"], [], [K("lnp")], K("lnp"))
    dma("sp", lnp[1], D["ln2b"], [], [K("lnp")], K("lnp"))

    NU = NE * 2
    hcnt = [0]
    for u in range(NU):
        e_, fh = u // 2, u % 2
        sl = u % 2
        if u + 1 < NU:
            load_unit(u + 1)
        wkey = K("wslot", sl)
        for t4 in range(NT4):
            hs = hcnt[0] % 2
            hcnt[0] += 1
            tok = slice(t4 * TT, (t4 + 1) * TT)
            for fc in range(4):
                gi = 0 + (fc % 2)
                ui = 2 + (fc % 2)
                for dc in range(8):
                    s.add("pe", (lambda sl, dc, fc, gi, tok: lambda e: e.matmul(ps[gi][:, 0:TT], Wg[sl][:, dc, fc * 128:(fc + 1) * 128], X1T[:, dc, tok],
                                                                               start=(dc == 0), stop=(dc == 7)))(sl, dc, fc, gi, tok),
                          reads=[wkey] + [K("X1T", t4 * NSUB + i) for i in range(NSUB)], writes=[("ps", gi)])
                for dc in range(8):
                    s.add("pe", (lambda sl, dc, fc, ui, tok: lambda e: e.matmul(ps[ui][:, 0:TT], Wu[sl][:, dc, fc * 128:(fc + 1) * 128], X1T[:, dc, tok],
                                                                               start=(dc == 0), stop=(dc == 7)))(sl, dc, fc, ui, tok),
                          reads=[wkey] + [K("X1T", t4 * NSUB + i) for i in range(NSUB)], writes=[("ps", ui)])
                si = fc % 2
                s.add("act", (lambda gi, si: lambda e: e.activation(out=sg[si][:, 0:TT], in_=ps[gi][:, 0:TT], func=AF.Silu))(gi, si),
                      reads=[("ps", gi)], writes=[K("sg", si)])
                s.add("dve", (lambda hs, fc, si, ui: lambda e: e.tensor_tensor(out=hT[hs][:, fc, :], in0=sg[si][:, 0:TT], in1=ps[ui][:, 0:TT], op=ALU.mult))(hs, fc, si, ui),
                      reads=[K("sg", si), ("ps", ui)], writes=[K("hT", hs, fc)])
            for sub in range(NSUB):
                ti = t4 * NSUB + sub
                for hh in range(2):
                    di = 4 + ((sub * 2 + hh) % 2)
                    for fc in range(4):
                        s.add("pe", (lambda sl, hs, fc, sub, hh, di: lambda e: e.matmul(ps[di][:, :], hT[hs][:, fc, sub * 128:(sub + 1) * 128], Wd[sl][:, fc, hh * 512:(hh + 1) * 512],
                                                                                         start=(fc == 0), stop=(fc == 3)))(sl, hs, fc, sub, hh, di),
                              reads=[wkey, K("hT", hs, fc)], writes=[("ps", di)])
                    s.add("dve", (lambda ti, hh, di, e_: lambda e: e.scalar_tensor_tensor(out=yacc[:, ti, hh * 512:(hh + 1) * 512], in0=ps[di][:, :], scalar=gates[:, ti, e_:e_ + 1],
                                                                                          in1=yacc[:, ti, hh * 512:(hh + 1) * 512], op0=ALU.mult, op1=ALU.add))(ti, hh, di, e_),
                          reads=[("ps", di), K("gates", ti), K("yacc", ti)], writes=[K("yacc", ti)])

    outs = []

    def ln2_gen(ti):
        sl = ti % 2
        pb = 4 * sl
        v32 = v32s[sl]
        s.add("dve", lambda e: e.tensor_copy(out=v32, in_=yacc[:, ti, :]), reads=[K("yacc", ti)], writes=[K("lnsrc", sl)])
        ln_stats(v32, sl)
        yield
        ln_apply(v32, xt[sl], 0, 1, K("xt", sl), sl)
        yield
        if last:
            outs.append(dma("sp", D["x2"][ti * 128:(ti + 1) * 128, :], xt[sl], [K("xt", sl)], [], K("x2out")))
        else:
            outs.append(dma("sp", D["x2s"][ti * 128:(ti + 1) * 128, :], xt[sl], [K("xt", sl)], [("x2s",)], K("x2out")))
            for q in range(2):
                for kq in range(4):
                    dc = q * 4 + kq
                    s.add("pe", (lambda q, kq, dc: lambda e: e.transpose(out=ps[pb + 2 + q][:, kq * 128:(kq + 1) * 128], in_=xt[sl][:, dc * 128:(dc + 1) * 128], identity=ident))(q, kq, dc),
                          reads=[K("xt", sl), K("c")], writes=[("ps", pb + 2 + q)])
            yield
            tq = ti % 4
            for q in range(2):
                s.add("dve", (lambda q: lambda e: e.tensor_copy(out=xstp[:, q * 4:(q + 1) * 4, tq * 128:(tq + 1) * 128], in_=ps[pb + 2 + q][:, :].rearrange("p (c t) -> p c t", c=4)))(q),
                      reads=[("ps", pb + 2 + q)], writes=[K("xstp", ti)])

    if last:
        run_two([(ti, ln2_gen(ti)) for ti in range(NT)])
    else:
        for p in range(NT // 4):
            run_two([(ti, ln2_gen(ti)) for ti in range(4 * p, 4 * p + 4)])
            for dc in range(8):
                outs.append(dma("sp", D["X2T"][p][dc * 128:(dc + 1) * 128, :], xstp[:, dc, :], [K("xstp", 4 * p + i) for i in range(4)],
                                [("X2T", p)] + [K("xstp", 4 * p + 4 + i) for i in range(4)], K("x2out")))
            if cc2 is not None:
                cc2(p)
    return outs

I32 = mybir.dt.int32
_GROUPS = [[0, 1, 2, 3], [4, 5, 6, 7]]


def _build_fused(S=8192, T=2048, NE=16, depth=2):
    nc = bass.Bass("TRN2", target_bir_lowering=False, num_devices=8)
    D = {}

    def inp(name, shape, dt=F32):
        D[name] = nc.dram_tensor(name, shape, dt, kind="ExternalInput").ap()
    inp("xT", [1024, S])
    inp("xres", [T, 1024])
    inp("qi", [1, 1], I32)
    inp("wA", [depth * 1024, 768])
    inp("gA", [depth * 256, 1])
    inp("biasm", [3, 2, 128, 256])
    C = consts_A()
    for k, v in C.items():
        inp(k, list(v.shape), BF16 if v.dtype == ml_dtypes.bfloat16 else F32)
    inp("wo", [depth * 1024, 1024])
    inp("lnp", [depth * 4 * 128, 1024])
    inp("wr", [1024, 16])
    inp("br", [128, 16])
    inp("ident32", [128, 128])
    for nm in ("wg", "wu", "wd"):
        inp(nm, [depth * NE, 1024, 1024])
    D["out"] = nc.dram_tensor("out", [T, 1024], F32, kind="ExternalOutput").ap()
    yA = [[nc.dram_tensor("yA%d_%d" % (l, p), [256, T], BF16).ap() for p in range(4)] for l in range(depth)]
    Gall = [nc.dram_tensor("Gall%d" % l, [4096, T], BF16).ap() for l in range(depth)]
    Gloc = [nc.dram_tensor("Gloc%d" % l, [1024, T], BF16).ap() for l in range(depth)]
    X2T = [nc.dram_tensor("X2T%d" % p, [1024, 512], BF16).ap() for p in range(4)]
    GX = [nc.dram_tensor("GX%d" % p, [4096, 512], BF16).ap() for p in range(4)]
    x2s = nc.dram_tensor("x2s", [T, 1024], F32).ap()
    with ExitStack() as es:
        arena = es.enter_context(nc.sbuf_tensor("arena", [128, 103 * 1024], BF16))
        qs = es.enter_context(nc.sbuf_tensor("qs", [1, 1], I32))
        reg = es.enter_context(nc.sync.register("qreg"))
        ps = [es.enter_context(nc.psum_tensor("ps%d" % i, [128, 512], F32)) for i in range(8)]
        s = Sched(nc)
        st = {}
        s.add("sp", lambda e: e.dma_start(out=qs[:, :], in_=D["qi"]), writes=[("qs",)], grp=("qs",))

        def ld(e):
            ins = e.reg_load(reg, qs[0:1, 0:1])
            st["reg"] = reg
            return ins
        s.add("sp", ld, reads=[("qs",)], writes=[("qreg",)])
        outs = []
        ccn = [0]
        for l in range(depth):
            last = (l == depth - 1)
            DA = dict(D)
            DA["w"] = D["wA"][l * 1024:(l + 1) * 1024, :]
            DA["g"] = D["gA"][l * 256:(l + 1) * 256, :]
            DA["yA"] = yA[l]
            DA["GX"] = GX

            def cc1(p, l=l):
                ccn[0] += 1
                s.add("pool", lambda e: e.collective_compute("AllGather", ALU.bypass, replica_groups=_GROUPS, ins=[yA[l][p]],
                                                             outs=[Gall[l][p * 1024:(p + 1) * 1024, :]]),
                      reads=[("A%d" % l, "yA", p)], writes=[("B%d" % l, "G", p)], grp=("cc", ccn[0]), inc=1)
            ar = Arena(arena)
            emit_A(nc, s, ar, ps, DA, S, l == 0, "A%d" % l, cc=cc1)
            s.fence()
            DB = dict(D)
            DB["Gall"] = Gall[l]
            DB["Gloc"] = Gloc[l]
            DB["x"] = D["xres"] if l == 0 else x2s
            DB["wo"] = D["wo"][l * 1024:(l + 1) * 1024, :]
            for i, nm in enumerate(("ln1g", "ln1b", "ln2g", "ln2b")):
                DB[nm] = D["lnp"][(l * 4 + i) * 128:(l * 4 + i + 1) * 128, :]
            for nm in ("wg", "wu", "wd"):
                DB[nm] = D[nm][l * NE:(l + 1) * NE]
            DB["x2"] = D["out"]
            DB["x2s"] = x2s
            DB["X2T"] = X2T

            def cc2(p):
                ccn[0] += 1
                s.add("pool", lambda e: e.collective_compute("AllGather", ALU.bypass, replica_groups=_GROUPS, ins=[X2T[p]], outs=[GX[p]]),
                      reads=[("X2T", p)], writes=[("GX", p)], grp=("cc", ccn[0]), inc=1)
            ar = Arena(arena)
            o = emit_B(nc, s, ar, ps, DB, T, "B%d" % l, NE=NE, st=st, last=last, cc2=None if last else cc2)
            if last:
                outs = o
            else:
                s.fence()
        keys = s.emit(final_wait_ops=outs)
        sems = {k: es.enter_context(nc.semaphore("s%d" % i)) for i, k in enumerate(keys)}
        s.run(sems, final_wait_ops=outs)
    return nc, C, len(s.ops), len(keys)


def kernel(x, w_in, g_sb, g_dil, w_out, ln1_g, ln1_b, ln2_g, ln2_b, rel_bias, w_router, b_router, w_gate, w_up, w_down):
    x = np.asarray(x, np.float32)
    B, S, Dm = x.shape
    depth = w_in.shape[0]
    T = 2048
    NE = w_gate.shape[1]
    nc, C, _, _ = _build_fused(S, T, NE, depth)
    f32 = lambda v: np.asarray(v, np.float32)
    rep = lambda v: np.broadcast_to(f32(v)[None, :], (128, v.shape[0]))
    rel_bias = f32(rel_bias)
    perm = np.concatenate([np.concatenate([np.arange(128 * j, 128 * j + 128), 512 + np.arange(128 * j, 128 * j + 128)]) for j in range(4)])
    wo = np.ascontiguousarray(np.concatenate([f32(w_out[l])[perm, :] for l in range(depth)], axis=0))
    lnp = np.ascontiguousarray(np.concatenate([rep(p[l]) for l in range(depth) for p in (ln1_g, ln1_b, ln2_g, ln2_b)], axis=0))
    shared = {"wo": wo, "lnp": lnp, "wr": np.ascontiguousarray(f32(w_router)), "br": np.ascontiguousarray(rep(b_router)),
              "ident32": np.eye(128, dtype=np.float32),
              "wg": f32(w_gate).reshape(depth * NE, 1024, 1024), "wu": f32(w_up).reshape(depth * NE, 1024, 1024),
              "wd": f32(w_down).reshape(depth * NE, 1024, 1024)}
    shared.update(C)
    xTs = [np.ascontiguousarray(x[b].T) for b in range(B)]
    in_maps = []
    for c in range(8):
        b, j = c // 4, c % 4
        wA = []
        gA = []
        for l in range(depth):
            wl = f32(w_in[l])
            wA.append(np.concatenate([wl[:, o + 128 * j:o + 128 * j + 128] for o in (0, 512, 1536, 2048, 1024, 2560)], axis=1))
            gA.append(np.concatenate([f32(g_sb[l])[128 * j:128 * j + 128], f32(g_dil[l])[128 * j:128 * j + 128]])[:, None])
        m = {"xT": xTs[b], "xres": np.ascontiguousarray(x[b, j * T:(j + 1) * T, :]), "qi": np.array([[1024 * j]], np.int32),
             "wA": np.ascontiguousarray(np.concatenate(wA, axis=0)), "gA": np.ascontiguousarray(np.concatenate(gA, axis=0)),
             "biasm": dil_bias_tables(rel_bias[:, 2 * j:2 * j + 2])}
        m.update(shared)
        in_maps.append(m)
    res = run_bass_kernel_spmd(nc, in_maps, core_ids=list(range(8)))
    out = np.zeros((B, S, Dm), np.float32)
    for c in range(8):
        b, j = c // 4, c % 4
        out[b, j * T:(j + 1) * T, :] = np.asarray(res.results[c]["out"], np.float32)
    return out
```

```python
from contextlib import ExitStack
from concourse.bass_utils import run_bass_kernel_spmd
import numpy as np
import concourse.bass as bass
import concourse.mybir as mybir

F32 = mybir.dt.float32
BF16 = mybir.dt.bfloat16
AF = mybir.ActivationFunctionType
ALU = mybir.AluOpType

COMPUTE = ("pe", "act", "dve", "pool")


class Sched:
    def __init__(self, nc):
        self.nc = nc
        self.ops = []
        self.last_writer = {}
        self.readers = {}
        self.dma_groups = {}
        self.fence_deps = set()
        self.fence_pending = set()

    def fence(self):
        last = {}
        for i, op in enumerate(self.ops):
            k = ("dma", op["grp"]) if op["dma"] else ("eng", op["eng"])
            last[k] = i
        self.fence_deps = set(last.values())
        self.fence_pending = {"pe", "act", "dve", "pool", "sp"}

    def add(self, eng, fn, reads=(), writes=(), grp=None, inc=16):
        idx = len(self.ops)
        deps = set()
        if eng in self.fence_pending:
            deps |= self.fence_deps
            self.fence_pending.discard(eng)
        for b in reads:
            w = self.last_writer.get(b)
            if w is not None:
                deps.add(w)
        for b in writes:
            w = self.last_writer.get(b)
            if w is not None:
                deps.add(w)
            for r in self.readers.get(b, ()):
                deps.add(r)
        deps.discard(idx)
        is_dma = grp is not None
        if eng == "pe":
            deps = {d for d in deps if not (self.ops[d]["eng"] == "pe" and not self.ops[d]["dma"])}
        op = dict(eng=eng, fn=fn, deps=deps, dma=is_dma, grp=grp, needed=False, inc=(inc if is_dma else 1))
        self.ops.append(op)
        for b in reads:
            self.readers.setdefault(b, []).append(idx)
        for b in writes:
            self.last_writer[b] = idx
            self.readers[b] = []
        return idx

    def emit(self, final_wait_ops=()):
        nc = self.nc
        ops = self.ops
        for op in ops:
            for d in op["deps"]:
                ops[d]["needed"] = True
        for d in final_wait_ops:
            ops[d]["needed"] = True
        sem_names = []
        counters = {}
        for i, op in enumerate(ops):
            if not op["needed"]:
                continue
            key = ("dma", op["grp"]) if op["dma"] else ("eng", op["eng"])
            if key not in counters:
                counters[key] = 0
                sem_names.append(key)
            counters[key] += op["inc"]
            op["sem"] = key
            op["val"] = counters[key]
        self.sem_keys = sem_names
        return sem_names

    def run(self, sems, final_wait_ops=()):
        nc = self.nc
        ops = self.ops
        per_eng = {}
        for i, op in enumerate(ops):
            per_eng.setdefault(op["eng"], []).append(i)

        def body(engname, eng):
            known = {}
            for i in per_eng.get(engname, []):
                op = ops[i]
                need = {}
                for d in op["deps"]:
                    p = ops[d]
                    k = p["sem"]
                    need[k] = max(need.get(k, 0), p["val"])
                for k, v in need.items():
                    if known.get(k, 0) >= v:
                        continue
                    eng.wait_ge(sems[k], v)
                    known[k] = v
                ins = op["fn"](eng)
                if op["needed"]:
                    ins.then_inc(sems[op["sem"]], op["inc"])
            if engname == "sp":
                fin = {}
                for d in final_wait_ops:
                    p = ops[d]
                    fin[p["sem"]] = max(fin.get(p["sem"], 0), p["val"])
                for k, v in fin.items():
                    if known.get(k, 0) < v:
                        eng.wait_ge(sems[k], v)
                        known[k] = v

        with nc.Block() as block:
            @block.sync
            def _(e):
                body("sp", e)

            @block.tensor
            def _(e):
                body("pe", e)

            @block.scalar
            def _(e):
                body("act", e)

            @block.vector
            def _(e):
                body("dve", e)

            @block.gpsimd
            def _(e):
                body("pool", e)


import math
import numpy as np
import ml_dtypes
import concourse.bass as bass
import concourse.mybir as mybir

NEG = -30000.0
SKIP = ['dveonly']
SC = 2048


def t5_bucket_np(dist):
    max_exact = 16
    d = np.maximum(dist, 0)
    large = max_exact + (np.log(np.maximum(d, 1).astype(np.float32) / np.float32(max_exact))
                         / np.float32(math.log(2048 / max_exact)) * np.float32(32 - max_exact)).astype(np.int32)
    large = np.minimum(large, 31)
    return np.where(d < max_exact, d, large)


def consts_A():
    j = np.arange(128)[:, None]
    s = np.arange(128)[None, :]
    negtri = np.where(j >= s, -1.0, 0.0).astype(ml_dtypes.bfloat16)
    negones = np.full((128, 128), -1.0, dtype=ml_dtypes.bfloat16)
    ident = np.eye(128, dtype=ml_dtypes.bfloat16)
    t = np.arange(512)[None, :]
    sbmask = np.stack([np.where(128 * a + j >= t, NEG, 0.0) for a in range(4)], 0).astype(ml_dtypes.bfloat16)
    onesblk = np.zeros((128, 128), np.float32)
    onesblk[:64, :64] = 1.0
    onesblk[64:, 64:] = 1.0
    sel65 = np.zeros((65, 64), np.float32)
    sel65[64, :] = 1.0
    ones65 = np.zeros((65, 64), np.float32)
    ones65[:64, :] = 1.0
    return dict(negtri=negtri, negones=negones, ident=ident, sbmask=sbmask, onesblk=onesblk, sel65=sel65, ones65=ones65)


def dil_bias_tables(rel_bias_heads):
    kj = np.arange(128)[:, None]
    qi = np.arange(128)[None, :]
    out = np.zeros((3, 2, 128, 256), np.float32)
    for ri, r in enumerate((1, 4, 16)):
        steps_cur = qi - kj
        steps_prev = qi - kj + 128
        b_cur = t5_bucket_np(np.maximum(steps_cur, 0) * r)
        b_prev = t5_bucket_np(np.maximum(steps_prev, 0) * r)
        for h in range(2):
            cur = np.where(steps_cur >= 0, rel_bias_heads[b_cur, h], np.float32(NEG))
            prev = np.where(steps_prev <= 128, rel_bias_heads[b_prev, h], np.float32(NEG))
            out[ri, h, :, :128] = prev
            out[ri, h, :, 128:] = cur
    return out


class Arena:
    def __init__(self, ap_bf16):
        self.ap = ap_bf16
        self.off = 0
        self.total = ap_bf16.shape[1]

    def take(self, nelem, dtype=BF16):
        nb = nelem * (4 if dtype == F32 else 2)
        nb = (nb + 63) // 64 * 64
        n16 = nb // 2
        assert self.off + n16 <= self.total, ("arena overflow", self.off, n16, self.total)
        v = self.ap[:, self.off:self.off + n16]
        self.off += n16
        if dtype == F32:
            return v.bitcast(F32)[:, :nelem]
        return v[:, :nelem]


def emit_A(nc, s, ar, ps, D, S, src_is_f32, lname, stop=99, cc=None):
    NSC = S // SC
    NB = S // 128
    L = lname
    K = lambda *a: (L,) + a

    Wb = ar.take(8 * 768).rearrange("p (f c) -> p f c", f=8)
    XT = ar.take(8 * SC).rearrange("p (f t) -> p f t", f=8)
    QTs = ar.take(SC)
    QTsB = ar.take(SC)
    QTd = ar.take(SC)
    KTs = ar.take(S)
    KTd = ar.take(S)
    Vs = ar.take(NB * 128).rearrange("p (b c) -> p b c", c=128)
    NW1 = NB
    Vd1 = ar.take(NW1 * 132).rearrange("p (b h c) -> p b h c", h=2, c=66)
    Vd4 = ar.take(32 * 132).rearrange("p (b h c) -> p b h c", h=2, c=66)
    Vd16 = ar.take(32 * 132).rearrange("p (b h c) -> p b h c", h=2, c=66)
    negtri = ar.take(128)
    negones = ar.take(128)
    ident = ar.take(128)
    sbmask = ar.take(4 * 512).rearrange("p (a t) -> p a t", a=4)
    onesblk = ar.take(128, F32)
    sel65 = ar.take(64, F32)
    ones65 = ar.take(64, F32)
    gvec = ar.take(4, F32)
    biasm16 = ar.take(6 * 256).rearrange("p (r h c) -> p r h c", r=3, h=2)
    E32 = [ar.take(512, F32) for _ in range(2)]
    Lp16 = [ar.take(512) for _ in range(4)]
    LA32 = [ar.take(512, F32) for _ in range(2)]
    A16 = [ar.take(512) for _ in range(4)]
    ncar = [ar.take(512, F32) for _ in range(2)]
    P16 = [ar.take(512) for _ in range(6)]
    acc = [ar.take(SC, F32) for _ in range(2)]
    o32 = ar.take(512, F32)
    sq32 = ar.take(512, F32)
    r32 = ar.take(512, F32)
    Yb = [ar.take(512) for _ in range(2)]

    def dma(eng, out, in_, reads, writes, grp):
        return s.add(eng, lambda e: e.dma_start(out=out, in_=in_), reads=reads, writes=writes, grp=grp)

    for fc in range(8):
        dma("pool", Wb[:, fc, :], D["w"][fc * 128:(fc + 1) * 128, :], [], [K("W")], K("W"))
    dma("sp", negtri, D["negtri"], [], [K("c")], K("c"))
    dma("sp", negones, D["negones"], [], [K("c")], K("c"))
    dma("sp", ident, D["ident"], [], [K("c")], K("c"))
    for a in range(4):
        dma("sp", sbmask[:, a, :], D["sbmask"][a], [], [K("c")], K("c"))
    dma("sp", onesblk, D["onesblk"], [], [K("c")], K("c"))
    dma("sp", sel65[0:65, :], D["sel65"], [], [K("c")], K("c"))
    dma("sp", ones65[0:65, :], D["ones65"], [], [K("c")], K("c"))
    dma("sp", gvec[:, 0:1], D["g"][0:128, :], [], [K("c")], K("c"))
    dma("sp", gvec[0:64, 1:2], D["g"][128:192, :], [], [K("c")], K("c"))
    dma("sp", gvec[0:64, 2:3], D["g"][192:256, :], [], [K("c")], K("c"))
    for ri in range(3):
        for h in range(2):
            dma("sp", acc[0][:, (ri * 2 + h) * 256:(ri * 2 + h + 1) * 256], D["biasm"][ri, h], [], [K("acc", 0)], K("bm"))
    s.add("act", lambda e: e.activation(out=biasm16.rearrange("p r h c -> p (r h c)"), in_=acc[0][:, 0:1536], func=AF.Exp), reads=[K("acc", 0)], writes=[K("ebias")])
    for vb in (Vd1, Vd4, Vd16):
        s.add("dve", (lambda vb: lambda e: e.memset(vb[:, :, :, 64:65], 1.0))(vb), writes=[K("vones")])

    psX = [ps[6], ps[7]]
    def dummy_out():
        return [dma("sp", D["yT"][0:128, 0:512], QTs[:, 0:512], [K("c"), K("W"), K("XT"), K("vones"), K("QTs"), K("QTd"), K("Vd4", 0), K("Vd16", 0), K("Vs", 0)], [], K("yout"))]
    if stop == 0:
        return dummy_out()
    xcnt = [0]

    def nextX():
        i = xcnt[0] % 2
        xcnt[0] += 1
        return i

    evac_cnt = [0]

    def evac(out, in_, reads, writes, scale=None):
        evac_cnt[0] += 1
        if evac_cnt[0] % 2 == 0 or 'dveonly' in SKIP:
            if scale is None:
                return s.add("dve", lambda e: e.tensor_copy(out=out, in_=in_), reads=reads, writes=writes)
            return s.add("dve", lambda e: e.tensor_scalar(out=out, in0=in_, scalar1=float(scale), scalar2=None, op0=ALU.mult),
                         reads=reads, writes=writes)
        sc = 1.0 if scale is None else float(scale)
        return s.add("act", lambda e: e.activation(out=out, in_=in_, func=AF.Copy, scale=sc), reads=reads, writes=writes)

    out_dmas = []
    ycnt = [0]

    def ss(start, n, step):
        return slice(start, start + (n - 1) * step + 1, step)

    QTs2 = [QTs, QTsB]

    def make(sc_i):
        t0 = sc_i * SC

        def load_xt(sci):
            for fc in range(8):
                if src_is_f32:
                    dma("pool", XT[:, fc, :], D["xT"][fc * 128:(fc + 1) * 128, sci * SC:(sci + 1) * SC], [], [K("XT")], K("XT"))
                else:
                    for p in range(4):
                        dma("sp", XT[:, fc, p * 512:(p + 1) * 512], D["GX"][p][sci * 1024 + fc * 128:sci * 1024 + (fc + 1) * 128, :], [("GX", p)], [K("XT")], K("XT"))

        def prep():
            if sc_i == 0:
                load_xt(0)
            for c in range(4):
                tl = c * 512
                for kind, col0, dst, scale in (("qs", 0, QTs2[sc_i % 2][:, tl:tl + 512], 0.125), ("ks", 128, KTs[:, t0 + tl:t0 + tl + 512], None),
                                               ("qd", 256, QTd[:, tl:tl + 512], 0.125), ("kd", 384, KTd[:, t0 + tl:t0 + tl + 512], None)):
                    yield
                    xi = nextX()
                    for fc in range(8):
                        s.add("pe", (lambda xi, fc, col0, tl: lambda e: e.matmul(psX[xi][:, :], Wb[:, fc, col0:col0 + 128], XT[:, fc, tl:tl + 512],
                                                                                 start=(fc == 0), stop=(fc == 7)))(xi, fc, col0, tl),
                              reads=[K("W"), K("XT")], writes=[("ps", 6 + xi)])
                    wkey = {"qs": K("QTs", sc_i % 2), "ks": K("KTs", sc_i), "qd": K("QTd"), "kd": K("KTd", sc_i)}[kind]
                    evac(dst, psX[xi][:, :], [("ps", 6 + xi)], [wkey], scale)
                for sub in range(4 if 'vnat' not in SKIP else 0):
                    tt = tl + sub * 128
                    blk = (t0 + tt) // 128
                    yield
                    xi = nextX()
                    for fc in range(8):
                        s.add("pe", (lambda xi, fc, tt: lambda e: e.matmul(psX[xi][:, 0:256], XT[:, fc, tt:tt + 128], Wb[:, fc, 512:768],
                                                                           start=(fc == 0), stop=(fc == 7)))(xi, fc, tt),
                              reads=[K("W"), K("XT")], writes=[("ps", 6 + xi)])
                    if 'evs' not in SKIP: evac(Vs[:, blk, :], psX[xi][:, 0:128], [("ps", 6 + xi)], [K("Vs", blk)])
                    for hh in range(2 if 'evd1' not in SKIP else 0):
                        evac(Vd1[:, blk, hh, 0:64], psX[xi][:, 128 + 64 * hh:192 + 64 * hh], [("ps", 6 + xi)], [K("Vd1", blk)])
                n4 = 4 * sc_i + c
                yield
                xi = nextX()
                for cls in range(4 if 'v4' not in SKIP else 0):
                    for fc in range(8):
                        s.add("pe", (lambda xi, fc, cls, tl: lambda e: e.matmul(psX[xi][:, cls * 128:(cls + 1) * 128],
                                                                                XT[:, fc, ss(tl + cls, 128, 4)], Wb[:, fc, 640:768],
                                                                                start=(fc == 0), stop=(fc == 7)))(xi, fc, cls, tl),
                              reads=[K("W"), K("XT")], writes=[("ps", 6 + xi)])
                slot0 = (n4 % 8) * 4
                for kk_ in range(4 if 'v4' not in SKIP else 0):
                    for hh in range(2):
                        evac(Vd4[:, slot0 + kk_, hh, 0:64], psX[xi][:, kk_ * 128 + 64 * hh:kk_ * 128 + 64 * hh + 64], [("ps", 6 + xi)], [K("Vd4", n4 % 8)])
            for c4 in range(4 if 'v16' not in SKIP else 0):
                yield
                xi = nextX()
                for k in range(4):
                    cls = c4 * 4 + k
                    for fc in range(8):
                        s.add("pe", (lambda xi, fc, cls, k: lambda e: e.matmul(psX[xi][:, k * 128:(k + 1) * 128],
                                                                               XT[:, fc, ss(cls, 128, 16)], Wb[:, fc, 640:768],
                                                                               start=(fc == 0), stop=(fc == 7)))(xi, fc, cls, k),
                              reads=[K("W"), K("XT")], writes=[("ps", 6 + xi)])
                slot0 = (sc_i % 2) * 16 + c4 * 4
                for kk_ in range(4 if 'v16' not in SKIP else 0):
                    for hh in range(2):
                        evac(Vd16[:, slot0 + kk_, hh, 0:64], psX[xi][:, kk_ * 128 + 64 * hh:kk_ * 128 + 64 * hh + 64], [("ps", 6 + xi)], [K("Vd16", sc_i % 2)])

            yield
            if sc_i + 1 < NSC:
                load_xt(sc_i + 1)
            yield
            groups = []
            for h in range(2):
                for ri, r in enumerate((1, 4, 16)):
                    nblk_sc = SC // (128 * r)
                    for c in range(r):
                        n0 = sc_i * nblk_sc
                        for g0 in range(n0, n0 + nblk_sc, 4):
                            blks = list(range(g0, min(g0 + 4, n0 + nblk_sc)))
                            groups.append(dict(h=h, ri=ri, r=r, c=c, g0=g0, blks=blks, gi=len(groups)))
            SBANKS = [(3, 6), (3, 6), (3, 6)]

            def d_s1(g):
                h, ri, r, c, blks, gi = g["h"], g["ri"], g["r"], g["c"], g["blks"], g["gi"]
                hp = slice(64 * h, 64 * h + 64)
                units = []
                for n in blks:
                    units += [(n - 1, n), (n, n)]
                g["uinfo"] = []
                g["tls"] = []
                for t_i in range(0, len(units), 4):
                    tu = units[t_i:t_i + 4]
                    bank = SBANKS[gi % 3][t_i // 4]
                    pslot = (gi % 3) * 2 + t_i // 4
                    lo = None
                    for u, (kb, n) in enumerate(tu):
                        co = u * 128
                        g["uinfo"].append((pslot, co, kb))
                        if kb < 0:
                            continue
                        if lo is None:
                            lo = co
                        qstart = c + r * 128 * n - t0
                        kstart = c + r * 128 * kb
                        role = u % 2
                        s.add("pe", (lambda bank, co, kstart, qstart, r, hp: lambda e: e.matmul(
                            ps[bank][:, co:co + 128], KTd[hp, ss(kstart, 128, r)], QTd[hp, ss(qstart, 128, r)],
                            start=True, stop=True))(bank, co, kstart, qstart, r, hp),
                            reads=[K("KTd", kstart // SC), K("QTd")], writes=[("ps", bank)])
                    g["tls"].append((bank, pslot, lo, len(tu) * 128))

            def d_s2(g):
                for (bank, pslot, lo, hi) in g["tls"]:
                    s.add("act", (lambda bank, pslot, lo, hi: lambda e: e.activation(out=P16[pslot][:, lo:hi], in_=ps[bank][:, lo:hi], func=AF.Exp))(bank, pslot, lo, hi),
                          reads=[("ps", bank)], writes=[K("P", pslot)])
                    for half in range(0, hi, 256):
                        a0 = max(half, lo)
                        a1 = half + 256
                        bo = a0 - half
                        s.add("pool", (lambda pslot, a0, a1, bo, ri, h: lambda e: e.tensor_tensor(out=P16[pslot][:, a0:a1], in0=P16[pslot][:, a0:a1], in1=biasm16[:, ri, h, bo:256], op=ALU.mult))(
                            pslot, a0, a1, bo, g["ri"], g["h"]),
                            reads=[K("P", pslot), K("ebias")], writes=[K("P", pslot)])

            def d_s3(g):
                h, r, c, blks, gi = g["h"], g["r"], g["c"], g["blks"], g["gi"]
                ob = 7
                vkey = {1: "Vd1", 4: "Vd4", 16: "Vd16"}[r]
                for bi, n in enumerate(blks):
                    first = True
                    for uu in (2 * bi, 2 * bi + 1):
                        pslot, co, kb = g["uinfo"][uu]
                        if kb < 0:
                            continue
                        if r == 1:
                            vap = Vd1[:, kb, h, 0:65]
                        elif r == 4:
                            vap = Vd4[:, (kb % 8) * 4 + c, h, 0:65]
                        else:
                            vap = Vd16[:, (kb % 2) * 16 + c, h, 0:65]
                        last = (uu == 2 * bi + 1)
                        s.add("pe", (lambda ob, bi, vap, pslot, co, first, last: lambda e: e.matmul(
                            ps[ob][0:65, bi * 128:(bi + 1) * 128], vap, P16[pslot][:, co:co + 128],
                            start=first, stop=last))(ob, bi, vap, pslot, co, first, last),
                            reads=[K("P", pslot), K(vkey, kb if r == 1 else (kb % 8 if r == 4 else kb % 2)), K("vones")], writes=[("ps", ob)])
                        first = False

            def d_s4(g):
                h, r, c, blks, gi, g0 = g["h"], g["r"], g["c"], g["blks"], g["gi"], g["g0"]
                ob = 7
                nb_ = len(blks)
                col = c + r * (128 * g0) - t0
                if r == 1:
                    s.add("dve", (lambda h, col, nb_, ob: lambda e: e.tensor_copy(out=acc[h][0:65, col:col + 128 * nb_], in_=ps[ob][0:65, 0:128 * nb_]))(h, col, nb_, ob),
                          reads=[("ps", ob)], writes=[K("acc", h)])
                else:
                    s.add("dve", (lambda h, col, nb_, r, ob: lambda e: e.tensor_tensor(
                        out=acc[h][0:65, ss(col, 128 * nb_, r)], in0=ps[ob][0:65, 0:128 * nb_], in1=acc[h][0:65, ss(col, 128 * nb_, r)], op=ALU.add))(h, col, nb_, r, ob),
                        reads=[("ps", ob), K("acc", h)], writes=[K("acc", h)])

            for g in groups:
                d_s1(g)
                yield
                d_s2(g)
                yield
                d_s3(g)
                yield
                d_s4(g)
                yield
            for h in range(2):
                for tq in range(4):
                    yield
                    cs = slice(tq * 512, tq * 512 + 512)
                    xi = nextX()
                    s.add("pe", (lambda xi, h, cs: lambda e: e.matmul(psX[xi][0:64, :], sel65[0:65, :], acc[h][0:65, cs], start=True, stop=True))(xi, h, cs),
                          reads=[K("acc", h), K("c")], writes=[("ps", 6 + xi)])
                    s.add("dve", (lambda xi: lambda e: e.reciprocal(out=r32[0:64, :], in_=psX[xi][0:64, :]))(xi), reads=[("ps", 6 + xi)], writes=[K("r32")])
                    s.add("dve", (lambda h, cs: lambda e: e.tensor_tensor(out=o32[0:64, :], in0=acc[h][0:64, cs], in1=r32[0:64, :], op=ALU.mult))(h, cs),
                          reads=[K("acc", h), K("r32")], writes=[K("o32")])
                    s.add("act", lambda e: e.activation(out=sq32[0:64, :], in_=o32[0:64, :], func=AF.Square), reads=[K("o32")], writes=[K("sq32")])
                    xi2 = nextX()
                    s.add("pe", (lambda xi2: lambda e: e.matmul(psX[xi2][0:64, :], ones65[0:64, :], sq32[0:64, :], start=True, stop=True))(xi2),
                          reads=[K("sq32"), K("c")], writes=[("ps", 6 + xi2)])
                    s.add("act", (lambda xi2: lambda e: e.activation(out=r32[0:64, :], in_=psX[xi2][0:64, :], func=AF.Sqrt, scale=1.0 / 64, bias=1e-6))(xi2),
                          reads=[("ps", 6 + xi2)], writes=[K("r32")])
                    s.add("dve", lambda e: e.reciprocal(out=sq32[0:64, :], in_=r32[0:64, :]), reads=[K("r32")], writes=[K("sq32")])
                    yi = ycnt[0] % 2
                    ycnt[0] += 1
                    s.add("dve", (lambda yi, h: lambda e: e.scalar_tensor_tensor(out=Yb[yi][0:64, :], in0=o32[0:64, :], scalar=gvec[0:64, 1 + h:2 + h],
                                                                                  in1=sq32[0:64, :], op0=ALU.mult, op1=ALU.mult))(yi, h),
                          reads=[K("o32"), K("sq32"), K("c")], writes=[K("Yb", yi)])
                    od = dma("sp", D["yA"][sc_i][128 + 64 * h:128 + 64 * h + 64, tq * 512:tq * 512 + 512], Yb[yi][0:64, :], [K("Yb", yi)], [K("yA", sc_i)], K("yout", yi))
                    out_dmas.append(od)


        def run_sb(drip):
            jobs = []
            for qt in range(4 * sc_i, 4 * sc_i + 4):
                kbl = list(range(4 * qt + 3, -1, -1))
                for ki, kb in enumerate(kbl):
                    for h in range(2):
                        jobs.append(dict(qt=qt, kb=kb, h=h, first=(ki == 0), last=(ki == len(kbl) - 1), idx=len(jobs)))

            def stage1(j):
                qt, kb, h, i = j["qt"], j["kb"], j["h"], j["idx"]
                hp = slice(64 * h, 64 * h + 64)
                a = kb - 4 * qt
                bb, se, sl = i % 3, i % 2, i % 4
                ql = (qt - 4 * sc_i) * 512
                diag = a >= 0
                kk = K("KTs", (kb * 128) // SC)
                s.add("pe", lambda e: e.matmul(ps[bb][:, :], KTs[hp, kb * 128:(kb + 1) * 128], QTs2[sc_i % 2][hp, ql:ql + 512], start=True, stop=not diag),
                      reads=[kk, K("QTs", sc_i % 2)], writes=[("ps", bb)])
                if diag:
                    s.add("pe", lambda e: e.matmul(ps[bb][:, :], ident, sbmask[:, a, :], start=False, stop=True), reads=[K("c")], writes=[("ps", bb)])
                s.add("act", lambda e: e.activation(out=E32[se], in_=ps[bb][:, :], func=AF.Exp), reads=[("ps", bb)], writes=[K("E", se)])
                s.add("act", lambda e: e.activation(out=Lp16[sl], in_=E32[se], func=AF.Ln, bias=1.0), reads=[K("E", se)], writes=[K("Lp", sl)])

            def stage2(j):
                qt, kb, h, i = j["qt"], j["kb"], j["h"], j["idx"]
                bb, sl, sla, sa16 = i % 3, i % 4, i % 2, i % 4
                cb = 5
                s.add("pe", lambda e: e.matmul(ps[bb][:, :], negtri, Lp16[sl], start=False, stop=True, skip_group_check=True), reads=[K("Lp", sl), K("c")], writes=[("ps", bb)])
                if not j["last"]:
                    s.add("pe", lambda e: e.matmul(ps[cb][:, :], negones, Lp16[sl], start=True, stop=True), reads=[K("Lp", sl), K("c")], writes=[("ps", cb)])
                if j["first"]:
                    s.add("act", lambda e: e.activation(out=A16[sa16], in_=ps[bb][:, :], func=AF.Exp), reads=[("ps", bb)], writes=[K("A", sa16)])
                    if not j["last"]:
                        s.add("dve", lambda e: e.tensor_copy(out=ncar[h], in_=ps[cb][:, :]), reads=[("ps", cb)], writes=[K("ncar", h)])
                else:
                    s.add("dve", lambda e: e.tensor_tensor(out=LA32[sla], in0=ps[bb][:, :], in1=ncar[h], op=ALU.add),
                          reads=[("ps", bb), K("ncar", h)], writes=[K("LA", sla)])
                    if not j["last"]:
                        s.add("dve", lambda e: e.tensor_tensor(out=ncar[h], in0=ps[cb][:, :], in1=ncar[h], op=ALU.add),
                              reads=[("ps", cb), K("ncar", h)], writes=[K("ncar", h)])
                    s.add("act", lambda e: e.activation(out=A16[sa16], in_=LA32[sla], func=AF.Exp), reads=[K("LA", sla)], writes=[K("A", sa16)])

            def stage3(j):
                qt, kb, h, i = j["qt"], j["kb"], j["h"], j["idx"]
                sa16 = i % 4
                s.add("pe", lambda e: e.matmul(ps[4][64 * h:64 * h + 64, :], Vs[:, kb, 64 * h:64 * h + 64], A16[sa16],
                                               start=j["first"], stop=j["last"]),
                      reads=[K("A", sa16), K("Vs", kb)], writes=[("ps", 4)])
                if j["last"] and h == 1:
                    finalize_sb(qt)

            def finalize_sb(qt):
                s.add("dve", lambda e: e.tensor_copy(out=o32, in_=ps[4][:, :]), reads=[("ps", 4)], writes=[K("o32")])
                s.add("act", lambda e: e.activation(out=sq32, in_=o32, func=AF.Square), reads=[K("o32")], writes=[K("sq32")])
                s.add("pe", lambda e: e.matmul(ps[5][:, :], onesblk, sq32, start=True, stop=True), reads=[K("sq32"), K("c")], writes=[("ps", 5)])
                s.add("act", lambda e: e.activation(out=r32, in_=ps[5][:, :], func=AF.Sqrt, scale=1.0 / 64, bias=1e-6), reads=[("ps", 5)], writes=[K("r32")])
                s.add("dve", lambda e: e.reciprocal(out=sq32, in_=r32), reads=[K("r32")], writes=[K("sq32")])
                yi = ycnt[0] % 2
                ycnt[0] += 1
                s.add("dve", lambda e: e.scalar_tensor_tensor(out=Yb[yi], in0=o32, scalar=gvec[:, 0:1], in1=sq32, op0=ALU.mult, op1=ALU.mult),
                      reads=[K("o32"), K("sq32"), K("c")], writes=[K("Yb", yi)])
                od = dma("sp", D["yA"][sc_i][0:128, (qt % 4) * 512:(qt % 4) * 512 + 512], Yb[yi], [K("Yb", yi)], [K("yA", sc_i)], K("yout", yi))
                out_dmas.append(od)

            n = len(jobs)
            for step in range(n + 2):
                if step < n:
                    stage1(jobs[step])
                if 0 <= step - 1 < n:
                    stage2(jobs[step - 1])
                if 0 <= step - 2 < n:
                    stage3(jobs[step - 2])
                drip(n)
        return prep, run_sb

    PREP_STEPS = 260
    mk = [make(i) for i in range(NSC)]
    g0 = mk[0][0]()
    for _ in g0:
        pass
    for sc_i in range(NSC):
        nxt = mk[sc_i + 1][0]() if sc_i + 1 < NSC else None
        state = [nxt, 0.0]

        def drip(njobs, state=state):
            if state[0] is None:
                return
            state[1] += PREP_STEPS / float(njobs)
            while state[1] >= 1.0 and state[0] is not None:
                state[1] -= 1.0
                try:
                    next(state[0])
                except StopIteration:
                    state[0] = None
        mk[sc_i][1](drip)
        if nxt is not None:
            for _ in nxt:
                pass
        if cc is not None:
            cc(sc_i)
    return out_dmas


import numpy as np
import ml_dtypes
import concourse.bass as bass
import concourse.mybir as mybir

ALPHA = (2.0 * 2) ** 0.25
LN_EPS = 1e-5
AX = mybir.AxisListType


def emit_B(nc, s, ar, ps, D, T, lname, NE=16, st=None, last=True, cc2=None):
    L = lname
    K = lambda *a: (L,) + a
    NT = T // 128
    TT = min(512, T)
    NT4 = T // TT
    NSUB = TT // 128

    yacc = ar.take(NT * 1024, F32).rearrange("p (t d) -> p t d", t=NT)
    X1T = ar.take(8 * T).rearrange("p (c t) -> p c t", c=8)
    wslot = [ar.take(3 * 4096) for _ in range(2)]
    Wg = [w[:, 0:4096].rearrange("p (c f) -> p c f", c=8) for w in wslot]
    Wu = [w[:, 4096:8192].rearrange("p (c f) -> p c f", c=8) for w in wslot]
    Wd = [w[:, 8192:12288].rearrange("p (c f) -> p c f", c=4) for w in wslot]
    Wo = wslot[1][:, 0:8192].rearrange("p (c f) -> p c f", c=8)
    YT = [ar.take(8 * 128).rearrange("p (c t) -> p c t", c=8) for _ in range(2)]
    xt = [ar.take(1024, F32) for _ in range(2)]
    v32s = [ar.take(1024, F32) for _ in range(2)]
    lnp = [ar.take(1024, F32) for _ in range(2)]
    X1T32s = [ar.take(8 * 128, F32).rearrange("p (c t) -> p c t", c=8) for _ in range(2)]
    hT = [ar.take(4 * TT).rearrange("p (c t) -> p c t", c=4) for _ in range(2)]
    sg = [ar.take(TT, F32) for _ in range(2)]
    gates = ar.take(NT * 16, F32).rearrange("p (t e) -> p t e", t=NT)
    Wr = ar.take(8 * 16, F32).rearrange("p (c e) -> p c e", c=8)
    br = ar.take(16, F32)
    ident = ar.take(128, F32)
    stats2 = [ar.take(16, F32) for _ in range(2)]
    mv2 = [ar.take(4, F32) for _ in range(2)]
    rt2 = [[ar.take(16, F32) for _ in range(6)] for _ in range(2)]
    rs2 = [[ar.take(4, F32) for _ in range(6)] for _ in range(2)]
    xstp = ar.take(8 * 512).rearrange("p (c t) -> p c t", c=8)

    def dma(eng, out, in_, reads, writes, grp):
        return s.add(eng, lambda e: e.dma_start(out=out, in_=in_), reads=reads, writes=writes, grp=grp)

    for i, nm in enumerate(("ln1g", "ln1b")):
        dma("sp", lnp[i], D[nm], [], [K("lnp")], K("lnp"))
    for dc in range(8):
        dma("sp", Wr[:, dc, :], D["wr"][dc * 128:(dc + 1) * 128, :], [], [K("c")], K("c"))
    dma("sp", br, D["br"], [], [K("c")], K("c"))
    dma("sp", ident, D["ident32"], [], [K("c")], K("c"))
    for ec in range(8):
        dma("pool", Wo[:, ec, :], D["wo"][ec * 128:(ec + 1) * 128, :], [], [K("wslot", 1)], K("wslot", 1))

    def load_unit(u):
        e, fh = u // 2, u % 2
        sl = u % 2
        key = K("wslot", sl)
        for dc in range(8):
            dma("pool", Wg[sl][:, dc, :], D["wg"][e, dc * 128:(dc + 1) * 128, fh * 512:(fh + 1) * 512], [], [key], key)
        for dc in range(8):
            dma("pool", Wu[sl][:, dc, :], D["wu"][e, dc * 128:(dc + 1) * 128, fh * 512:(fh + 1) * 512], [], [key], key)
        for fc in range(4):
            dma("pool", Wd[sl][:, fc, :], D["wd"][e, fh * 512 + fc * 128:fh * 512 + (fc + 1) * 128, :], [], [key], key)

    load_unit(0)

    if D.get("Gall") is not None:
        s.add("sp", lambda e: e.dma_start(out=D["Gloc"], in_=D["Gall"][bass.ds(e.snap(st["reg"]), 1024), :]),
              reads=[K("G", q_) for q_ in range(4)], writes=[K("Gloc")], grp=K("Gloc"))
    def ln_stats(src, sl):
        for hh in range(2):
            s.add("dve", (lambda hh: lambda e: e.bn_stats(out=stats2[sl][:, hh * 6:(hh + 1) * 6], in_=src[:, hh * 512:(hh + 1) * 512]))(hh),
                  reads=[K("lnsrc", sl)], writes=[K("stats", sl)])
        s.add("dve", lambda e: e.bn_aggr(out=mv2[sl][:, 0:2], in_=stats2[sl][:, 0:12]), reads=[K("stats", sl)], writes=[K("mv", sl)])
        s.add("act", lambda e: e.activation(out=mv2[sl][:, 2:3], in_=mv2[sl][:, 1:2], func=AF.Sqrt, bias=LN_EPS), reads=[K("mv", sl)], writes=[K("mvb", sl)])

    def ln_apply(src, dst, gi, bi, dkey, sl):
        s.add("dve", lambda e: e.reciprocal(out=mv2[sl][:, 3:4], in_=mv2[sl][:, 2:3]), reads=[K("mvb", sl)], writes=[K("mvc", sl)])
        s.add("dve", lambda e: e.tensor_scalar(out=src, in0=src, scalar1=mv2[sl][:, 0:1], scalar2=mv2[sl][:, 3:4], op0=ALU.subtract, op1=ALU.mult),
              reads=[K("mv", sl), K("mvc", sl), K("lnsrc", sl)], writes=[K("lnsrc", sl)])
        s.add("dve", lambda e: e.tensor_tensor(out=src, in0=src, in1=lnp[gi], op=ALU.mult), reads=[K("lnsrc", sl), K("lnp")], writes=[K("lnsrc", sl)])
        s.add("dve", lambda e: e.tensor_tensor(out=dst, in0=src, in1=lnp[bi], op=ALU.add), reads=[K("lnsrc", sl), K("lnp")], writes=[dkey])

    def loads(ti):
        sl = ti % 2
        for ec in range(8):
            dma("sp", YT[sl][:, ec, :], D["Gloc"][ec * 128:(ec + 1) * 128, ti * 128:(ti + 1) * 128], [K("Gloc")], [K("YT", sl)], K("YT", sl))
        dma("sp", xt[sl], D["x"][ti * 128:(ti + 1) * 128, :], [("x2s",)], [K("xt", sl)], K("xt", sl))

    def phase1_gen(ti):
        sl = ti % 2
        pb = 4 * sl
        v32 = v32s[sl]
        X1T32 = X1T32s[sl]
        loads(ti)
        yield
        for hh in range(2):
            for ec in range(8):
                s.add("pe", (lambda hh, ec: lambda e: e.matmul(ps[pb + hh][:, :], YT[sl][:, ec, :], Wo[:, ec, hh * 512:(hh + 1) * 512],
                                                               start=(ec == 0), stop=(ec == 7)))(hh, ec),
                      reads=[K("YT", sl), K("wslot", 1)], writes=[("ps", pb + hh)])
            s.add("dve", (lambda hh: lambda e: e.scalar_tensor_tensor(out=v32[:, hh * 512:(hh + 1) * 512], in0=xt[sl][:, hh * 512:(hh + 1) * 512],
                                                                      scalar=float(ALPHA), in1=ps[pb + hh][:, :], op0=ALU.mult, op1=ALU.add))(hh),
                  reads=[K("xt", sl), ("ps", pb + hh)], writes=[K("lnsrc", sl)])
        yield
        ln_stats(v32, sl)
        yield
        ln_apply(v32, xt[sl], 0, 1, K("xt", sl), sl)
        s.add("act", lambda e: e.activation(out=yacc[:, ti, :], in_=xt[sl], func=AF.Copy, scale=float(ALPHA)),
              reads=[K("xt", sl)], writes=[K("yacc", ti)])
        yield
        for q in range(2):
            for kq in range(4):
                dc = q * 4 + kq
                s.add("pe", (lambda q, kq, dc: lambda e: e.transpose(out=ps[pb + 2 + q][:, kq * 128:(kq + 1) * 128], in_=xt[sl][:, dc * 128:(dc + 1) * 128], identity=ident))(q, kq, dc),
                      reads=[K("xt", sl), K("c")], writes=[("ps", pb + 2 + q)])
            yield
            s.add("dve", (lambda q: lambda e: e.tensor_copy(out=X1T32[:, q * 4:(q + 1) * 4, :], in_=ps[pb + 2 + q][:, :].rearrange("p (c t) -> p c t", c=4)))(q),
                  reads=[("ps", pb + 2 + q)], writes=[K("X1T32", sl, q)])
            s.add("act", (lambda q: lambda e: e.activation(out=X1T[:, q * 4:(q + 1) * 4, ti * 128:(ti + 1) * 128], in_=X1T32[:, q * 4:(q + 1) * 4, :], func=AF.Copy))(q),
                  reads=[K("X1T32", sl, q)], writes=[K("X1T", ti)])
        yield
        for dc in range(8):
            s.add("pe", (lambda dc: lambda e: e.matmul(ps[pb][:, 0:16], X1T32[:, dc, :], Wr[:, dc, :], start=(dc == 0), stop=(dc == 7)))(dc),
                  reads=[K("X1T32", sl, dc // 4), K("c")], writes=[("ps", pb)])
        yield
        lg, ex, eq, p2, selm, msk = rt2[sl]
        v1, v2, gs, gsel, gmx, den = rs2[sl]
        R = K("rt", sl)
        s.add("dve", lambda e: e.tensor_tensor(out=lg, in0=ps[pb][:, 0:16], in1=br, op=ALU.add), reads=[("ps", pb), K("c")], writes=[R])
        s.add("dve", lambda e: e.tensor_reduce(out=gmx[:, 0:1], in_=lg, axis=AX.X, op=ALU.max), reads=[R], writes=[R])
        s.add("dve", lambda e: e.tensor_scalar(out=lg, in0=lg, scalar1=gmx[:, 0:1], scalar2=None, op0=ALU.subtract), reads=[R], writes=[R])
        s.add("act", lambda e: e.activation(out=ex, in_=lg, func=AF.Exp), reads=[R], writes=[K("rtb", sl)])
        yield
        R2 = K("rtb", sl)
        ex3 = ex.rearrange("p (g k) -> p g k", g=4)
        eq3 = eq.rearrange("p (g k) -> p g k", g=4)
        p23 = p2.rearrange("p (g k) -> p g k", g=4)
        sel3 = selm.rearrange("p (g k) -> p g k", g=4)
        msk3 = msk.rearrange("p (g k) -> p g k", g=4)
        s.add("dve", lambda e: e.tensor_reduce(out=v1, in_=ex3, axis=AX.X, op=ALU.max), reads=[R2], writes=[R2])
        s.add("dve", lambda e: e.tensor_tensor(out=eq3, in0=ex3, in1=v1.unsqueeze(2).to_broadcast([128, 4, 4]), op=ALU.is_equal), reads=[R2], writes=[R2])
        s.add("dve", lambda e: e.scalar_tensor_tensor(out=p2, in0=eq, scalar=-2.0, in1=ex, op0=ALU.mult, op1=ALU.add), reads=[R2], writes=[R2])
        s.add("dve", lambda e: e.tensor_reduce(out=v2, in_=p23, axis=AX.X, op=ALU.max), reads=[R2], writes=[R2])
        s.add("dve", lambda e: e.tensor_tensor(out=gs, in0=v1, in1=v2, op=ALU.add), reads=[R2], writes=[R2])
        s.add("dve", lambda e: e.tensor_reduce(out=gmx[:, 1:2], in_=gs, axis=AX.X, op=ALU.max), reads=[R2], writes=[R2])
        yield
        s.add("dve", lambda e: e.tensor_scalar(out=gsel, in0=gs, scalar1=gmx[:, 1:2], scalar2=None, op0=ALU.is_equal), reads=[R2], writes=[R2])
        s.add("dve", lambda e: e.tensor_tensor(out=sel3, in0=ex3, in1=v2.unsqueeze(2).to_broadcast([128, 4, 4]), op=ALU.is_ge), reads=[R2], writes=[R2])
        s.add("dve", lambda e: e.tensor_tensor(out=msk3, in0=sel3, in1=gsel.unsqueeze(2).to_broadcast([128, 4, 4]), op=ALU.mult), reads=[R2], writes=[R2])
        s.add("dve", lambda e: e.tensor_tensor(out=v1, in0=gs, in1=gsel, op=ALU.mult), reads=[R2], writes=[R2])
        s.add("dve", lambda e: e.tensor_reduce(out=den[:, 0:1], in_=v1, axis=AX.X, op=ALU.add), reads=[R2], writes=[R2])
        s.add("dve", lambda e: e.reciprocal(out=den[:, 1:2], in_=den[:, 0:1]), reads=[R2], writes=[R2])
        s.add("dve", lambda e: e.tensor_tensor(out=msk, in0=msk, in1=ex, op=ALU.mult), reads=[R2], writes=[R2])
        s.add("dve", lambda e: e.tensor_scalar(out=gates[:, ti, :], in0=msk, scalar1=den[:, 1:2], scalar2=None, op0=ALU.mult),
              reads=[R2], writes=[K("gates", ti)])

    def run_two(gens):
        gens = list(gens)
        active = [None, None]
        nxt = [0]

        def refill(slot):
            for j in range(nxt[0], len(gens)):
                if gens[j] is not None and gens[j][0] % 2 == slot:
                    g = gens[j][1]
                    gens[j] = None
                    return g
            return None
        active[0] = refill(0)
        active[1] = refill(1)
        for _ in range(3):
            if active[0] is not None:
                try:
                    next(active[0])
                except StopIteration:
                    active[0] = refill(0)
        while active[0] is not None or active[1] is not None:
            for slot in (0, 1):
                if active[slot] is None:
                    continue
                try:
                    next(active[slot])
                except StopIteration:
                    active[slot] = refill(slot)

    run_two([(ti, phase1_gen(ti)) for ti in range(NT)])
    dma("sp", lnp[0], D["ln2g"], [], [K("lnp")], K("lnp"))
    dma("sp", lnp[1], D["ln2b"], [], [K("lnp")], K("lnp"))

    NU = NE * 2
    hcnt = [0]
    for u in range(NU):
        e_, fh = u // 2, u % 2
        sl = u % 2
        if u + 1 < NU:
            load_unit(u + 1)
        wkey = K("wslot", sl)
        for t4 in range(NT4):
            hs = hcnt[0] % 2
            hcnt[0] += 1
            tok = slice(t4 * TT, (t4 + 1) * TT)
            for fc in range(4):
                gi = 0 + (fc % 2)
                ui = 2 + (fc % 2)
                for dc in range(8):
                    s.add("pe", (lambda sl, dc, fc, gi, tok: lambda e: e.matmul(ps[gi][:, 0:TT], Wg[sl][:, dc, fc * 128:(fc + 1) * 128], X1T[:, dc, tok],
                                                                               start=(dc == 0), stop=(dc == 7)))(sl, dc, fc, gi, tok),
                          reads=[wkey] + [K("X1T", t4 * NSUB + i) for i in range(NSUB)], writes=[("ps", gi)])
                for dc in range(8):
                    s.add("pe", (lambda sl, dc, fc, ui, tok: lambda e: e.matmul(ps[ui][:, 0:TT], Wu[sl][:, dc, fc * 128:(fc + 1) * 128], X1T[:, dc, tok],
                                                                               start=(dc == 0), stop=(dc == 7)))(sl, dc, fc, ui, tok),
                          reads=[wkey] + [K("X1T", t4 * NSUB + i) for i in range(NSUB)], writes=[("ps", ui)])
                si = fc % 2
                s.add("act", (lambda gi, si: lambda e: e.activation(out=sg[si][:, 0:TT], in_=ps[gi][:, 0:TT], func=AF.Silu))(gi, si),
                      reads=[("ps", gi)], writes=[K("sg", si)])
                s.add("dve", (lambda hs, fc, si, ui: lambda e: e.tensor_tensor(out=hT[hs][:, fc, :], in0=sg[si][:, 0:TT], in1=ps[ui][:, 0:TT], op=ALU.mult))(hs, fc, si, ui),
                      reads=[K("sg", si), ("ps", ui)], writes=[K("hT", hs, fc)])
            for sub in range(NSUB):
                ti = t4 * NSUB + sub
                for hh in range(2):
                    di = 4 + ((sub * 2 + hh) % 2)
                    for fc in range(4):
                        s.add("pe", (lambda sl, hs, fc, sub, hh, di: lambda e: e.matmul(ps[di][:, :], hT[hs][:, fc, sub * 128:(sub + 1) * 128], Wd[sl][:, fc, hh * 512:(hh + 1) * 512],
                                                                                         start=(fc == 0), stop=(fc == 3)))(sl, hs, fc, sub, hh, di),
                              reads=[wkey, K("hT", hs, fc)], writes=[("ps", di)])
                    s.add("dve", (lambda ti, hh, di, e_: lambda e: e.scalar_tensor_tensor(out=yacc[:, ti, hh * 512:(hh + 1) * 512], in0=ps[di][:, :], scalar=gates[:, ti, e_:e_ + 1],
                                                                                          in1=yacc[:, ti, hh * 512:(hh + 1) * 512], op0=ALU.mult, op1=ALU.add))(ti, hh, di, e_),
                          reads=[("ps", di), K("gates", ti), K("yacc", ti)], writes=[K("yacc", ti)])

    outs = []

    def ln2_gen(ti):
        sl = ti % 2
        pb = 4 * sl
        v32 = v32s[sl]
        s.add("dve", lambda e: e.tensor_copy(out=v32, in_=yacc[:, ti, :]), reads=[K("yacc", ti)], writes=[K("lnsrc", sl)])
        ln_stats(v32, sl)
        yield
        ln_apply(v32, xt[sl], 0, 1, K("xt", sl), sl)
        yield
        if last:
            outs.append(dma("sp", D["x2"][ti * 128:(ti + 1) * 128, :], xt[sl], [K("xt", sl)], [], K("x2out", sl)))
        else:
            outs.append(dma("sp", D["x2s"][ti * 128:(ti + 1) * 128, :], xt[sl], [K("xt", sl)], [("x2s",)], K("x2out", sl)))
            for q in range(2):
                for kq in range(4):
                    dc = q * 4 + kq
                    s.add("pe", (lambda q, kq, dc: lambda e: e.transpose(out=ps[pb + 2 + q][:, kq * 128:(kq + 1) * 128], in_=xt[sl][:, dc * 128:(dc + 1) * 128], identity=ident))(q, kq, dc),
                          reads=[K("xt", sl), K("c")], writes=[("ps", pb + 2 + q)])
            yield
            tq = ti % 4
            for q in range(2):
                s.add("dve", (lambda q: lambda e: e.tensor_copy(out=xstp[:, q * 4:(q + 1) * 4, tq * 128:(tq + 1) * 128], in_=ps[pb + 2 + q][:, :].rearrange("p (c t) -> p c t", c=4)))(q),
                      reads=[("ps", pb + 2 + q)], writes=[K("xstp", ti)])

    if last:
        run_two([(ti, ln2_gen(ti)) for ti in range(NT)])
    else:
        for p in range(NT // 4):
            run_two([(ti, ln2_gen(ti)) for ti in range(4 * p, 4 * p + 4)])
            for dc in range(8):
                outs.append(dma("sp", D["X2T"][p][dc * 128:(dc + 1) * 128, :], xstp[:, dc, :], [K("xstp", 4 * p + i) for i in range(4)],
                                [("X2T", p)] + [K("xstp", 4 * p + 4 + i) for i in range(4)], K("x2t")))
            if cc2 is not None:
                cc2(p)
    return outs

I32 = mybir.dt.int32
_GROUPS = [[0, 1, 2, 3], [4, 5, 6, 7]]


def _build_fused(S=8192, T=2048, NE=16, depth=2):
    nc = bass.Bass("TRN2", target_bir_lowering=False, num_devices=8)
    D = {}

    def inp(name, shape, dt=F32):
        D[name] = nc.dram_tensor(name, shape, dt, kind="ExternalInput").ap()
    inp("xT", [1024, S])
    inp("xres", [T, 1024])
    inp("qi", [1, 1], I32)
    inp("wA", [depth * 1024, 768])
    inp("gA", [depth * 256, 1])
    inp("biasm", [3, 2, 128, 256])
    C = consts_A()
    for k, v in C.items():
        inp(k, list(v.shape), BF16 if v.dtype == ml_dtypes.bfloat16 else F32)
    inp("wo", [depth * 1024, 1024])
    inp("lnp", [depth * 4 * 128, 1024])
    inp("wr", [1024, 16])
    inp("br", [128, 16])
    inp("ident32", [128, 128])
    for nm in ("wg", "wu", "wd"):
        inp(nm, [depth * NE, 1024, 1024])
    D["out"] = nc.dram_tensor("out", [T, 1024], F32, kind="ExternalOutput").ap()
    yA = [[nc.dram_tensor("yA%d_%d" % (l, p), [256, T], BF16).ap() for p in range(4)] for l in range(depth)]
    Gall = [nc.dram_tensor("Gall%d" % l, [4096, T], BF16).ap() for l in range(depth)]
    Gloc = [nc.dram_tensor("Gloc%d" % l, [1024, T], BF16).ap() for l in range(depth)]
    X2T = [nc.dram_tensor("X2T%d" % p, [1024, 512], BF16).ap() for p in range(4)]
    GX = [nc.dram_tensor("GX%d" % p, [4096, 512], BF16).ap() for p in range(4)]
    x2s = nc.dram_tensor("x2s", [T, 1024], F32).ap()
    with ExitStack() as es:
        arena = es.enter_context(nc.sbuf_tensor("arena", [128, 103 * 1024], BF16))
        qs = es.enter_context(nc.sbuf_tensor("qs", [1, 1], I32))
        reg = es.enter_context(nc.sync.register("qreg"))
        ps = [es.enter_context(nc.psum_tensor("ps%d" % i, [128, 512], F32)) for i in range(8)]
        s = Sched(nc)
        st = {}
        s.add("sp", lambda e: e.dma_start(out=qs[:, :], in_=D["qi"]), writes=[("qs",)], grp=("qs",))

        def ld(e):
            ins = e.reg_load(reg, qs[0:1, 0:1])
            st["reg"] = reg
            return ins
        s.add("sp", ld, reads=[("qs",)], writes=[("qreg",)])
        outs = []
        ccn = [0]
        for l in range(depth):
            last = (l == depth - 1)
            DA = dict(D)
            DA["w"] = D["wA"][l * 1024:(l + 1) * 1024, :]
            DA["g"] = D["gA"][l * 256:(l + 1) * 256, :]
            DA["yA"] = yA[l]
            DA["GX"] = GX

            def cc1(p, l=l):
                ccn[0] += 1
                s.add("pool", lambda e: e.collective_compute("AllGather", ALU.bypass, replica_groups=_GROUPS, ins=[yA[l][p]],
                                                             outs=[Gall[l][p * 1024:(p + 1) * 1024, :]]),
                      reads=[("A%d" % l, "yA", p)], writes=[("B%d" % l, "G", p)], grp=("cc", ccn[0]), inc=1)
            ar = Arena(arena)
            emit_A(nc, s, ar, ps, DA, S, l == 0, "A%d" % l, cc=cc1)
            s.fence()
            DB = dict(D)
            DB["Gall"] = Gall[l]
            DB["Gloc"] = Gloc[l]
            DB["x"] = D["xres"] if l == 0 else x2s
            DB["wo"] = D["wo"][l * 1024:(l + 1) * 1024, :]
            for i, nm in enumerate(("ln1g", "ln1b", "ln2g", "ln2b")):
                DB[nm] = D["lnp"][(l * 4 + i) * 128:(l * 4 + i + 1) * 128, :]
            for nm in ("wg", "wu", "wd"):
                DB[nm] = D[nm][l * NE:(l + 1) * NE]
            DB["x2"] = D["out"]
            DB["x2s"] = x2s
            DB["X2T"] = X2T

            def cc2(p):
                ccn[0] += 1
                s.add("pool", lambda e: e.collective_compute("AllGather", ALU.bypass, replica_groups=_GROUPS, ins=[X2T[p]], outs=[GX[p]]),
                      reads=[("X2T", p)], writes=[("GX", p)], grp=("cc", ccn[0]), inc=1)
            ar = Arena(arena)
            o = emit_B(nc, s, ar, ps, DB, T, "B%d" % l, NE=NE, st=st, last=last, cc2=None if last else cc2)
            if last:
                outs = o
            else:
                s.fence()
        keys = s.emit(final_wait_ops=outs)
        sems = {k: es.enter_context(nc.semaphore("s%d" % i)) for i, k in enumerate(keys)}
        s.run(sems, final_wait_ops=outs)
    return nc, C, len(s.ops), len(keys)


def kernel(x, w_in, g_sb, g_dil, w_out, ln1_g, ln1_b, ln2_g, ln2_b, rel_bias, w_router, b_router, w_gate, w_up, w_down):
    x = np.asarray(x, np.float32)
    B, S, Dm = x.shape
    depth = w_in.shape[0]
    T = 2048
    NE = w_gate.shape[1]
    nc, C, _, _ = _build_fused(S, T, NE, depth)
    f32 = lambda v: np.asarray(v, np.float32)
    rep = lambda v: np.broadcast_to(f32(v)[None, :], (128, v.shape[0]))
    rel_bias = f32(rel_bias)
    perm = np.concatenate([np.concatenate([np.arange(128 * j, 128 * j + 128), 512 + np.arange(128 * j, 128 * j + 128)]) for j in range(4)])
    wo = np.ascontiguousarray(np.concatenate([f32(w_out[l])[perm, :] for l in range(depth)], axis=0))
    lnp = np.ascontiguousarray(np.concatenate([rep(p[l]) for l in range(depth) for p in (ln1_g, ln1_b, ln2_g, ln2_b)], axis=0))
    shared = {"wo": wo, "lnp": lnp, "wr": np.ascontiguousarray(f32(w_router)), "br": np.ascontiguousarray(rep(b_router)),
              "ident32": np.eye(128, dtype=np.float32),
              "wg": f32(w_gate).reshape(depth * NE, 1024, 1024), "wu": f32(w_up).reshape(depth * NE, 1024, 1024),
              "wd": f32(w_down).reshape(depth * NE, 1024, 1024)}
    shared.update(C)
    xTs = [np.ascontiguousarray(x[b].T) for b in range(B)]
    in_maps = []
    for c in range(8):
        b, j = c // 4, c % 4
        wA = []
        gA = []
        for l in range(depth):
            wl = f32(w_in[l])
            wA.append(np.concatenate([wl[:, o + 128 * j:o + 128 * j + 128] for o in (0, 512, 1536, 2048, 1024, 2560)], axis=1))
            gA.append(np.concatenate([f32(g_sb[l])[128 * j:128 * j + 128], f32(g_dil[l])[128 * j:128 * j + 128]])[:, None])
        m = {"xT": xTs[b], "xres": np.ascontiguousarray(x[b, j * T:(j + 1) * T, :]), "qi": np.array([[1024 * j]], np.int32),
             "wA": np.ascontiguousarray(np.concatenate(wA, axis=0)), "gA": np.ascontiguousarray(np.concatenate(gA, axis=0)),
             "biasm": dil_bias_tables(rel_bias[:, 2 * j:2 * j + 2])}
        m.update(shared)
        in_maps.append(m)
    res = run_bass_kernel_spmd(nc, in_maps, core_ids=list(range(8)))
    out = np.zeros((B, S, Dm), np.float32)
    for c in range(8):
        b, j = c // 4, c % 4
        out[b, j * T:(j + 1) * T, :] = np.asarray(res.results[c]["out"], np.float32)
    return out
```
